# Optimizing a Trainium2 kernel written in Bass

```python
import jax, jax.numpy as jnp
from jax import lax
import numpy as np

D_MODEL = 1024
BATCH = 4
SEQ = 8192
DEPTH = 1

D_MIX = D_MODEL
HEAD_DIM = 64
GDN_HEADS = (D_MIX // 2) // HEAD_DIM
GDN_WIDTH = GDN_HEADS * HEAD_DIM
CONV_K = 4
CHUNK = 64
SWA_Q_HEADS = (D_MIX // 2) // HEAD_DIM
SWA_KV_HEADS = 2
SWA_GROUP = SWA_Q_HEADS // SWA_KV_HEADS
SWA_Q_WIDTH = SWA_Q_HEADS * HEAD_DIM
SWA_KV_WIDTH = SWA_KV_HEADS * HEAD_DIM
WINDOW = 128
BLOCK = 128
ROPE_THETA = 10000.0
N_GROUPS = 4
EXPERTS_PER_GROUP = 8
N_EXPERTS = N_GROUPS * EXPERTS_PER_GROUP
TOP_K = 2
D_EXPERT = D_MODEL // 4
EPS = 1e-6
IN_WIDTHS = (GDN_WIDTH, GDN_WIDTH, GDN_WIDTH, GDN_WIDTH, GDN_HEADS, GDN_HEADS,
             SWA_Q_WIDTH, SWA_KV_WIDTH, SWA_KV_WIDTH)
IN_WIDTH = sum(IN_WIDTHS)
MIX_OUT_WIDTH = GDN_WIDTH + SWA_Q_WIDTH

kernel_name = "hybrid_gdn_swa_sink_hmoe_adaln"


def rms_norm(x, gain):
    xf = x.astype(jnp.float32)
    y = xf * lax.rsqrt(jnp.mean(xf * xf, axis=-1, keepdims=True) + EPS)
    return (y * gain.astype(jnp.float32)).astype(x.dtype)


def l2_norm(x):
    return x * lax.rsqrt(jnp.sum(x * x, axis=-1, keepdims=True) + EPS)


def modulate(x, shift, scale):
    return x * (1 + scale[:, None, :]) + shift[:, None, :]


def causal_conv_silu(x, w):
    seq = x.shape[1]
    xp = jnp.pad(x, ((0, 0), (CONV_K - 1, 0), (0, 0)))
    y = xp[:, 0:seq] * w[0]
    for j in range(1, CONV_K):
        y = y + xp[:, j:j + seq] * w[j]
    return jax.nn.silu(y)


def rope(x, positions):
    half = x.shape[-1] // 2
    inv_freq = jnp.power(jnp.float32(ROPE_THETA), -jnp.arange(half, dtype=jnp.float32) / half)
    ang = positions.astype(jnp.float32)[..., None] * inv_freq
    cos = jnp.cos(ang)[:, :, None, :]
    sin = jnp.sin(ang)[:, :, None, :]
    xf = x.astype(jnp.float32)
    x1, x2 = xf[..., :half], xf[..., half:]
    out = jnp.concatenate([x1 * cos - x2 * sin, x1 * sin + x2 * cos], axis=-1)
    return out.astype(x.dtype)


def chunked_gated_delta_rule(q, k, v, g, beta):
    bsz, seq, heads, dh = q.shape
    n_chunks = seq // CHUNK

    def to_chunks(t):
        return t.reshape(bsz, n_chunks, CHUNK, heads, -1).transpose(0, 3, 1, 2, 4)

    q, k, v = to_chunks(q), to_chunks(k), to_chunks(v)
    g = g.reshape(bsz, n_chunks, CHUNK, heads).transpose(0, 3, 1, 2)
    beta = beta.reshape(bsz, n_chunks, CHUNK, heads).transpose(0, 3, 1, 2)
    G = jnp.cumsum(g, axis=-1)

    idx = jnp.arange(CHUNK)
    lower_incl = idx[:, None] >= idx[None, :]
    strict = idx[:, None] > idx[None, :]
    diff = G[..., :, None] - G[..., None, :]
    decay = jnp.where(lower_incl, jnp.exp(jnp.where(lower_incl, diff, 0.0)), 0.0)

    kb = k * beta[..., None]
    vb = v * beta[..., None]
    m = jnp.where(strict, jnp.einsum('bhncd,bhnsd->bhncs', kb, k) * decay, 0.0)
    eye = jnp.eye(CHUNK, dtype=jnp.float32)
    a_mat = eye + m
    t_mat = lax.linalg.triangular_solve(a_mat, jnp.broadcast_to(eye, a_mat.shape),
                                        left_side=True, lower=True)
    u = jnp.einsum('bhncs,bhnse->bhnce', t_mat, vb)
    w = jnp.einsum('bhncs,bhnsd->bhncd', t_mat, kb * jnp.exp(G)[..., None])
    qg = q * jnp.exp(G)[..., None]
    attn = jnp.einsum('bhncd,bhnsd->bhncs', q, k) * decay
    g_last = G[..., -1]
    k_dec = k * jnp.exp(g_last[..., None] - G)[..., None]

    xs = tuple(jnp.moveaxis(t, 2, 0) for t in (u, w, qg, attn, k_dec, g_last))

    def step(state, inp):
        u_n, w_n, qg_n, attn_n, kdec_n, gl_n = inp
        v_new = u_n - jnp.einsum('bhcd,bhde->bhce', w_n, state)
        o_n = (jnp.einsum('bhcd,bhde->bhce', qg_n, state)
               + jnp.einsum('bhcs,bhse->bhce', attn_n, v_new))
        state = (state * jnp.exp(gl_n)[..., None, None]
                 + jnp.einsum('bhcd,bhce->bhde', kdec_n, v_new))
        return state, o_n

    s0 = jnp.zeros((bsz, heads, dh, v.shape[-1]), jnp.float32)
    _, o = lax.scan(step, s0, xs)
    return o.transpose(1, 0, 3, 2, 4).reshape(bsz, seq, heads, -1)


def gated_deltanet(q_in, k_in, v_in, z, a, b, conv_w, a_log, dt_bias, out_norm):
    bsz, seq, _ = q_in.shape
    qkv = causal_conv_silu(jnp.concatenate([q_in, k_in, v_in], axis=-1), conv_w)
    q, k, v = jnp.split(qkv.astype(jnp.float32), 3, axis=-1)
    q = l2_norm(q.reshape(bsz, seq, GDN_HEADS, HEAD_DIM)) * (HEAD_DIM ** -0.5)
    k = l2_norm(k.reshape(bsz, seq, GDN_HEADS, HEAD_DIM))
    v = v.reshape(bsz, seq, GDN_HEADS, HEAD_DIM)
    g = -jnp.exp(a_log.astype(jnp.float32)) * jax.nn.softplus(
        a.astype(jnp.float32) + dt_bias.astype(jnp.float32))
    beta = jax.nn.sigmoid(b.astype(jnp.float32))
    o = chunked_gated_delta_rule(q, k, v, g, beta)
    zf = z.astype(jnp.float32).reshape(bsz, seq, GDN_HEADS, HEAD_DIM)
    o = rms_norm(o, out_norm) * jax.nn.silu(zf)
    return o.reshape(bsz, seq, GDN_WIDTH).astype(q_in.dtype)


def sliding_window_attention(q, k, v, q_norm, k_norm, sinks, positions):
    bsz, seq, _ = q.shape
    n_blk = seq // BLOCK
    q = rope(rms_norm(q.reshape(bsz, seq, SWA_Q_HEADS, HEAD_DIM), q_norm), positions)
    k = rope(rms_norm(k.reshape(bsz, seq, SWA_KV_HEADS, HEAD_DIM), k_norm), positions)
    v = v.reshape(bsz, seq, SWA_KV_HEADS, HEAD_DIM)

    qb = q.reshape(bsz, n_blk, BLOCK, SWA_KV_HEADS, SWA_GROUP, HEAD_DIM)

    def band(t):
        tb = t.reshape(bsz, n_blk, BLOCK, SWA_KV_HEADS, HEAD_DIM)
        prev = jnp.pad(tb[:, :-1], ((0, 0), (1, 0), (0, 0), (0, 0), (0, 0)))
        return jnp.concatenate([prev, tb], axis=2)

    k_band, v_band = band(k), band(v)
    scores = jnp.einsum('bnqhgd,bnkhd->bnhgqk', qb, k_band).astype(jnp.float32) * (HEAD_DIM ** -0.5)

    qi = jnp.arange(BLOCK)[:, None]
    kj = jnp.arange(2 * BLOCK)[None, :]
    rel = qi + BLOCK - kj
    in_window = (rel >= 0) & (rel < WINDOW)
    has_prev = jnp.arange(n_blk)[:, None, None] > 0
    valid = in_window[None] & (has_prev | (kj >= BLOCK)[None])
    scores = jnp.where(valid[None, :, None, None], scores, -jnp.inf)

    sink = sinks.astype(jnp.float32).reshape(SWA_KV_HEADS, SWA_GROUP)[None, None, :, :, None]
    mx = jnp.maximum(scores.max(axis=-1), sink)
    p = jnp.exp(scores - mx[..., None])
    denom = p.sum(axis=-1) + jnp.exp(sink - mx)
    p = p / denom[..., None]
    out = jnp.einsum('bnhgqk,bnkhd->bnqhgd', p, v_band.astype(jnp.float32))
    return out.reshape(bsz, seq, SWA_Q_WIDTH).astype(q.dtype)


def hierarchical_moe(h, w_group, b_group, w_router, b_router, w_gate, w_up, w_down):
    bsz, seq, d = h.shape
    t = h.reshape(-1, d)
    group_prob = jax.nn.softmax((t @ w_group + b_group).astype(jnp.float32), axis=-1)
    p_group, g_idx = lax.top_k(group_prob, 1)
    expert_logits = (t @ w_router + b_router).astype(jnp.float32).reshape(
        -1, N_GROUPS, EXPERTS_PER_GROUP)
    g_onehot = jax.nn.one_hot(g_idx[:, 0], N_GROUPS, dtype=jnp.float32)
    in_group = jnp.einsum('tg,tge->te', g_onehot, expert_logits)
    p_exp = jax.nn.softmax(in_group, axis=-1)
    w_top, e_idx = lax.top_k(p_exp, TOP_K)
    w_top = w_top / jnp.sum(w_top, axis=-1, keepdims=True) * p_group
    expert_ids = g_idx * EXPERTS_PER_GROUP + e_idx
    combine = jnp.sum(jax.nn.one_hot(expert_ids, N_EXPERTS, dtype=jnp.float32)
                      * w_top[..., None], axis=1)
    out = jnp.zeros(t.shape, jnp.float32)
    for e in range(N_EXPERTS):
        hid = jax.nn.silu(t @ w_gate[e]) * (t @ w_up[e])
        out = out + combine[:, e:e + 1] * (hid @ w_down[e]).astype(jnp.float32)
    return out.astype(h.dtype).reshape(bsz, seq, d)


def setup_inputs(seed: int = 0) -> dict:
    key = jax.random.key(seed)
    ks = jax.random.split(key, 24)
    f32 = jnp.float32
    nrm = lambda k, shape: jax.random.normal(k, shape, f32)
    dt = jnp.exp(jax.random.uniform(ks[6], (DEPTH, GDN_HEADS), f32,
                                    jnp.log(jnp.float32(1e-3)), jnp.log(jnp.float32(1e-1))))
    return {
        "x": nrm(ks[0], (BATCH, SEQ, D_MODEL)),
        "c": nrm(ks[1], (BATCH, D_MODEL)),
        "positions": jnp.broadcast_to(jnp.arange(SEQ, dtype=jnp.int32), (BATCH, SEQ)),
        "w_ada": nrm(ks[2], (DEPTH, D_MODEL, 6 * D_MODEL)) * (0.5 * D_MODEL ** -0.5),
        "b_ada": 0.01 * nrm(ks[3], (DEPTH, 6 * D_MODEL)),
        "norm_mix": 1.0 + 0.1 * nrm(ks[4], (DEPTH, D_MODEL)),
        "w_in": nrm(ks[5], (DEPTH, D_MODEL, IN_WIDTH)) * D_MODEL ** -0.5,
        "conv_w": nrm(ks[7], (DEPTH, CONV_K, 3 * GDN_WIDTH)) * CONV_K ** -0.5,
        "a_log": jnp.log(jax.random.uniform(ks[8], (DEPTH, GDN_HEADS), f32, 1.0, 16.0)),
        "dt_bias": jnp.log(jnp.expm1(dt)),
        "gdn_out_norm": 1.0 + 0.1 * nrm(ks[9], (DEPTH, HEAD_DIM)),
        "q_norm": 1.0 + 0.1 * nrm(ks[10], (DEPTH, HEAD_DIM)),
        "k_norm": 1.0 + 0.1 * nrm(ks[11], (DEPTH, HEAD_DIM)),
        "sinks": nrm(ks[12], (DEPTH, SWA_Q_HEADS)),
        "w_out": nrm(ks[13], (DEPTH, MIX_OUT_WIDTH, D_MODEL)) * MIX_OUT_WIDTH ** -0.5,
        "norm_ffn": 1.0 + 0.1 * nrm(ks[14], (DEPTH, D_MODEL)),
        "w_group": nrm(ks[15], (DEPTH, D_MODEL, N_GROUPS)) * D_MODEL ** -0.5,
        "b_group": 0.01 * nrm(ks[16], (DEPTH, N_GROUPS)),
        "w_router": nrm(ks[17], (DEPTH, D_MODEL, N_EXPERTS)) * D_MODEL ** -0.5,
        "b_router": 0.01 * nrm(ks[18], (DEPTH, N_EXPERTS)),
        "w_gate": nrm(ks[19], (DEPTH, N_EXPERTS, D_MODEL, D_EXPERT)) * D_MODEL ** -0.5,
        "w_up": nrm(ks[20], (DEPTH, N_EXPERTS, D_MODEL, D_EXPERT)) * D_MODEL ** -0.5,
        "w_down": nrm(ks[21], (DEPTH, N_EXPERTS, D_EXPERT, D_MODEL)) * D_EXPERT ** -0.5,
    }


def reference(x, c, positions, w_ada, b_ada, norm_mix, w_in, conv_w, a_log, dt_bias,
              gdn_out_norm, q_norm, k_norm, sinks, w_out, norm_ffn, w_group, b_group,
              w_router, b_router, w_gate, w_up, w_down):
    split_points = tuple(int(s) for s in np.cumsum(IN_WIDTHS)[:-1])
    c_act = jax.nn.silu(c)
    for l in range(DEPTH):
        mod = c_act @ w_ada[l] + b_ada[l]
        shift1, scale1, gate1, shift2, scale2, gate2 = jnp.split(mod, 6, axis=-1)

        h = modulate(rms_norm(x, norm_mix[l]), shift1, scale1)
        proj = h @ w_in[l]
        (g_q, g_k, g_v, g_z, g_a, g_b, s_q, s_k, s_v) = jnp.split(proj, split_points, axis=-1)
        gdn_out = gated_deltanet(g_q, g_k, g_v, g_z, g_a, g_b, conv_w[l], a_log[l],
                                 dt_bias[l], gdn_out_norm[l])
        swa_out = sliding_window_attention(s_q, s_k, s_v, q_norm[l], k_norm[l],
                                           sinks[l], positions)
        mixed = jnp.concatenate([gdn_out, swa_out], axis=-1) @ w_out[l]
        x = x + gate1[:, None, :] * mixed

        h2 = modulate(rms_norm(x, norm_ffn[l]), shift2, scale2)
        ffn = hierarchical_moe(h2, w_group[l], b_group[l], w_router[l], b_router[l],
                               w_gate[l], w_up[l], w_down[l])
        x = x + gate2[:, None, :] * ffn
    return x
```

```python
import numpy as np
import concourse.bass as bass
import concourse.mybir as mybir
from concourse.bass_utils import run_bass_kernel_spmd
from contextlib import ExitStack
import ml_dtypes

F32 = mybir.dt.float32
BF16 = mybir.dt.bfloat16
I32 = mybir.dt.int32
ALU = mybir.AluOpType
AF = mybir.ActivationFunctionType
AX = mybir.AxisListType
NPBF = ml_dtypes.bfloat16


class Buf:
    __slots__ = ("name", "w", "readers", "dsem")

    def __init__(self, name):
        self.name = name
        self.w = None
        self.readers = []
        self.dsem = None


class Prog:
    ENG = ("pe", "act", "dve", "pool", "sp")
    LAT = 150.0

    def __init__(self, nc, es):
        self.nc = nc
        self.es = es
        self.recs = []
        self.cnt = {e: 0 for e in self.ENG}
        self.sems = {}
        self.semcnt = {}
        self.waited = {e: {} for e in self.ENG}
        for e in self.ENG[:4]:
            self._sem("E_" + e)
        self.nd = 0
        self.phase = 0
        self.pending_barrier = False
        self.sched = True
        self.final_wait_bufs = None

    def _sem(self, key):
        if key not in self.sems:
            self.sems[key] = self.es.enter_context(self.nc.semaphore("s_" + key))
            self.semcnt[key] = 0
        return key

    def op(self, eng, fn, reads=(), writes=(), cost=200.0):
        self.recs.append(dict(eng=eng, fn=fn, reads=list(reads), writes=list(writes), cost=float(cost), dma=False))

    def dma(self, fn, reads=(), writes=(), sembuf=None, eng="sp", cost=3000.0, issue=60.0):
        sb = sembuf if sembuf is not None else (writes[0] if writes else reads[0])
        if sb.dsem is None:
            self.nd += 1
            sb.dsem = self._sem("D%d_%s" % (self.nd, sb.name))
        key = sb.dsem
        self.semcnt[key] += 16
        self.recs.append(dict(eng=eng, fn=fn, reads=list(reads), writes=list(writes), cost=float(cost), dma=True,
                              tok=(key, self.semcnt[key]), issue=float(issue)))

    def wait_all(self, eng, bufs):
        self.final_wait_bufs = (eng, list(bufs))

    def barrier(self):
        self.pending_barrier = True

    def emit(self):
        import heapq
        recs = self.recs
        n = len(recs)
        ENG = self.ENG
        preds = [[] for _ in range(n)]
        ph = self.phase
        for i, r in enumerate(recs):
            for b in r["reads"]:
                if b.w is not None and b.w[0] == ph:
                    preds[i].append((b.w[1], "raw"))
            for b in r["writes"]:
                if b.w is not None and b.w[0] == ph:
                    preds[i].append((b.w[1], "waw"))
                for (p2, rid) in b.readers:
                    if p2 == ph and rid != i:
                        preds[i].append((rid, "war"))
            for b in r["reads"]:
                b.readers.append((ph, i))
            for b in r["writes"]:
                b.w = (ph, i)
                b.readers = []
        last_dma = {}
        for i, r in enumerate(recs):
            if r["dma"]:
                if r["eng"] in last_dma:
                    preds[i].append((last_dma[r["eng"]], "issue"))
                last_dma[r["eng"]] = i
        final = None
        if self.final_wait_bufs is not None:
            feng, fb = self.final_wait_bufs
            fp = []
            for b in fb:
                if b.w is not None and b.w[0] == ph:
                    fp.append(b.w[1])
                for (p2, rid) in b.readers:
                    if p2 == ph:
                        fp.append(rid)
            final = (feng, fp)
            self.final_wait_bufs = None
        order = {e: [] for e in ENG}
        if self.sched:
            succ = [[] for _ in range(n)]
            npred = [0] * n
            for i in range(n):
                ps = {}
                for (p, kd) in preds[i]:
                    ps[p] = ps.get(p, True) and kd == "issue"
                npred[i] = len(ps)
                for p, io in ps.items():
                    succ[p].append((i, io))
            finish = [0.0] * n
            startt = [0.0] * n
            ready = [0.0] * n
            heaps = {e: [] for e in ENG}
            free = {e: 0.0 for e in ENG}
            for i in range(n):
                if npred[i] == 0:
                    heapq.heappush(heaps[recs[i]["eng"]], (0.0, i))
            done = 0
            while done < n:
                best = None
                for e in ENG:
                    h = heaps[e]
                    if not h:
                        continue
                    t0 = max(free[e], h[0][0])
                    if best is None or t0 < best[0]:
                        best = (t0, e)
                t0, e = best
                h = heaps[e]
                cands = []
                while h and h[0][0] <= t0:
                    cands.append(heapq.heappop(h))
                cands.sort(key=lambda x: x[1])
                rt_, i = cands[0]
                for c in cands[1:]:
                    heapq.heappush(h, c)
                r = recs[i]
                startt[i] = t0
                if r["dma"]:
                    free[e] = t0 + r["issue"]
                    finish[i] = t0 + r["cost"]
                else:
                    free[e] = t0 + r["cost"]
                    finish[i] = free[e]
                order[e].append(i)
                done += 1
                for (s_, io) in succ[i]:
                    npred[s_] -= 1
                    lat = self.LAT if recs[s_]["eng"] != e or r["dma"] else 60.0
                    if io:
                        ready[s_] = max(ready[s_], t0 + r["issue"])
                    else:
                        ready[s_] = max(ready[s_], finish[i] + lat)
                    if npred[s_] == 0:
                        heapq.heappush(heaps[recs[s_]["eng"]], (ready[s_], s_))
            self.model_time = max(finish) if n else 0.0
            busy = {e: sum(recs[i]['cost'] if not recs[i]['dma'] else recs[i]['issue'] for i in order[e]) for e in ENG}
            print('[sched] phase', self.phase, 'ops', n, 'model_us', round(self.model_time / 1000, 1), 'busy_us', {e: round(v / 1000, 1) for e, v in busy.items()})
        else:
            for i, r in enumerate(recs):
                order[r["eng"]].append(i)
        tok = [None] * n
        for e in ENG:
            c = self.cnt[e]
            for i in order[e]:
                r = recs[i]
                if r["dma"]:
                    tok[i] = r["tok"]
                else:
                    c += 1
                    tok[i] = ("E_" + e, c)
            self.cnt[e] = c
        ins = {e: [] for e in ENG}
        for e in ENG:
            waited = self.waited[e]
            first = True
            for i in order[e]:
                r = recs[i]
                waits = {}
                if first and self.pending_barrier:
                    for key, val in self._barrier_vals.items():
                        if val > 0 and waited.get(key, 0) < val:
                            waited[key] = val
                            waits[key] = val
                first = False
                for (p, kind) in preds[i]:
                    pr = recs[p]
                    if kind == "issue":
                        continue
                    same = (not pr["dma"]) and (not r["dma"]) and pr["eng"] == e
                    if same and (e == "pe" or (kind != "raw" and e != "pool")):
                        continue
                    key, val = tok[p]
                    if waited.get(key, 0) >= val:
                        continue
                    waited[key] = val
                    waits[key] = max(waits.get(key, 0), val)
                if r["dma"]:
                    ins[e].append((list(waits.items()), r["fn"], r["tok"][0], 16))
                else:
                    ins[e].append((list(waits.items()), r["fn"], "E_" + e, 1))
            if first and self.pending_barrier:
                waits = {}
                for key, val in self._barrier_vals.items():
                    if val > 0 and waited.get(key, 0) < val:
                        waited[key] = val
                        waits[key] = val
                if waits:
                    ins[e].append((list(waits.items()), None, None, 0))
        if final is not None:
            feng, fp = final
            waits = {}
            waited = self.waited[feng]
            for p in fp:
                key, val = tok[p]
                if waited.get(key, 0) < val:
                    waited[key] = val
                    waits[key] = max(waits.get(key, 0), val)
            if waits:
                ins[feng].append((list(waits.items()), None, None, 0))
        self.pending_barrier = False
        self._barrier_vals = {}
        for e in ENG[:4]:
            self._barrier_vals["E_" + e] = self.cnt[e]
        for key in self.sems:
            if not key.startswith("E_"):
                self._barrier_vals[key] = self.semcnt[key]
        nc = self.nc
        sems = self.sems
        with nc.Block() as block:
            def run(engname):
                def body(e):
                    for (waits, fn, key, inc) in ins[engname]:
                        for (k, v) in waits:
                            e.wait_ge(sems[k], v)
                        if fn is not None:
                            fn(e).then_inc(sems[key], inc)
                return body
            block.tensor(run("pe"))
            block.scalar(run("act"))
            block.vector(run("dve"))
            block.gpsimd(run("pool"))
            block.sync(run("sp"))
        self.recs = []
        self.phase += 1


NT = 64
OWN0 = 32
NTL = 47
NSUB = NTL * 4
NSLOT = NSUB * 128
EPS = 1e-6
DBG = False
CFG = dict(tiles=None, moe=True, nex=32)


class Cut(Exception):
    pass


def build(dbg=False):
    nc = bass.Bass("TRN2", target_bir_lowering=False)

    def din(name, shape, dt=F32):
        return nc.dram_tensor(name, shape, dt, kind="ExternalInput").ap()

    xg = din("xg", [8192, 1024]); c_col = din("c_col", [128, 8]); pos = din("pos", [128, 33], I32)
    flag = din("flag", [128, 1])
    w_ada = din("w_ada", [1024, 6144]); b_ada = din("b_ada", [1, 6144])
    b_ada_col = din("b_ada_col", [128, 48]); nm_col = din("nm_col", [128, 8]); nf_col = din("nf_col", [128, 8])
    w_in = din("w_in", [1024, 2832]); conv_wT = din("conv_wT", [128, 48]); a_log = din("a_log", [1, 8])
    dt_bias = din("dt_bias", [1, 8]); gon = din("gon", [1, 64]); qnw = din("qnw", [1, 64]); knw = din("knw", [1, 64])
    sinks = din("sinks", [1, 8]); w_out = din("w_out", [1024, 1024])
    w_gr = din("w_gr", [1024, 36]); b_gr = din("b_gr", [1, 36])
    wg_l = din("wg_l", [4096, 2048]); wu_l = din("wu_l", [4096, 2048]); wd_l = din("wd_l", [4096, 2048])
    c_Us = din("c_Us", [128, 128], BF16); c_meta0 = din("c_meta0", [128, 256], I32); c_minit = din("c_minit", [128, NSUB * 4], I32)
    c_thr = din("c_thr", [128, NTL]); c_pcol = din("c_pcol", [128, 1]); c_bnd = din("c_bnd", [128, 4], I32)
    c_ident = din("c_ident", [128, 128], BF16); c_U = din("c_U", [128, 128]); c_B = din("c_B", [128, 128])
    c_ind = din("c_ind", [128, 256]); c_mincl = din("c_mincl", [128, 128]); c_mslow = din("c_mslow", [128, 128])
    c_blk = din("c_blk", [128, 128], BF16); c_m01 = din("c_m01", [128, 256], BF16); c_invf = din("c_invf", [128, 32])
    y = nc.dram_tensor("y", [4096, 1024], F32, kind="ExternalOutput").ap()
    h2r_d = nc.dram_tensor("h2r_d", [4096, 1024], BF16, kind="Internal").ap()
    meta_d = nc.dram_tensor("meta_d", [NSLOT, 4], I32, kind="Internal").ap()
    contrib_d = nc.dram_tensor("contrib_d", [8192, 1024], F32, kind="Internal").ap()
    if dbg:
        dbg_o = nc.dram_tensor("dbg_o", [4096, 1024], F32, kind="ExternalOutput").ap()

    es0 = ExitStack()
    try:
      with es0:
        P = Prog(nc, es0)

        def cut(n):
            if CFG.get("cut") == n:
                P.emit()
                raise Cut()

        def mk(es):
            def sb(name, shape, dt=F32):
                return es.enter_context(nc.sbuf_tensor(name, shape, dt)), Buf(name)

            def ps(name, shape, dt=F32):
                return es.enter_context(nc.psum_tensor(name, shape, dt)), Buf(name)
            return sb, ps

        def fsz(ap):
            n = 1
            for d in ap.shape[1:]:
                n *= int(d)
            return n

        def MM(out, lhsT, rhs, start, stop, r, w):
            n = fsz(out)
            c = (30 + 1.8 * n) if lhsT.dtype == F32 else (30 + 0.45 * n)
            P.op("pe", lambda e: e.matmul(out, lhsT=lhsT, rhs=rhs, start=start, stop=stop), r, w, cost=c)

        def TR(out, in_, ident, r, w):
            P.op("pe", lambda e: e.transpose(out, in_, ident), r, w, cost=90)

        def ACT(out, in_, func, r, w, **kw):
            P.op("act", lambda e: e.activation(out=out, in_=in_, func=func, **kw), r, w, cost=200 + 0.83 * fsz(out))

        def TT(eng, out, in0, in1, op, r, w):
            c = (100 + 1.05 * fsz(out)) if eng == "dve" else (250 + 2.2 * fsz(out))
            P.op(eng, lambda e: e.tensor_tensor(out=out, in0=in0, in1=in1, op=op), r, w, cost=c)

        def TS(out, in0, s1, s2, op0, op1, r, w, eng="dve"):
            c = 100 + 1.05 * fsz(out)
            if op1 is None:
                P.op(eng, lambda e: e.tensor_scalar(out, in0, s1, None, op0=op0), r, w, cost=c)
            else:
                P.op(eng, lambda e: e.tensor_scalar(out, in0, s1, s2, op0=op0, op1=op1), r, w, cost=c)

        def STT(out, in0, scalar, in1, op0, op1, r, w):
            P.op("dve", lambda e: e.scalar_tensor_tensor(out=out, in0=in0, scalar=scalar, in1=in1, op0=op0, op1=op1), r, w,
                 cost=100 + 1.05 * fsz(out))

        def CP(eng, out, in_, r, w):
            c = (100 + 1.0 * fsz(out)) if eng == "dve" else (250 + 2.0 * fsz(out))
            P.op(eng, lambda e: e.tensor_copy(out, in_), r, w, cost=c)

        def RED(out, in_, op, r, w):
            P.op("dve", lambda e: e.tensor_reduce(out=out, in_=in_, axis=AX.X, op=op), r, w, cost=100 + 1.05 * fsz(in_))

        def RCP(out, in_, r, w):
            P.op("dve", lambda e: e.reciprocal(out, in_), r, w, cost=100 + 6.4 * fsz(out))

        def DMA(out, in_, r, w, sembuf=None, eng="sp"):
            nb = fsz(out) * int(out.shape[0]) * (2 if out.dtype == BF16 else 4)
            P.dma(lambda e: e.dma_start(out=out, in_=in_), r, w, sembuf, eng=eng, cost=2000 + nb / 150.0)

        def rsqrt_(dst, src, mul, add, bufs):
            TS(dst, src, mul, add, ALU.mult, ALU.add, bufs, bufs)
            ACT(dst, dst, AF.Sqrt, bufs, bufs)
            RCP(dst, dst, bufs, bufs)

        sb0, _ = mk(es0)
        G1, bG1 = sb0("G1", [128, 1024]); G2, bG2 = sb0("G2", [128, 1024])
        modc, bmodc = sb0("modc", [128, 32])
        epst, beps = sb0("epst", [128, 1])
        OH1, bOH1 = sb0("OH1", [128, 1024], BF16); OH2, bOH2 = sb0("OH2", [128, 1024], BF16)
        W12, bW12 = sb0("W12", [128, 64])
        flg, bflg = sb0("flg", [128, 1])
        by_d = [Buf("yd%d" % t) for t in range(32)]
        ysem = [Buf("ysem%d" % t) for t in range(2)]
        hsem = [Buf("hsem%d" % t) for t in range(2)]
        ysem2 = [Buf("ysemb%d" % t) for t in range(2)]
        bh2d = [Buf("h2d%d" % t) for t in range(32)]

        esW = ExitStack()
        with esW:
            sbW, _ = mk(esW)
            Winb, bWin = sbW("Winb", [128, 8, 2832], BF16)
            Woutb, bWout = sbW("Woutb", [128, 8, 1024], BF16)
            Wgrb, bWgr = sbW("Wgrb", [128, 8, 36], BF16)
            dg, bdg = sbW("dg", [128, 48, 128], BF16)
            ident, bid = sbW("ident", [128, 128], BF16)
            U32, bU = sbW("U32", [128, 128]); B32, bB = sbW("B32", [128, 128]); Cind, bCi = sbW("Cind", [128, 256])
            mincl, bmi = sbW("mincl", [128, 128]); mslow, bms = sbW("mslow", [128, 128])
            blk, bblk = sbW("blk", [128, 128], BF16); m01, bm01 = sbW("m01", [128, 256], BF16)
            invf, binv = sbW("invf", [128, 32])
            cst, bcst = sbW("cst", [128, 512])
            DMA(ident[:], c_ident[:, :], [], [bid]); DMA(U32[:], c_U[:, :], [], [bU]); DMA(B32[:], c_B[:, :], [], [bB])
            DMA(Cind[:], c_ind[:, :], [], [bCi]); DMA(mincl[:], c_mincl[:, :], [], [bmi]); DMA(mslow[:], c_mslow[:, :], [], [bms])
            DMA(blk[:], c_blk[:, :], [], [bblk]); DMA(m01[:], c_m01[:, :], [], [bm01]); DMA(invf[:], c_invf[:, :], [], [binv])
            DMA(flg[:], flag[:, :], [], [bflg])
            cst_l = [Buf("cst%d" % i) for i in range(8)]
            DMA(cst[:, 0:8], dt_bias[0:1, :].partition_broadcast(128), [], [cst_l[0]])
            DMA(cst[:, 8:16], a_log[0:1, :].partition_broadcast(128), [], [cst_l[1]])
            DMA(cst[:, 16:80], gon[0:1, :].partition_broadcast(128), [], [cst_l[2]])
            DMA(cst[:, 80:144], qnw[0:1, :].partition_broadcast(128), [], [cst_l[3]])
            DMA(cst[:, 144:208], knw[0:1, :].partition_broadcast(128), [], [cst_l[4]])
            DMA(cst[:, 208:216], sinks[0:1, :].partition_broadcast(128), [], [cst_l[5]])
            DMA(cst[:, 220:256], b_gr[0:1, :].partition_broadcast(128), [], [cst_l[6]])
            DMA(cst[:, 256:304], conv_wT[:, :], [], [cst_l[7]])
            cb = cst_l + [bcst]

            esP = ExitStack()
            with esP:
                sbP, psP = mk(esP)
                stg = [sbP("stg%d" % i, [128, 8, 512]) for i in range(2)]
                badt, bbad = sbP("badt", [128, 512])
                cact, bcact = sbP("cact", [128, 8]); cbb, bcbb = sbP("cbb", [128, 8, 128])
                pmod = [psP("pmod%d" % i, [128, 512]) for i in range(2)]
                ccol, bccol = sbP("ccol", [128, 8])
                tmpc, btmpc = sbP("tmpc", [128, 64])
                cut(1)
                ACT(cst[:, 8:16], cst[:, 8:16], AF.Exp, cb, cb)
                TS(cst[:, 8:16], cst[:, 8:16], -1.0, None, ALU.mult, None, cb, cb)
                TT("dve", tmpc[:, 0:64], cst[:, 80:144], cst[:, 80:144], ALU.mult, cb, [btmpc])
                RED(cst[:, 304:305], tmpc[:, 0:64], ALU.max, [btmpc], cb)
                TT("dve", tmpc[:, 0:64], cst[:, 144:208], cst[:, 144:208], ALU.mult, cb, [btmpc])
                RED(cst[:, 305:306], tmpc[:, 0:64], ALU.max, [btmpc], cb)
                TT("dve", cst[:, 306:307], cst[:, 304:305], cst[:, 305:306], ALU.mult, cb, cb)
                ACT(cst[:, 306:307], cst[:, 306:307], AF.Sqrt, cb, cb, scale=64.0)
                RED(cst[:, 307:308], cst[:, 208:216], ALU.max, cb, cb)
                TT("dve", cst[:, 306:307], cst[:, 306:307], cst[:, 307:308], ALU.max, cb, cb)
                TS(cst[:, 216:217], cst[:, 306:307], -1.0, None, ALU.mult, None, cb, cb)
                ACT(cst[:, 208:216], cst[:, 208:216], AF.Exp, cb, cb, bias=cst[:, 216:217])
                TS(cst[:, 80:144], cst[:, 80:144], 0.125, None, ALU.mult, None, cb, cb)
                cut(2)
                for jt in range(48):
                    TS(dg[:, jt, :], ident[:], cst[:, 256 + jt:257 + jt], None, ALU.mult, None, [bid] + cb, [bdg])
                cut(3)
                si = 0
                engs = ["act", "dve", "pool"]

                def cast(eng, out, in_, r, w):
                    if eng == "act":
                        ACT(out, in_, AF.Copy, r, w)
                    else:
                        CP(eng, out, in_, r, w)
                w_in_v = w_in.rearrange("(k p) n -> p k n", p=128)
                for cch in range(8):
                    st, bst = stg[si % 2]
                    DMA(st[:, :, 0:354], w_in_v[:, :, cch * 354:(cch + 1) * 354], [], [bst])
                    cast(engs[si % 3], Winb[:, :, cch * 354:(cch + 1) * 354], st[:, :, 0:354], [bst], [bWin])
                    si += 1
                w_out_v = w_out.rearrange("(k p) n -> p k n", p=128)
                for cch in range(2):
                    st, bst = stg[si % 2]
                    DMA(st[:, :, :], w_out_v[:, :, cch * 512:(cch + 1) * 512], [], [bst])
                    cast(engs[si % 3], Woutb[:, :, cch * 512:(cch + 1) * 512], st[:, :, :], [bst], [bWout])
                    si += 1
                st, bst = stg[si % 2]
                DMA(st[:, :, 0:36], w_gr.rearrange("(k p) n -> p k n", p=128), [], [bst])
                cast(engs[si % 3], Wgrb[:, :, :], st[:, :, 0:36], [bst], [bWgr])
                si += 1
                cut(4)
                P.op("pool", lambda e: e.memset(epst[:], EPS), [], [beps])
                DMA(ccol[:], c_col[:, :], [], [bccol])
                ACT(cact[:], ccol[:], AF.Silu, [bccol], [bcact])
                CP("dve", cbb[:], cact[:].unsqueeze(2).to_broadcast([128, 8, 128]), [bcact], [bcbb])
                bcolt, bbcol = sbP("bcolt", [128, 48]); nmc, bnmc = sbP("nmc", [128, 16]); colm, bcolm = sbP("colm", [128, 48])
                DMA(bcolt[:], b_ada_col[:, :], [], [bbcol])
                DMA(nmc[:, 0:8], nm_col[:, :], [], [bnmc]); DMA(nmc[:, 8:16], nf_col[:, :], [], [bnmc])
                pcol, bpcol = psP("pcol", [128, 64])
                w_ada_v = w_ada.rearrange("(k p) n -> p k n", p=128)
                for n in range(12):
                    st, bst = stg[si % 2]
                    si += 1
                    DMA(st[:, :, :], w_ada_v[:, :, n * 512:(n + 1) * 512], [], [bst])
                    if n in (4, 5, 10, 11):
                        DMA(badt[:], b_ada[0:1, n * 512:(n + 1) * 512].partition_broadcast(128), [], [bbad])
                        pm, bpm = pmod[n % 2]
                        for k in range(8):
                            MM(pm[:], cbb[:, k, :], st[:, k, :], k == 0, k == 7, [bcbb, bst], [bpm])
                        d, bd = (G1, bG1) if n < 6 else (G2, bG2)
                        dsl = d[:, (n % 2) * 512:(n % 2) * 512 + 512]
                        TT("dve", dsl, pm[:], badt[:], ALU.add, [bpm, bbad], [bd])
                    else:
                        for cc in range(4):
                            c = n * 4 + cc
                            for k in range(8):
                                MM(pcol[:, c:c + 1], st[:, k, cc * 128:(cc + 1) * 128], cact[:, k:k + 1], k == 0, k == 7, [bst, bcact], [bpcol])
                TT("dve", colm[:, 0:16], pcol[:, 0:16], bcolt[:, 0:16], ALU.add, [bpcol, bbcol], [bcolm])
                TT("dve", colm[:, 24:40], pcol[:, 24:40], bcolt[:, 24:40], ALU.add, [bpcol, bbcol], [bcolm])
                CP("dve", modc[:, 8:16], colm[:, 0:8], [bcolm], [bmodc])
                STT(modc[:, 0:8], colm[:, 8:16], 1.0, nmc[:, 0:8], ALU.add, ALU.mult, [bcolm, bnmc], [bmodc])
                CP("dve", modc[:, 24:32], colm[:, 24:32], [bcolm], [bmodc])
                STT(modc[:, 16:24], colm[:, 32:40], 1.0, nmc[:, 8:16], ALU.add, ALU.mult, [bcolm, bnmc], [bmodc])
                P.emit()
            P.barrier()

            S2, bS2 = sbW("S2", [128, 512]); Sb2, bSb = sbW("Sb2", [128, 512], BF16)
            pre, bpre = sbW("pre", [128, 12, 131], BF16)
            kTs = [sbW("kTs%d" % i, [128, 128], BF16) for i in range(3)]
            Vaug = [sbW("Vaug%d" % i, [128, 130], BF16) for i in range(3)]
            posi, bposi = sbW("posi", [128, 33], I32); posf, bposf = sbW("posf", [128, 33])

            def mixer_phase(tile_list, nset, own_phase, first):
              esM = ExitStack()
              with esM:
                sbM0, psM0 = mk(esM)
                tag = "o" if own_phase else "p"

                def sbM(name, shape, dt=F32):
                    return sbM0(name + tag, shape, dt)

                def psM(name, shape, dt=F32):
                    return psM0(name + tag, shape, dt)
                xt = [sbM("xt%d" % i, [128, 1024]) for i in range(3)]
                T4a_l = [sbM("T4a%d" % i, [128, 1024]) for i in range(nset)]
                T4b_l = [sbM("T4b%d" % i, [128, 1024]) for i in range(nset)]
                T4c_l = [sbM("T4c%d" % i, [128, 1024]) for i in range(nset)]
                jb_l = [sbM("jb%d" % i, [128, 1024], BF16) for i in range(nset)]
                hb_l = [sbM("hb%d" % i, [128, 1024], BF16) for i in range(nset)]
                hT_l = [sbM("hT%d" % i, [128, 1024], BF16) for i in range(nset)]
                vT_l = [sbM("vT%d" % i, [128, 4, 128], BF16) for i in range(nset)]
                vb_l = [sbM("vb%d" % i, [128, 512], BF16) for i in range(nset)]
                kbg_l = [sbM("kbg%d" % i, [128, 512], BF16) for i in range(nset)]
                Mm_l = [sbM("Mm%d" % i, [128, 1024], BF16) for i in range(nset)]
                MT_l = [sbM("MT%d" % i, [128, 1024], BF16) for i in range(nset)]
                XT_l = [[sbM("XT%d_%d" % (s, i), [128, 1024], BF16) for i in range(2)] for s in range(nset)]
                Pm_l = [[sbM("Pm%d_%d" % (s, i), [128, 1024], BF16) for i in range(2)] for s in range(nset)]
                PT_l = [[sbM("PT%d_%d" % (s, i), [128, 1024], BF16) for i in range(2)] for s in range(nset)]
                st8_l = [sbM("st8_%d" % i, [128, 8]) for i in range(nset)]
                qkT_ = [sbM("qkT%d" % i, [128, 8, 128], BF16) for i in range(2)]
                sc_ = [sbM("sc%d" % i, [128, 160]) for i in range(2)]
                sgl_ = [sbM("sgl%d" % i, [128, 16]) for i in range(2)]
                kdec_ = [sbM("kdec%d" % i, [128, 8, 128], BF16) for i in range(2)]
                u__ = [sbM("u%d" % i, [128, 512]) for i in range(2)]
                wT_ = [sbM("wT%d" % i, [128, 8, 128], BF16) for i in range(2)]
                vnew, bvn = sbM("vnew", [128, 512], BF16)
                if own_phase:
                    TL, bTL = sbM("TL", [128, 1024])
                    hb2, bhb2 = sbM("hb2", [128, 1024], BF16)
                    st8b, bst8b = sbM("st8b", [128, 8])
                    atT_ = [sbM("atT%d" % i, [128, 1024], BF16) for i in range(2)]
                    szb_ = [sbM("szb%d" % i, [128, 512], BF16) for i in range(2)]
                    o_, bo = sbM("o", [128, 512])
                    ocat, boc = sbM("ocat", [128, 1024], BF16); ocT, bocT = sbM("ocT", [128, 1024], BF16)
                    qr, bqr = sbM("qr", [128, 640], BF16)
                    qTs_ = [sbM("qTs%d" % i, [128, 512], BF16) for i in range(2)]
                    Pex = [sbM("Pex%d" % i, [128, 512], BF16) for i in range(2)]
                    Pmk = [sbM("Pmk%d" % i, [128, 512], BF16) for i in range(2)]
                    cs, bcs = sbM("cs", [128, 256]); ki, bki = sbM("ki", [128, 64], I32)
                    h2T, bh2T = sbM("h2T", [128, 1024], BF16)
                    rt, brt = sbM("rt", [128, 128]); rt1, brt1 = sbM("rt1", [128, 64])
                    qkc_t = sbM("qkc", [128, 1024]); rinv_t = sbM("rinv", [128, 1024]); swaraw, bswr = sbM("swaraw", [128, 768])
                else:
                    atT_ = szb_ = qTs_ = [(None, None), (None, None)]
                pT = [psM("pT%d" % i, [128, 1024], BF16) for i in range(2)]
                pF = [psM("pF%d" % i, [128, 512]) for i in range(6)]
                print("SBUF remaining (mixer phase own=%s)" % own_phase, nc.sbuf_bytes_remaining)

                if first:
                    P.op("pool", lambda e: e.memset(S2[:], 0.0), [], [bS2])
                    P.op("pool", lambda e: e.memset(Sb2[:], 0.0), [], [bSb])
                    P.op("pool", lambda e: e.memset(pre[:], 0.0), [], [bpre])
                    for i in range(3):
                        va, bva = Vaug[i]
                        P.op("pool", (lambda va: (lambda e: e.memset(va[:], 1.0)))(va), [], [bva])
                    DMA(posi[:], pos[:, :], [], [bposi])
                    CP("dve", posf[:], posi[:], [bposi], [bposf])

                def load_x(i):
                    x_, bx_ = xt[i % 3]
                    DMA(x_[:], xg[i * 128:(i + 1) * 128, :], [], [bx_])

                def norm_T(x_, bx_, c0, hbt, bhbt, stt_, bstt_, dstT, bdstT, pTi):
                    P.op("pool", lambda e: e.memset(stt_[:, 0:1], 0.0), [], [bstt_])
                    ACT(hbt[:], x_[:], AF.Square, [bx_], [bhbt, bstt_], accum_out=stt_[:, 0:1])
                    rsqrt_(stt_[:, 1:2], stt_[:, 0:1], 1.0 / 1024, EPS, [bstt_])
                    TS(hbt[:], x_[:], stt_[:, 1:2], None, ALU.mult, None, [bx_, bstt_], [bhbt])
                    p_, bp_ = pT[pTi]
                    for k in range(8):
                        TR(p_[:, k * 128:(k + 1) * 128], hbt[:, k * 128:(k + 1) * 128], ident[:], [bhbt, bid], [bp_])
                    for k in range(8):
                        ACT(dstT[:, k * 128:(k + 1) * 128], p_[:, k * 128:(k + 1) * 128], AF.Identity, [bp_, bmodc], [bdstT],
                            scale=modc[:, c0 + k:c0 + k + 1], bias=modc[:, c0 + 8 + k:c0 + 9 + k])

                def transpose8(src, bsrc, dstt, bdst, pTi, evac="act"):
                    p_, bp_ = pT[pTi]
                    for k in range(8):
                        TR(p_[:, k * 128:(k + 1) * 128], src[:, k * 128:(k + 1) * 128], ident[:], [bsrc, bid], [bp_])
                    if evac == "act":
                        ACT(dstt[:], p_[:], AF.Copy, [bp_], [bdst])
                    else:
                        CP("dve", dstt[:], p_[:], [bp_], [bdst])

                def stage1(i):
                    own = i >= OWN0
                    par = i % 2
                    ss_ = i % nset
                    T4a, bTa = T4a_l[ss_]; T4b, bTb = T4b_l[ss_]; T4c, bTc = T4c_l[ss_]
                    T4a_, bTa_ = T4a, bTa
                    jb, bjb = jb_l[ss_]; hb, bhb = hb_l[ss_]; hT, bhT = hT_l[ss_]; vT, bvT = vT_l[ss_]
                    vb, bvb = vb_l[ss_]; kbg, bkbg = kbg_l[ss_]; Mm, bMm = Mm_l[ss_]; MTt, bMT = MT_l[ss_]
                    XT = XT_l[ss_]; Pm_ = Pm_l[ss_]; PT_ = PT_l[ss_]; st8, bst8 = st8_l[ss_]
                    qkc, bqkc = qkc_t if own_phase else (T4c, bTc)
                    rinv, brinv = rinv_t if own_phase else (T4b, bTb)
                    BM = [0, 1, 2, 3] if own_phase else [2 * ss_, 2 * ss_ + 1, 2 * ss_, 2 * ss_ + 1]
                    pTs = 0 if own_phase else ss_

                    def LB(k):
                        return pF[BM[k]]
                    x_, bx_ = xt[i % 3]
                    qkT, bqkT = qkT_[par]; sc, bsc = sc_[par]; sgl, bsgl = sgl_[par]; kdec, bkdec = kdec_[par]
                    atT, batT = atT_[par]; u_, bu = u__[par]; wT, bwT = wT_[par]; szb, bszb = szb_[par]; qTs, bqTs = qTs_[par]
                    load_x(i)
                    norm_T(x_, bx_, 0, hb, bhb, st8, bst8, hT, bhT, pTs)
                    yield
                    jlist = list(range(12)) if i >= OWN0 - 1 else list(range(4, 12))
                    for j in jlist:
                        p_, bp_ = LB(j // 4)
                        for k in range(8):
                            MM(p_[:, (j % 4) * 128:(j % 4) * 128 + 128], Winb[:, k, j * 128:(j + 1) * 128],
                               hT[:, k * 128:(k + 1) * 128], k == 0, k == 7, [bWin, bhT], [bp_])
                        yield
                    for g in ([0, 1, 2] if i >= OWN0 - 1 else [1, 2]):
                        p_, bp_ = LB(g)
                        ACT(pre[:, g * 4:(g + 1) * 4, 3:131], p_[:].rearrange("p (a b) -> p a b", a=4), AF.Copy, [bp_], [bpre])
                    if i == OWN0:
                        TS(pre[:, :, 0:3], pre[:, :, 0:3], flg[:, 0:1], None, ALU.mult, None, [bpre, bflg], [bpre])
                    pab, bpab = LB(3)
                    for k in range(8):
                        MM(pab[:, 0:16], hT[:, k * 128:(k + 1) * 128], Winb[:, k, 2048:2064], k == 0, k == 7, [bhT, bWin], [bpab])
                    CP("dve", sc[:, 112:128], pab[:, 0:16], [bpab], [bsc])
                    yield
                    if i >= OWN0 - 1:
                        pkv, bpkv = LB(3)
                        for k in range(8):
                            MM(pkv[:, 0:256], hT[:, k * 128:(k + 1) * 128], Winb[:, k, 2576:2832], k == 0, k == 7, [bhT, bWin], [bpkv])
                        ACT(swaraw[:, 512:640], pkv[:, 0:128], AF.Copy, [bpkv], [bswr])
                        slot = i % 3
                        va, bva = Vaug[slot]
                        P.op("pool", (lambda va: (lambda e: e.memset(va[:].rearrange("p (h d) -> p h d", h=2)[:, :, 64:65], 1.0)))(va), [], [bva])
                        ACT(va[:].rearrange("p (h d) -> p h d", h=2)[:, :, 0:64], pkv[:, 128:256].rearrange("p (h d) -> p h d", h=2), AF.Copy, [bpkv], [bva])
                        if i == OWN0 - 1:
                            TS(va[:], va[:], flg[:, 0:1], None, ALU.mult, None, [bva, bflg], [bva])
                        if own:
                            pq, bpq = LB(0)
                            for k in range(8):
                                MM(pq[:], hT[:, k * 128:(k + 1) * 128], Winb[:, k, 2064:2576], k == 0, k == 7, [bhT, bWin], [bpq])
                            ACT(swaraw[:, 0:512], pq[:], AF.Copy, [bpq], [bswr])
                            pz, bpz = LB(1)
                            for k in range(8):
                                MM(pz[:], hT[:, k * 128:(k + 1) * 128], Winb[:, k, 1536:2048], k == 0, k == 7, [bhT, bWin], [bpz])
                            ACT(szb[:], pz[:], AF.Silu, [bpz], [bszb])
                        yield
                    def conv_group(g, bank):
                        p_, bp_ = LB(bank)
                        for jj in range(4):
                            j = g * 4 + jj
                            for tap in range(4):
                                MM(p_[:, jj * 128:(jj + 1) * 128], dg[:, j * 4 + tap, :], pre[:, j, tap:tap + 128],
                                   tap == 0, tap == 3, [bdg, bpre], [bp_])
                        return p_, bp_
                    pk_, bpk_ = conv_group(1, 0)
                    ACT(qkc[:, 512:1024], pk_[:], AF.Silu, [bpk_], [bqkc])
                    yield
                    pv_, bpv_ = conv_group(2, 1)
                    ACT(vT[:].rearrange("p a b -> p (a b)"), pv_[:], AF.Silu, [bpv_], [bvT])
                    yield
                    if own:
                        pq_, bpq_ = conv_group(0, 2)
                        ACT(qkc[:, 0:512], pq_[:], AF.Silu, [bpq_], [bqkc])
                    CP("pool", pre[:, :, 0:3], pre[:, :, 128:131], [bpre], [bpre])
                    yield
                    lo = 0 if own else 512
                    ACT(jb[:, lo:1024], qkc[:, lo:1024], AF.Square, [bqkc], [bjb])
                    for c in range(lo // 128, 8):
                        p_, bp_ = LB((3, 0)[c // 4])
                        MM(p_[:, (c % 4) * 128:(c % 4) * 128 + 128], blk[:], jb[:, c * 128:(c + 1) * 128], True, True, [bblk, bjb], [bp_])
                    for g in ([0, 1] if own else [1]):
                        p_, bp_ = LB((3, 0)[g])
                        ACT(rinv[:, g * 512:(g + 1) * 512], p_[:], AF.Ln, [bp_, beps], [brinv], bias=epst[:, 0:1])
                    ACT(rinv[:, lo:1024], rinv[:, lo:1024], AF.Exp, [brinv], [brinv], scale=-0.5)
                    if own:
                        STT(qkT[:, 0:4, :].rearrange("p a b -> p (a b)"), qkc[:, 0:512], 0.125, rinv[:, 0:512], ALU.mult, ALU.mult, [bqkc, brinv], [bqkT])
                    TT("pool", qkT[:, 4:8, :].rearrange("p a b -> p (a b)"), qkc[:, 512:1024], rinv[:, 512:1024], ALU.mult, [bqkc, brinv], [bqkT])
                    yield
                    TT("dve", sc[:, 0:8], sc[:, 112:120], cst[:, 0:8], ALU.add, [bsc] + cb, [bsc])
                    ACT(sc[:, 8:16], sc[:, 0:8], AF.Exp, [bsc], [bsc])
                    ACT(sc[:, 16:24], sc[:, 8:16], AF.Ln, [bsc], [bsc], bias=1.0)
                    TT("dve", sc[:, 24:32], sc[:, 16:24], cst[:, 8:16], ALU.mult, [bsc] + cb, [bsc])
                    ACT(sc[:, 32:40], sc[:, 120:128], AF.Exp, [bsc], [bsc], scale=-1.0)
                    ACT(sc[:, 56:64], sc[:, 32:40], AF.Ln, [bsc], [bsc], bias=1.0)
                    TS(sc[:, 56:64], sc[:, 56:64], -1.0, None, ALU.mult, None, [bsc], [bsc])
                    ACT(sc[:, 48:56], sc[:, 56:64], AF.Exp, [bsc], [bsc])
                    yield
                    pg, bpg = LB(2)
                    MM(pg[:, 0:8], U32[:], sc[:, 24:32], True, True, [bU, bsc], [bpg])
                    MM(pg[:, 8:16], B32[:], sc[:, 24:32], True, True, [bB, bsc], [bpg])
                    MM(pg[:, 16:24], Cind[:, 0:128], sc[:, 24:32], True, True, [bCi, bsc], [bpg])
                    MM(pg[:, 24:32], Cind[:, 128:256], sc[:, 24:32], True, True, [bCi, bsc], [bpg])
                    CP("dve", sc[:, 128:160], pg[:, 0:32], [bpg], [bsc])
                    CP("dve", sc[:, 64:72], sc[:, 128:136], [bsc], [bsc])
                    TT("dve", sc[:, 72:80], sc[:, 64:72], sc[:, 56:64], ALU.add, [bsc], [bsc])
                    TT("dve", sc[:, 80:88], sc[:, 136:144], sc[:, 64:72], ALU.subtract, [bsc], [bsc])
                    ACT(sc[:, 88:96], sc[:, 64:72], AF.Exp, [bsc], [bsc])
                    ACT(sc[:, 96:104], sc[:, 80:88], AF.Exp, [bsc], [bsc])
                    ACT(sgl[:, 0:16], sc[:, 144:160], AF.Exp, [bsc], [bsgl])
                    TT("dve", sc[:, 104:112], sc[:, 48:56], sc[:, 88:96], ALU.mult, [bsc], [bsc])
                    yield
                    ptk, bptk = pT[pTs]
                    for m in range(4):
                        TR(ptk[:, m * 128:(m + 1) * 128], qkT[:, 4 + m, :], ident[:], [bqkT, bid], [bptk])
                        TR(ptk[:, 512 + m * 128:512 + (m + 1) * 128], vT[:, m, :], ident[:], [bvT, bid], [bptk])
                    k_tm = ptk[:, 0:512].rearrange("p (h d) -> p h d", h=8)
                    v_tm = ptk[:, 512:1024].rearrange("p (h d) -> p h d", h=8)

                    def bc8(col):
                        return sc[:, col:col + 8].unsqueeze(2).to_broadcast([128, 8, 64])
                    TT("dve", vb[:].rearrange("p (h d) -> p h d", h=8), v_tm, bc8(48), ALU.mult, [bptk, bsc], [bvb])
                    TT("dve", kbg[:].rearrange("p (h d) -> p h d", h=8), k_tm, bc8(104), ALU.mult, [bptk, bsc], [bkbg])
                    TT("dve", kdec[:, :, 0:64], k_tm, bc8(96), ALU.mult, [bptk, bsc], [bkdec])
                    CP("pool", kdec[:, :, 64:128], kdec[:, :, 0:64], [bkdec], [bkdec])
                    TT("pool", T4a[:].rearrange("p (h c) -> p h c", h=8), U32[:].unsqueeze(1).to_broadcast([128, 8, 128]),
                       sc[:, 24:32].unsqueeze(2).to_broadcast([128, 8, 128]), ALU.mult, [bU, bsc], [bTa])
                    yield
                    for hg in range(2):
                        pgr, bpgr = LB(hg)
                        MM(pgr[:], B32[:], T4a[:, hg * 512:(hg + 1) * 512], True, True, [bB, bTa], [bpgr])
                        gsl = slice(hg * 512, (hg + 1) * 512)
                        v3 = lambda t_: t_[:, gsl].rearrange("p (h c) -> p h c", h=4)
                        if own:
                            TT("dve", v3(T4b), pgr[:].rearrange("p (h c) -> p h c", h=4),
                               sc[:, 64 + hg * 4:68 + hg * 4].unsqueeze(2).to_broadcast([128, 4, 128]), ALU.subtract, [bpgr, bsc], [bTb])
                            TT("dve", v3(T4b), v3(T4b), mincl[:].unsqueeze(1).to_broadcast([128, 4, 128]), ALU.min, [bTb, bmi], [bTb])
                            ACT(T4b[:, gsl], T4b[:, gsl], AF.Exp, [bTb], [bTb])
                        TT("dve", v3(T4c), pgr[:].rearrange("p (h c) -> p h c", h=4),
                           sc[:, 72 + hg * 4:76 + hg * 4].unsqueeze(2).to_broadcast([128, 4, 128]), ALU.subtract, [bpgr, bsc], [bTc])
                        STT(v3(T4c), v3(T4c), -1.0, mslow[:].unsqueeze(1).to_broadcast([128, 4, 128]), ALU.mult, ALU.min, [bTc, bms], [bTc])
                        ACT(T4c[:, gsl], T4c[:, gsl], AF.Exp, [bTc], [bTc])
                        yield

                    def parv(t_, par_):
                        return t_[:].rearrange("p (m two c) -> p two m c", two=2, c=128)[:, par_]
                    for par_ in range(2):
                        base = par_ * 64
                        pkk, bpkk = LB(2 + par_)
                        for m in range(4):
                            MM(pkk[:, m * 128:(m + 1) * 128], qkT[base:base + 64, 4 + m, :], qkT[base:base + 64, 4 + m, :], True, True, [bqkT], [bpkk])
                        if own:
                            pqk, bpqk = LB(par_)
                            for m in range(4):
                                MM(pqk[:, m * 128:(m + 1) * 128], qkT[base:base + 64, 4 + m, :], qkT[base:base + 64, m, :], True, True, [bqkT], [bpqk])
                    for par_ in range(2):
                        pkk, bpkk = LB(2 + par_)
                        TT("dve", parv(Mm, par_), pkk[:].rearrange("p (m c) -> p m c", m=4), parv(T4c, par_), ALU.mult, [bpkk, bTc], [bMm])
                        if own:
                            pqk, bpqk = LB(par_)
                            TT("dve", parv(atT, par_), pqk[:].rearrange("p (m c) -> p m c", m=4), parv(T4b, par_), ALU.mult, [bpqk, bTb], [batT])
                    yield
                    pmt, bpmt = pT[pTs]
                    for h in range(8):
                        TR(pmt[:, h * 128:(h + 1) * 128], Mm[:, h * 128:(h + 1) * 128], ident[:], [bMm, bid], [bpmt])
                    ACT(MTt[:], pmt[:], AF.Copy, [bpmt], [bMT])
                    X0, bX0 = XT[0]
                    TT("pool", X0[:].rearrange("p (h c) -> p h c", h=8), ident[:].unsqueeze(1).to_broadcast([128, 8, 128]),
                       MTt[:].rearrange("p (h c) -> p h c", h=8), ALU.subtract, [bid, bMT], [bX0])
                    yield
                    curP, bcurP = Mm, bMm
                    curPT, bcurPT = MTt, bMT
                    curX, bcurX = XT[0]
                    for j in range(1, 6):
                        nP, bnP = Pm_[j % 2]
                        nPT, bnPT = PT_[j % 2]
                        nX, bnX = XT[j % 2]
                        for hg in range(2):
                            gsl = slice(hg * 512, (hg + 1) * 512)
                            pp, bpp = LB(hg)
                            for hh in range(4):
                                hs = slice((hg * 4 + hh) * 128, (hg * 4 + hh + 1) * 128)
                                MM(pp[:, hh * 128:(hh + 1) * 128], curPT[:, hs], curP[:, hs], True, True, [bcurPT, bcurP], [bpp])
                            ACT(nP[:, gsl], pp[:], AF.Copy, [bpp], [bnP])
                            yield
                            if j < 5:
                                pq2, bpq2 = LB(2 + hg)
                                for hh in range(4):
                                    hs = slice((hg * 4 + hh) * 128, (hg * 4 + hh + 1) * 128)
                                    MM(pq2[:, hh * 128:(hh + 1) * 128], curP[:, hs], curPT[:, hs], True, True, [bcurPT, bcurP], [bpq2])
                                CP("dve", nPT[:, gsl], pq2[:], [bpq2], [bnPT])
                                yield
                            px, bpx = LB(hg)
                            for hh in range(4):
                                hs = slice((hg * 4 + hh) * 128, (hg * 4 + hh + 1) * 128)
                                MM(px[:, hh * 128:(hh + 1) * 128], nP[:, hs], curX[:, hs], True, True, [bnP, bcurX], [bpx])
                            TT("dve", nX[:, gsl], px[:], curX[:, gsl], ALU.add, [bpx, bcurX], [bnX])
                            yield
                        curP, bcurP, curPT, bcurPT, curX, bcurX = nP, bnP, nPT, bnPT, nX, bnX
                    TTm, bTTm = curX, bcurX
                    pu, bpu = LB(0)
                    for h in range(8):
                        hs = slice(h * 128, (h + 1) * 128)
                        MM(pu[:, h * 64:(h + 1) * 64], TTm[:, hs], vb[:, h * 64:(h + 1) * 64], True, True, [bTTm, bvb], [bpu])
                    ACT(u_[:], pu[:], AF.Copy, [bpu], [bu])
                    yield
                    for hg in range(2):
                        pw_, bpw_ = LB(1) if hg == 0 else LB(0)
                        for hh in range(4):
                            h = hg * 4 + hh
                            hs = slice(h * 128, (h + 1) * 128)
                            MM(pw_[0:64, hh * 128:(hh + 1) * 128], kbg[:, h * 64:(h + 1) * 64], TTm[:, hs], True, True, [bkbg, bTTm], [bpw_])
                        CP("dve", wT[0:64, hg * 4:(hg + 1) * 4, :].rearrange("p a b -> p (a b)"), pw_[0:64, :], [bpw_], [bwT])
                    yield
                    if i >= OWN0 - 1:
                        ti = i - (OWN0 - 1)
                        TS(cs[:, 64:96], invf[:], posf[:, ti:ti + 1], None, ALU.mult, None, [binv, bposf], [bcs])
                        for (col, shift) in ((0, 0.75), (32, 0.5)):
                            t_ = cs[:, 96:128]
                            TS(t_, cs[:, 64:96], 1.0 / (2 * np.pi), shift, ALU.mult, ALU.add, [bcs], [bcs])
                            CP("dve", ki[:, 0:32], t_, [bcs], [bki])
                            CP("dve", cs[:, 128:160], ki[:, 0:32], [bki], [bcs])
                            TT("dve", t_, t_, cs[:, 128:160], ALU.subtract, [bcs], [bcs])
                            P.op("dve", lambda e: e.tensor_single_scalar(cs[:, 160:192], cs[:, 96:128], 0.0, op=ALU.is_lt), [bcs], [bcs])
                            TT("dve", t_, t_, cs[:, 160:192], ALU.add, [bcs], [bcs])
                            TS(t_, t_, 2 * np.pi, -np.pi, ALU.mult, ALU.add, [bcs], [bcs])
                            TS(t_, t_, 3.1415925, -3.1415925, ALU.min, ALU.max, [bcs], [bcs])
                            ACT(cs[:, col:col + 32], t_, AF.Sin, [bcs], [bcs])
                        yield
                        nh_l = [(swaraw, bswr, 512, 2, cst[:, 144:208], 8)]
                        if own:
                            nh_l.append((swaraw, bswr, 0, 8, cst[:, 80:144], 0))
                        for (pp, bpp, c0, nh, gain, dsth) in nh_l:
                            Tq, bTq = (T4a, bTa) if nh == 2 else (T4b, bTb)
                            ACT(Tq[:, 0:nh * 64], pp[:, c0:c0 + nh * 64], AF.Copy, [bpp], [bTq])
                        yield
                        for (pp, bpp, c0, nh, gain, dsth) in nh_l:
                            W = nh * 64
                            T4a, bTa = (T4a_, bTa_) if nh == 2 else (T4b, bTb)
                            a3 = T4a[:, 0:W].rearrange("p (h d) -> p h d", h=nh)
                            ACT(T4a[:, 512:512 + W], T4a[:, 0:W], AF.Square, [bTa], [bTa])
                            RED(rt1[:, 0:nh], T4a[:, 512:512 + W].rearrange("p (h d) -> p h d", h=nh), ALU.add, [bTa], [brt1])
                            rsqrt_(rt1[:, 16:16 + nh], rt1[:, 0:nh], 1.0 / 64, EPS, [brt1])
                            TT("dve", a3, a3, rt1[:, 16:16 + nh].unsqueeze(2).to_broadcast([128, nh, 64]), ALU.mult, [bTa, brt1], [bTa])
                            TT("dve", a3, a3, gain.unsqueeze(1).to_broadcast([128, nh, 64]), ALU.mult, [bTa] + cb, [bTa])
                            cosb = cs[:, 0:32].unsqueeze(1).to_broadcast([128, nh, 32])
                            sinb = cs[:, 32:64].unsqueeze(1).to_broadcast([128, nh, 32])
                            b3 = T4a[:, 512:512 + W].rearrange("p (h d) -> p h d", h=nh)
                            x1, x2 = a3[:, :, 0:32], a3[:, :, 32:64]
                            TT("pool", b3[:, :, 0:32], x1, cosb, ALU.mult, [bTa, bcs], [bTa])
                            TT("pool", b3[:, :, 32:64], x2, sinb, ALU.mult, [bTa, bcs], [bTa])
                            if nh == 8:
                                d3 = qr[:, 0:512].rearrange("p (a g d) -> p g a d", a=4, g=2)
                                def dv(lo_, hi_, d3=d3):
                                    return d3[:, :, :, lo_:hi_]
                                def sv(t3, lo_, hi_):
                                    return t3.rearrange("p (g a) d -> p g a d", g=2)[:, :, :, lo_:hi_]
                            else:
                                d3 = qr[:, 512:640].rearrange("p (h d) -> p h d", h=2)
                                def dv(lo_, hi_, d3=d3):
                                    return d3[:, :, lo_:hi_]
                                def sv(t3, lo_, hi_):
                                    return t3[:, :, lo_:hi_]
                            TT("pool", dv(0, 32), sv(b3, 0, 32), sv(b3, 32, 64), ALU.subtract, [bTa], [bqr])
                            TT("pool", b3[:, :, 0:32], x1, sinb, ALU.mult, [bTa, bcs], [bTa])
                            TT("pool", b3[:, :, 32:64], x2, cosb, ALU.mult, [bTa, bcs], [bTa])
                            TT("pool", dv(32, 64), sv(b3, 0, 32), sv(b3, 32, 64), ALU.add, [bTa], [bqr])
                            yield
                        pts, bpts = pT[pTs]
                        kT_, bkT_ = kTs[slot]
                        TR(pts[:, 512:640], qr[:, 512:640], ident[:], [bqr, bid], [bpts])
                        if own:
                            for a in range(4):
                                TR(pts[:, a * 128:(a + 1) * 128], qr[:, a * 128:(a + 1) * 128], ident[:], [bqr, bid], [bpts])
                            ACT(qTs[:], pts[:, 0:512], AF.Copy, [bpts], [bqTs])
                        ACT(kT_[:], pts[:, 512:640], AF.Copy, [bpts], [bkT_])
                        yield

                def stage2(i):
                    own = i >= OWN0
                    oi = i - OWN0
                    par = i % 2
                    x_, bx_ = xt[i % 3]
                    qkT, bqkT = qkT_[par]; sc, bsc = sc_[par]; sgl, bsgl = sgl_[par]; kdec, bkdec = kdec_[par]
                    atT, batT = atT_[par]; u_, bu = u__[par]; wT, bwT = wT_[par]; szb, bszb = szb_[par]; qTs, bqTs = qTs_[par]
                    if i == OWN0:
                        TS(S2[:], S2[:], flg[:, 0:1], None, ALU.mult, None, [bS2, bflg], [bS2])
                        ACT(Sb2[:], S2[:], AF.Copy, [bS2], [bSb])
                    for ch in range(2):
                        rows = slice(ch * 64, ch * 64 + 64)
                        pv2, bpv2 = pF[5]
                        for h in range(8):
                            MM(pv2[:, h * 64:(h + 1) * 64], wT[0:64, h, :], Sb2[0:64, h * 64:(h + 1) * 64], True, True, [bwT, bSb], [bpv2])
                        STT(vnew[rows, :], pv2[rows, :], -1.0, u_[rows, :], ALU.mult, ALU.add, [bu, bpv2], [bvn])
                        yield
                        if own:
                            po1 = [pF[4], pF[5]]
                            po2, bpo2 = pF[4]
                            for par_ in range(2):
                                base = par_ * 64
                                pp1, bpp1 = po1[par_]
                                for m in range(4):
                                    h = 2 * m + par_
                                    MM(pp1[:, m * 64:(m + 1) * 64], qkT[base:base + 64, m, :], Sb2[base:base + 64, h * 64:(h + 1) * 64], True, True, [bqkT, bSb], [bpp1])
                            for par_ in range(2):
                                pp1, bpp1 = po1[par_]
                                TT("dve", o_[rows, :].rearrange("p (m two d) -> p two m d", two=2, d=64)[:, par_],
                                   pp1[rows, 0:256].rearrange("p (m d) -> p m d", m=4),
                                   sc[rows, 88:96].rearrange("p (m two) -> p two m", two=2)[:, par_].unsqueeze(2).to_broadcast([64, 4, 64]),
                                   ALU.mult, [bpp1, bsc], [bo])
                            yield
                            for h in range(8):
                                MM(po2[:, h * 64:(h + 1) * 64], atT[rows, h * 128:(h + 1) * 128], vnew[rows, h * 64:(h + 1) * 64], True, True, [batT, bvn], [bpo2])
                            TT("dve", o_[rows, :], po2[rows, :], o_[rows, :], ALU.add, [bo, bpo2], [bo])
                            yield
                        pc, bpc = pF[5]
                        for h in range(8):
                            MM(pc[:, h * 64:(h + 1) * 64], kdec[rows, h, :], vnew[rows, h * 64:(h + 1) * 64], True, True, [bkdec, bvn], [bpc])
                        TT("dve", S2[:].rearrange("p (h d) -> p h d", h=8), S2[:].rearrange("p (h d) -> p h d", h=8),
                           sgl[:, ch * 8:(ch + 1) * 8].unsqueeze(2).to_broadcast([128, 8, 64]), ALU.mult, [bS2, bsgl], [bS2])
                        TT("dve", S2[:], pc[:], S2[:], ALU.add, [bS2, bpc], [bS2])
                        ACT(Sb2[:], S2[:], AF.Copy, [bS2], [bSb])
                        yield
                    if not own:
                        return
                    ACT(TL[:, 0:512], o_[:], AF.Square, [bo], [bTL])
                    RED(rt[:, 32:40], TL[:, 0:512].rearrange("p (h d) -> p h d", h=8), ALU.add, [bTL], [brt])
                    rsqrt_(rt[:, 40:48], rt[:, 32:40], 1.0 / 64, EPS, [brt])
                    o3 = o_[:].rearrange("p (h d) -> p h d", h=8)
                    TT("pool", o3, o3, rt[:, 40:48].unsqueeze(2).to_broadcast([128, 8, 64]), ALU.mult, [bo, brt], [bo])
                    TT("pool", o3, o3, cst[:, 16:80].unsqueeze(1).to_broadcast([128, 8, 64]), ALU.mult, [bo] + cb, [bo])
                    TT("pool", ocat[:, 0:512], o_[:], szb[:], ALU.mult, [bo, bszb], [boc])
                    yield
                    ppv = [pF[4], pF[4]]
                    for kvh in range(2):
                        ps_ = slice(kvh * 64, kvh * 64 + 64)
                        for bi_, sl_ in enumerate(((i - 1) % 3, i % 3)):
                            kT_, bkT_ = kTs[sl_]
                            psc, bpsc = pF[4 + bi_]
                            MM(psc[:], kT_[ps_, :], qTs[ps_, :], True, True, [bkT_, bqTs], [bpsc])
                            pe_, bpe_ = Pex[bi_]
                            pk2, bpk2 = Pmk[bi_]
                            ACT(pe_[:], psc[:], AF.Exp, [bpsc] + cb, [bpe_], bias=cst[:, 216:217])
                            TT("pool", pk2[:].rearrange("p (a q) -> p a q", a=4), pe_[:].rearrange("p (a q) -> p a q", a=4),
                               m01[:, bi_ * 128:(bi_ + 1) * 128].unsqueeze(1).to_broadcast([128, 4, 128]), ALU.mult, [bpe_, bm01], [bpk2])
                        pv_, bpv_ = ppv[kvh]
                        for a in range(4):
                            for bi_, sl_ in enumerate(((i - 1) % 3, i % 3)):
                                va, bva = Vaug[sl_]
                                pk2, bpk2 = Pmk[bi_]
                                MM(pv_[:, a * 65:(a + 1) * 65], pk2[:, a * 128:(a + 1) * 128], va[:, kvh * 65:(kvh + 1) * 65], bi_ == 0, bi_ == 1, [bpk2, bva], [bpv_])
                        pv3 = pv_[:, 0:260].rearrange("p (a d) -> p a d", a=4)
                        TT("dve", rt[:, 48:52], pv3[:, :, 64], cst[:, 208 + kvh * 4:212 + kvh * 4], ALU.add, [bpv_] + cb, [brt])
                        RCP(rt[:, 52:56], rt[:, 48:52], [brt], [brt])
                        TT("dve", ocat[:, 512 + kvh * 256:768 + kvh * 256].rearrange("p (a d) -> p a d", a=4), pv3[:, :, 0:64],
                           rt[:, 52:56].unsqueeze(2).to_broadcast([128, 4, 64]), ALU.mult, [bpv_, brt], [boc])
                        yield
                    transpose8(ocat, boc, ocT, bocT, 1)
                    py2 = [pF[4], pF[5]]
                    for nh_ in range(2):
                        p_, bp_ = py2[nh_]
                        for k in range(8):
                            MM(p_[:], ocT[:, k * 128:(k + 1) * 128], Woutb[:, k, nh_ * 512:(nh_ + 1) * 512], k == 0, k == 7, [bocT, bWout], [bp_])
                        sl_ = slice(nh_ * 512, (nh_ + 1) * 512)
                        TT("dve", TL[:, sl_], p_[:], G1[:, sl_], ALU.mult, [bp_, bG1], [bTL])
                        TT("dve", x_[:, sl_], TL[:, sl_], x_[:, sl_], ALU.add, [bTL, bx_], [bx_])
                    DMA(y[oi * 128:(oi + 1) * 128, :], x_[:], [bx_], [by_d[oi]], ysem[oi % 2])
                    yield
                    norm_T(x_, bx_, 16, hb2, bhb2, st8b, bst8b, h2T, bh2T, 1)
                    DMA(h2r_d[oi * 128:(oi + 1) * 128, :], hb2[:], [bhb2], [bh2d[oi]], hsem[oi % 2])
                    pr, bpr = pF[4]
                    for k in range(8):
                        MM(pr[:, 0:36], h2T[:, k * 128:(k + 1) * 128], Wgrb[:, k, :], k == 0, k == 7, [bh2T, bWgr], [bpr])
                    R = rt
                    bR = [brt]
                    TT("dve", R[:, 64:100], pr[:, 0:36], cst[:, 220:256], ALU.add, [bpr] + cb, bR)
                    RED(R[:, 100:101], R[:, 64:68], ALU.max, bR, bR)
                    TS(R[:, 101:102], R[:, 100:101], -1.0, None, ALU.mult, None, bR, bR)
                    P.op("pool", lambda e: e.memset(rt[:, 102:103], 0.0), [], bR)
                    ACT(R[:, 104:108], R[:, 64:68], AF.Exp, bR, bR, bias=R[:, 101:102], accum_out=R[:, 102:103])
                    RCP(R[:, 103:104], R[:, 102:103], bR, bR)
                    TS(R[:, 104:108], R[:, 64:68], R[:, 100:101], None, ALU.is_equal, None, bR, bR)
                    TT("dve", TL[:, 0:32].rearrange("p (g e) -> p g e", g=4), R[:, 68:100].rearrange("p (g e) -> p g e", g=4),
                       R[:, 104:108].unsqueeze(2).to_broadcast([128, 4, 8]), ALU.mult, bR, [bTL])
                    RED(R[:, 108:116], TL[:, 0:32].rearrange("p (g e) -> p e g", g=4), ALU.add, [bTL], bR)
                    RED(R[:, 116:117], R[:, 108:116], ALU.max, bR, bR)
                    TS(TL[:, 32:40], R[:, 108:116], R[:, 116:117], None, ALU.is_equal, None, bR, [bTL])
                    STT(TL[:, 40:48], TL[:, 32:40], -1e30, R[:, 108:116], ALU.mult, ALU.add, [bTL] + bR, [bTL])
                    RED(R[:, 117:118], TL[:, 40:48], ALU.max, [bTL], bR)
                    TS(TL[:, 48:56], TL[:, 40:48], R[:, 117:118], None, ALU.is_equal, None, [bTL] + bR, [bTL])
                    TT("dve", R[:, 118:119], R[:, 117:118], R[:, 116:117], ALU.subtract, bR, bR)
                    ACT(R[:, 119:120], R[:, 118:119], AF.Exp, bR, bR)
                    TS(R[:, 120:121], R[:, 119:120], 1.0, None, ALU.add, None, bR, bR)
                    RCP(R[:, 121:122], R[:, 120:121], bR, bR)
                    TT("dve", R[:, 122:123], R[:, 121:122], R[:, 103:104], ALU.mult, bR, bR)
                    TT("dve", R[:, 123:124], R[:, 122:123], R[:, 119:120], ALU.mult, bR, bR)
                    g48 = R[:, 104:108].unsqueeze(2).to_broadcast([128, 4, 8])
                    TT("dve", OH1[:, oi * 32:(oi + 1) * 32].rearrange("p (g e) -> p g e", g=4),
                       TL[:, 32:40].unsqueeze(1).to_broadcast([128, 4, 8]), g48, ALU.mult, [bTL] + bR, [bOH1])
                    TT("dve", OH2[:, oi * 32:(oi + 1) * 32].rearrange("p (g e) -> p g e", g=4),
                       TL[:, 48:56].unsqueeze(1).to_broadcast([128, 4, 8]), g48, ALU.mult, [bTL] + bR, [bOH2])
                    CP("dve", W12[:].rearrange("p (r t) -> p r t", r=2)[:, :, oi], R[:, 122:124], bR, [bW12])
                    yield

                tiles = list(tile_list)
                s1g = {}
                nxt = 0
                active = []

                def start_s1():
                    nonlocal nxt
                    g = stage1(tiles[nxt])
                    s1g[nxt] = g
                    active.append(g)
                    nxt += 1

                def step(g):
                    try:
                        next(g)
                        return True
                    except StopIteration:
                        if g in active:
                            active.remove(g)
                        return False
                for n, i in enumerate(tiles):
                    if nxt <= n:
                        start_s1()
                    g1 = s1g[n]
                    while g1 in active:
                        step(g1)
                    g2 = stage2(i)
                    active.append(g2)
                    while nxt < len(tiles) and nxt <= n + nset:
                        start_s1()
                    while g2 in active:
                        for g in list(active):
                            step(g)
                while active:
                    for g in list(active):
                        step(g)
                P.emit()

            tl_all = list(CFG['tiles']) if CFG['tiles'] is not None else list(range(NT))
            tl_pre = [t for t in tl_all if t < OWN0 - 1]
            tl_own = [t for t in tl_all if t >= OWN0 - 1]
            mixer_phase(tl_pre, CFG.get('nset', 2), False, True)
            P.barrier()
            mixer_phase(tl_own, 1, True, False)
            P.barrier()

        esE = ExitStack()
        with esE:
            sbE, psE = mk(esE)
            identE, bidE = sbE("m_identE", [128, 128], BF16)
            UsE, bUsE = sbE("m_UsE", [128, 128], BF16)
            onesE, bonesE = sbE("m_onesE", [128, 128], BF16)
            Asb, bAsb = sbE("m_Asb", [128, 2048])
            Bx, bBx = sbE("m_Bx", [128, 65 * 32])
            srt, bsrt = sbE("m_srt", [128, 256])
            E3, bE3 = sbE("m_E3", [128, NTL * 32])
            posf, bposf = sbE("m_posf", [128, 64]); posi, bposi = sbE("m_posi", [128, 64], I32)
            META, bMETA = sbE("m_META", [128, 256], I32)
            minit, bminit = sbE("m_minit", [128, NSUB * 4], I32)
            metaS, _bms = sbE("m_metaS", [128, NSUB * 4], I32)
            thr, bthr = sbE("m_thr", [128, NTL]); pcol, bpcol = sbE("m_pcol", [128, 1])
            idxw, bidxw = sbE("m_idxw", [128, 64], I32)
            DMA(identE[:], c_ident[:, :], [], [bidE]); DMA(UsE[:], c_Us[:, :], [], [bUsE])
            P.op("pool", lambda e: e.memset(onesE[:], 1.0), [], [bonesE])
            esS = ExitStack()
            with esS:
                _, psS = mk(esS)
                pA = [psS("m_pA%d" % i, [128, 512]) for i in range(4)]
                pB = [psS("m_pB%d" % i, [128, 512]) for i in range(4)]
                for v in range(64):
                    OHt, bOHt = (OH1, bOH1) if v < 32 else (OH2, bOH2)
                    rhs = OHt[:, (v % 32) * 32:(v % 32 + 1) * 32]
                    a_, ba_ = pA[v // 16]; b_, bb_ = pB[v // 16]
                    c0 = (v % 16) * 32
                    MM(a_[:, c0:c0 + 32], UsE[:], rhs, True, True, [bUsE, bOHt], [ba_])
                    MM(b_[:, c0:c0 + 32], onesE[:], rhs, True, True, [bonesE, bOHt], [bb_])
                for i in range(4):
                    ACT(Asb[:, i * 512:(i + 1) * 512], pA[i][0][:], AF.Copy, [pA[i][1]], [bAsb])
                    CP("dve", Bx[:, 32 + i * 512:32 + (i + 1) * 512], pB[i][0][:], [pB[i][1]], [bBx])
                P.emit()
            P.barrier()
            P.op("pool", lambda e: e.memset(Bx[:, 0:32], 0.0), [], [bBx])
            for v in range(2, 65):
                TT("dve", Bx[:, v * 32:(v + 1) * 32], Bx[:, v * 32:(v + 1) * 32], Bx[:, (v - 1) * 32:v * 32], ALU.add, [bBx], [bBx])
            cnt = Bx[:, 2048:2080]
            nt_ = srt[:, 0:32]; pc_ = srt[:, 32:64]; st_ = srt[:, 64:96]; en_ = srt[:, 96:128]
            TS(nt_, cnt, 0.0, None, ALU.is_gt, None, [bBx], [bsrt])
            for jj in range(1, 8):
                STT(nt_, cnt, 512.0 * jj, nt_, ALU.is_gt, ALU.add, [bBx, bsrt], [bsrt])
            TS(pc_, nt_, 512.0, None, ALU.mult, None, [bsrt], [bsrt])
            P.op("pool", lambda e: e.memset(srt[:, 64:65], 0.0), [bsrt], [bsrt])
            for ee in range(1, 32):
                TT("dve", st_[:, ee:ee + 1], st_[:, ee - 1:ee], pc_[:, ee - 1:ee], ALU.add, [bsrt], [bsrt])
            TT("dve", en_, st_, pc_, ALU.add, [bsrt], [bsrt])
            A3 = Asb[:].rearrange("p (v e) -> p v e", e=32)
            TT("dve", Asb[:], Asb[:], Bx[:, 0:2048], ALU.add, [bAsb, bBx], [bAsb])
            TT("dve", A3, A3, st_.unsqueeze(1).to_broadcast([128, 64, 32]), ALU.add, [bAsb, bsrt], [bAsb])
            TT("dve", Asb[:, 0:1024], Asb[:, 0:1024], OH1[:], ALU.mult, [bAsb, bOH1], [bAsb])
            TT("dve", Asb[:, 1024:2048], Asb[:, 1024:2048], OH2[:], ALU.mult, [bAsb, bOH2], [bAsb])
            RED(posf[:], A3, ALU.add, [bAsb], [bposf])
            CP("dve", posi[:], posf[:], [bposf], [bposi])
            DMA(thr[:], c_thr[:, :], [], [bthr]); DMA(pcol[:], c_pcol[:, :], [], [bpcol])
            E33 = E3[:].rearrange("p (j e) -> p j e", e=32)
            TT("dve", E33, en_.unsqueeze(1).to_broadcast([128, NTL, 32]), thr[:].unsqueeze(2).to_broadcast([128, NTL, 32]), ALU.is_le,
               [bsrt, bthr], [bE3])
            bsrt2 = Buf("srt2")
            RED(srt[:, 128:128 + NTL], E33, ALU.add, [bE3], [bsrt2])
            TS(srt[:, 192:192 + NTL], srt[:, 128:128 + NTL], 128.0, pcol[:, 0:1], ALU.mult, ALU.add, [bsrt2, bpcol], [bsrt2])
            CP("dve", idxw[:, 0:NTL], srt[:, 192:192 + NTL], [bsrt2], [bidxw])
            DMA(META[:], c_meta0[:, :], [], [bMETA])
            MF = META[:].bitcast(F32).rearrange("p (v c) -> p v c", c=4)
            CP("dve", MF[:, :, 1], W12[:], [bW12, bMETA], [bMETA])
            bminit_d = Buf("minit_d")
            DMA(minit[:], c_minit[:, :], [], [bminit])
            DMA(meta_d.rearrange("(p n) c -> p (n c)", p=128), minit[:], [bminit], [bminit_d])

            bndt, bbndt = sbE("m_bndt", [128, 4], I32)
            DMA(bndt[:], c_bnd[:, :], [], [bbndt])
            bregs = Buf("bregs")
            REG = {}
            for bi_, bv_ in enumerate((4095, 8191, NSLOT - 1)):
                REG[bv_] = nc.alloc_registers("bnd%d" % bi_, engines=[mybir.EngineType.Pool])
                P.op("pool", (lambda rg, ap_: (lambda e: nc.regs_load(rg, ap_)[-1]))(REG[bv_], bndt[0:1, bi_:bi_ + 1]), [bbndt, bregs], [bregs], cost=300)

            def IGATHER(out, src_, idx_ap, bound, r, w, sembuf, nbytes):
                P.dma(lambda e: e.indirect_dma_start(out=out, out_offset=None, in_=src_,
                                                    in_offset=bass.IndirectOffsetOnAxis(ap=idx_ap, axis=0),
                                                    bounds_check=REG[bound], oob_is_err=False),
                      list(r) + [bregs], w, sembuf, eng="pool", cost=2500 + nbytes / 150.0, issue=1200.0)

            def ISCATTER(dst, idx_ap, src_, bound, r, w, sembuf, nbytes):
                def f_(e):
                    try:
                        return e.indirect_dma_start(out=dst, out_offset=bass.IndirectOffsetOnAxis(ap=idx_ap, axis=0),
                                                    in_=src_, in_offset=None, bounds_check=REG[bound], oob_is_err=False)
                    except Exception:
                        print("ISCATTER fail", dst.shape, dst.dtype, src_.shape, src_.dtype, idx_ap.shape, idx_ap.dtype, bound)
                        raise
                P.dma(f_, list(r) + [bregs], w, sembuf, eng="pool", cost=2500 + nbytes / 150.0, issue=1200.0)
            bmsc = Buf("msc")
            bmsAll = Buf("msAll"); bcoAll = Buf("coAll")
            bmsv = [Buf("msv%d" % v) for v in range(64)]
            for v in range(64):
                ISCATTER(meta_d[:, :], posi[:, v:v + 1], META[:, v * 4:(v + 1) * 4], NSLOT - 1, [bposi, bMETA, bminit_d], [bmsv[v], bmsAll], bmsc, 2048)
            bmS = [Buf("metaS%d" % q) for q in range(4)]
            bmSs = Buf("metaSs")
            for q in range(4):
                DMA(metaS[:, q * NTL * 4:(q + 1) * NTL * 4].rearrange("p (t c) -> p t c", c=4),
                    meta_d[q * NTL * 128:(q + 1) * NTL * 128, :].rearrange("(t p) c -> p t c", p=128), bmsv + [bminit_d], [bmS[q]], bmSs)
            MSI = metaS[:].rearrange("p (t c) -> p t c", c=4)
            MSF = metaS[:].bitcast(F32).rearrange("p (t c) -> p t c", c=4)

            Wg = [sbE("m_Wg%d" % i, [128, 8, 256], BF16) for i in range(3)]
            Wu = [sbE("m_Wu%d" % i, [128, 8, 256], BF16) for i in range(3)]
            Wd = [sbE("m_Wd%d" % i, [128, 2, 1024], BF16) for i in range(3)]
            Xg = [sbE("m_Xg%d" % i, [128, 4, 1024], BF16)[0] for i in range(3)]
            bXg = [[Buf("Xg%d_%d" % (i, s)) for s in range(4)] for i in range(3)]
            bXgs = [Buf("Xgs%d" % i) for i in range(3)]
            h2s = [sbE("m_h2s%d" % i, [128, 8, 512], BF16) for i in range(2)]
            sg = [sbE("m_sg%d" % i, [128, 512], BF16) for i in range(2)]
            hid = [sbE("m_hid%d" % i, [128, 512], BF16) for i in range(4)]
            yo = [sbE("m_yo%d" % i, [128, 1024]) for i in range(3)]
            xo = [sbE("m_xo%d" % i, [128, 1024]) for i in range(2)]
            c0b = [sbE("m_c0b%d" % i, [128, 1024]) for i in range(2)]
            c1b = [sbE("m_c1b%d" % i, [128, 1024]) for i in range(2)]
            pT2 = [psE("m_pT2_%d" % i, [128, 1024], BF16) for i in range(2)]
            pg_ = [psE("m_pg%d" % i, [128, 512]) for i in range(2)]
            pu_ = [psE("m_pu%d" % i, [128, 512]) for i in range(2)]
            pd, bpd = psE("m_pd", [128, 1024])
            print("SBUF remaining (moe)", nc.sbuf_bytes_remaining)
            for i in range(3):
                for s in range(4):
                    P.op("pool", (lambda i, s: (lambda e: e.memset(Xg[i][:, s, :], 0.0)))(i, s), [], [bXg[i][s]], cost=1500)
            bco = [Buf("co%d" % u) for u in range(NSUB)]

            def w_gather(j):
                sl = j % 3
                IGATHER(Wg[sl][0][:].rearrange("p k n -> p (k n)"), wg_l[:, :], idxw[:, j:j + 1], 4095, [bidxw], [Wg[sl][1]], None, 1 << 20)
                IGATHER(Wu[sl][0][:].rearrange("p k n -> p (k n)"), wu_l[:, :], idxw[:, j:j + 1], 4095, [bidxw], [Wu[sl][1]], None, 1 << 20)
                IGATHER(Wd[sl][0][:].rearrange("p k n -> p (k n)"), wd_l[:, :], idxw[:, j:j + 1], 4095, [bidxw], [Wd[sl][1]], None, 1 << 20)

            def x_gather(j):
                b3 = j % 3
                for s in range(4):
                    u = 4 * j + s
                    IGATHER(Xg[b3][:, s, :], h2r_d[:, :], MSI[:, u, 0:1], 4095, bmS, [bXg[b3][s]], bXgs[b3], 1 << 18)
            NTR = CFG.get('ntl', NTL)
            for j in range(min(2, NTR)):
                w_gather(j)
                x_gather(j)
            for j in range(NTR):
                if j + 2 < NTR:
                    w_gather(j + 2)
                    x_gather(j + 2)
                sl = j % 3
                wg_, bwg_ = Wg[sl]; wu_, bwu_ = Wu[sl]; wd_, bwd_ = Wd[sl]
                xg_ = Xg[sl]
                h2s_, bh2s_ = h2s[j % 2]
                for kp in range(4):
                    pt, bpt = pT2[kp % 2]
                    for kk in range(2):
                        k = kp * 2 + kk
                        for s in range(4):
                            TR(pt[:, kk * 512 + s * 128:kk * 512 + (s + 1) * 128], xg_[:, s, k * 128:(k + 1) * 128], identE[:],
                               bXg[sl] + [bidE], [bpt])
                    for kk in range(2):
                        k = kp * 2 + kk
                        ACT(h2s_[:, k, :], pt[:, kk * 512:(kk + 1) * 512], AF.Identity, [bpt, bmodc], [bh2s_],
                            scale=modc[:, 16 + k:17 + k], bias=modc[:, 24 + k:25 + k])
                for fc in range(2):
                    pgx, bpgx = pg_[fc]; pux, bpux = pu_[fc]
                    for k in range(8):
                        MM(pgx[:], wg_[:, k, fc * 128:(fc + 1) * 128], h2s_[:, k, :], k == 0, k == 7, [bwg_, bh2s_], [bpgx])
                    for k in range(8):
                        MM(pux[:], wu_[:, k, fc * 128:(fc + 1) * 128], h2s_[:, k, :], k == 0, k == 7, [bwu_, bh2s_], [bpux])
                    s_, bs_ = sg[fc]
                    hd, bhd = hid[(j % 2) * 2 + fc]
                    ACT(s_[:], pgx[:], AF.Silu, [bpgx], [bs_])
                    TT("dve", hd[:], pux[:], s_[:], ALU.mult, [bs_, bpux], [bhd])
                for t in range(4):
                    u = 4 * j + t
                    for nh_ in range(2):
                        for fc in range(2):
                            hd, bhd = hid[(j % 2) * 2 + fc]
                            MM(pd[:, nh_ * 512:(nh_ + 1) * 512], hd[:, t * 128:(t + 1) * 128], wd_[:, fc, nh_ * 512:(nh_ + 1) * 512], fc == 0, fc == 1,
                               [bhd, bwd_], [bpd])
                    yo_, byo_ = yo[u % 3]
                    STT(yo_[:], pd[:], MSF[:, u, 1:2], G2[:], ALU.mult, ALU.mult, [bpd, bG2] + bmS, [byo_])
                    ISCATTER(contrib_d[:, :], MSI[:, u, 2:3], yo_[:], 8191, [byo_] + bmS, [bco[u], bcoAll], byo_, 1 << 19)
            for oi in range(32):
                xo_, bxo_ = xo[oi % 2]; c0_, bc0_ = c0b[oi % 2]; c1_, bc1_ = c1b[oi % 2]
                DMA(xo_[:], y[oi * 128:(oi + 1) * 128, :], [by_d[oi]], [bxo_])
                DMA(c0_[:], contrib_d[oi * 128:(oi + 1) * 128, :], bco, [bc0_], eng="act")
                DMA(c1_[:], contrib_d[4096 + oi * 128:4096 + (oi + 1) * 128, :], bco, [bc1_], eng="act")
                TT("dve", c0_[:], c0_[:], c1_[:], ALU.add, [bc0_, bc1_], [bc0_])
                TT("dve", xo_[:], xo_[:], c0_[:], ALU.add, [bxo_, bc0_], [bxo_])
                DMA(y[oi * 128:(oi + 1) * 128, :], xo_[:], [bxo_], [by_d[oi]], ysem2[oi % 2], eng="pool")
            P.wait_all("sp", by_d)
            P.emit()
    except Cut:
        pass
    return nc


def _consts():
    idx = np.arange(128)
    same = (idx[:, None] // 64) == (idx[None, :] // 64)
    U = (same & (idx[:, None] <= idx[None, :])).astype(np.float32)
    B = same.astype(np.float32)
    ind = np.zeros((128, 2, 128), np.float32)
    ind[:64, 0, :] = 1.0
    ind[64:, 1, :] = 1.0
    mincl = np.where(same & (idx[None, :] >= idx[:, None]), 0.0, -30000.0).astype(np.float32)
    mslow = np.where(same & (idx[None, :] < idx[:, None]), 0.0, -30000.0).astype(np.float32)
    m01 = np.zeros((128, 2, 128), np.float32)
    m01[:, 0, :] = (idx[:, None] > idx[None, :])
    m01[:, 1, :] = (idx[:, None] <= idx[None, :])
    half = 32
    invf = (10000.0 ** (-np.arange(half, dtype=np.float32) / half)).astype(np.float32)
    Us = (idx[:, None] < idx[None, :]).astype(np.float32).astype(NPBF)
    meta0 = np.zeros((128, 64, 4), np.int32)
    vv = np.arange(64)
    meta0[:, :, 0] = (vv[None, :] % 32) * 128 + idx[:, None]
    meta0[:, :, 2] = (vv[None, :] // 32) * 4096 + (vv[None, :] % 32) * 128 + idx[:, None]
    extra = dict(c_Us=Us, c_meta0=meta0.reshape(128, 256), c_minit=np.full((128, NSUB * 4), 1 << 30, np.int32),
                 c_thr=np.ascontiguousarray(np.broadcast_to((512.0 * np.arange(NTL, dtype=np.float32))[None, :], (128, NTL))),
                 c_pcol=idx.astype(np.float32).reshape(128, 1),
                 c_bnd=np.ascontiguousarray(np.broadcast_to(np.array([4095, 8191, NSLOT - 1, 0], np.int32)[None, :], (128, 4))))
    return dict(c_ident=np.eye(128, dtype=np.float32).astype(NPBF), c_U=U, c_B=B, c_ind=ind.reshape(128, 256), **extra,
                c_mincl=mincl, c_mslow=mslow, c_blk=B.astype(NPBF), c_m01=m01.reshape(128, 256).astype(NPBF),
                c_invf=np.ascontiguousarray(np.broadcast_to(invf[None, :], (128, 32))))


_NC_CACHE = {}


def kernel(x, c, positions, w_ada, b_ada, norm_mix, w_in, conv_w, a_log, dt_bias, gdn_out_norm, q_norm, k_norm,
           sinks, w_out, norm_ffn, w_group, b_group, w_router, b_router, w_gate, w_up, w_down):
    f = lambda a: np.ascontiguousarray(np.asarray(a, dtype=np.float32))
    x = f(x); c = f(c); positions = np.asarray(positions).astype(np.int32)
    if "nc" not in _NC_CACHE:
        _NC_CACHE["nc"] = build(DBG)
    nc = _NC_CACHE["nc"]
    consts = _consts()
    shared = dict(
        w_ada=f(w_ada)[0], b_ada=f(b_ada), w_in=f(w_in)[0],
        b_ada_col=np.ascontiguousarray(f(b_ada).reshape(48, 128).T), nm_col=np.ascontiguousarray(f(norm_mix).reshape(8, 128).T),
        nf_col=np.ascontiguousarray(f(norm_ffn).reshape(8, 128).T),
        conv_wT=np.ascontiguousarray(f(conv_w)[0].T.reshape(12, 128, 4).transpose(1, 0, 2).reshape(128, 48)),
        a_log=f(a_log), dt_bias=f(dt_bias), gon=f(gdn_out_norm), qnw=f(q_norm), knw=f(k_norm), sinks=f(sinks),
        w_out=f(w_out)[0],
        w_gr=np.ascontiguousarray(np.concatenate([f(w_group)[0], f(w_router)[0]], axis=1)),
        b_gr=np.ascontiguousarray(np.concatenate([f(b_group), f(b_router)], axis=1)),
        wg_l=np.ascontiguousarray(f(w_gate)[0].reshape(32, 8, 128, 256).transpose(0, 2, 1, 3).reshape(4096, 2048)),
        wu_l=np.ascontiguousarray(f(w_up)[0].reshape(32, 8, 128, 256).transpose(0, 2, 1, 3).reshape(4096, 2048)),
        wd_l=np.ascontiguousarray(f(w_down)[0].reshape(32, 2, 128, 1024).transpose(0, 2, 1, 3).reshape(4096, 2048)),
        **consts)
    in_maps = []
    for core in range(8):
        b, half = core // 2, core % 2
        own = x[b, half * 4096:(half + 1) * 4096]
        xg = np.ascontiguousarray(np.concatenate([x[b, 0:4096], own], axis=0))
        if half == 1:
            pp = positions[b, 4096 - 128:8192]
        else:
            pp = np.concatenate([positions[b, 0:128], positions[b, 0:4096]])
        pos = np.ascontiguousarray(pp.reshape(33, 128).T)
        m = dict(shared)
        m.update(xg=xg, c_col=np.ascontiguousarray(c[b].reshape(8, 128).T), pos=pos,
                 flag=np.full((128, 1), float(half), np.float32))
        in_maps.append(m)
    res = run_bass_kernel_spmd(nc, in_maps, core_ids=list(range(8)))
    out = np.zeros((4, 8192, 1024), np.float32)
    for core in range(8):
        b, half = core // 2, core % 2
        out[b, half * 4096:(half + 1) * 4096] = res.results[core]["y"]
    if DBG:
        kernel.dbg = [res.results[core].get("dbg_o") for core in range(8)]
    return out
```

```python
import numpy as np
import concourse.bass as bass
import concourse.mybir as mybir
from concourse.bass_utils import run_bass_kernel_spmd
from contextlib import ExitStack
import ml_dtypes

F32 = mybir.dt.float32
BF16 = mybir.dt.bfloat16
I32 = mybir.dt.int32
ALU = mybir.AluOpType
AF = mybir.ActivationFunctionType
AX = mybir.AxisListType
NPBF = ml_dtypes.bfloat16


class Buf:
    __slots__ = ("name", "w", "readers", "dsem")

    def __init__(self, name):
        self.name = name
        self.w = None
        self.readers = []
        self.dsem = None


class Prog:
    ENG = ("pe", "act", "dve", "pool", "sp")
    LAT = 150.0

    def __init__(self, nc, es):
        self.nc = nc
        self.es = es
        self.recs = []
        self.cnt = {e: 0 for e in self.ENG}
        self.sems = {}
        self.semcnt = {}
        self.waited = {e: {} for e in self.ENG}
        for e in self.ENG[:4]:
            self._sem("E_" + e)
        self.nd = 0
        self.phase = 0
        self.pending_barrier = False
        self.sched = True
        self.final_wait_bufs = None

    def _sem(self, key):
        if key not in self.sems:
            self.sems[key] = self.es.enter_context(self.nc.semaphore("s_" + key))
            self.semcnt[key] = 0
        return key

    def op(self, eng, fn, reads=(), writes=(), cost=200.0):
        self.recs.append(dict(eng=eng, fn=fn, reads=list(reads), writes=list(writes), cost=float(cost), dma=False))

    def dma(self, fn, reads=(), writes=(), sembuf=None, eng="sp", cost=3000.0, issue=60.0):
        sb = sembuf if sembuf is not None else (writes[0] if writes else reads[0])
        if sb.dsem is None:
            self.nd += 1
            sb.dsem = self._sem("D%d_%s" % (self.nd, sb.name))
        key = sb.dsem
        self.semcnt[key] += 16
        self.recs.append(dict(eng=eng, fn=fn, reads=list(reads), writes=list(writes), cost=float(cost), dma=True,
                              tok=(key, self.semcnt[key]), issue=float(issue)))

    def wait_all(self, eng, bufs):
        self.final_wait_bufs = (eng, list(bufs))

    def barrier(self):
        self.pending_barrier = True

    def emit(self):
        import heapq
        recs = self.recs
        n = len(recs)
        ENG = self.ENG
        preds = [[] for _ in range(n)]
        ph = self.phase
        for i, r in enumerate(recs):
            for b in r["reads"]:
                if b.w is not None and b.w[0] == ph:
                    preds[i].append((b.w[1], "raw"))
            for b in r["writes"]:
                if b.w is not None and b.w[0] == ph:
                    preds[i].append((b.w[1], "waw"))
                for (p2, rid) in b.readers:
                    if p2 == ph and rid != i:
                        preds[i].append((rid, "war"))
            for b in r["reads"]:
                b.readers.append((ph, i))
            for b in r["writes"]:
                b.w = (ph, i)
                b.readers = []
        last_dma = {}
        for i, r in enumerate(recs):
            if r["dma"]:
                if r["eng"] in last_dma:
                    preds[i].append((last_dma[r["eng"]], "issue"))
                last_dma[r["eng"]] = i
        final = None
        if self.final_wait_bufs is not None:
            feng, fb = self.final_wait_bufs
            fp = []
            for b in fb:
                if b.w is not None and b.w[0] == ph:
                    fp.append(b.w[1])
                for (p2, rid) in b.readers:
                    if p2 == ph:
                        fp.append(rid)
            final = (feng, fp)
            self.final_wait_bufs = None
        order = {e: [] for e in ENG}
        if self.sched:
            succ = [[] for _ in range(n)]
            npred = [0] * n
            for i in range(n):
                ps = {}
                for (p, kd) in preds[i]:
                    ps[p] = ps.get(p, True) and kd == "issue"
                npred[i] = len(ps)
                for p, io in ps.items():
                    succ[p].append((i, io))
            finish = [0.0] * n
            startt = [0.0] * n
            ready = [0.0] * n
            heaps = {e: [] for e in ENG}
            free = {e: 0.0 for e in ENG}
            for i in range(n):
                if npred[i] == 0:
                    heapq.heappush(heaps[recs[i]["eng"]], (0.0, i))
            done = 0
            while done < n:
                best = None
                for e in ENG:
                    h = heaps[e]
                    if not h:
                        continue
                    t0 = max(free[e], h[0][0])
                    if best is None or t0 < best[0]:
                        best = (t0, e)
                t0, e = best
                h = heaps[e]
                cands = []
                while h and h[0][0] <= t0:
                    cands.append(heapq.heappop(h))
                cands.sort(key=lambda x: x[1])
                rt_, i = cands[0]
                for c in cands[1:]:
                    heapq.heappush(h, c)
                r = recs[i]
                startt[i] = t0
                if r["dma"]:
                    free[e] = t0 + r["issue"]
                    finish[i] = t0 + r["cost"]
                else:
                    free[e] = t0 + r["cost"]
                    finish[i] = free[e]
                order[e].append(i)
                done += 1
                for (s_, io) in succ[i]:
                    npred[s_] -= 1
                    lat = self.LAT if recs[s_]["eng"] != e or r["dma"] else 60.0
                    if io:
                        ready[s_] = max(ready[s_], t0 + r["issue"])
                    else:
                        ready[s_] = max(ready[s_], finish[i] + lat)
                    if npred[s_] == 0:
                        heapq.heappush(heaps[recs[s_]["eng"]], (ready[s_], s_))
            self.model_time = max(finish) if n else 0.0
            busy = {e: sum(recs[i]['cost'] if not recs[i]['dma'] else recs[i]['issue'] for i in order[e]) for e in ENG}
            print('[sched] phase', self.phase, 'ops', n, 'model_us', round(self.model_time / 1000, 1), 'busy_us', {e: round(v / 1000, 1) for e, v in busy.items()})
        else:
            for i, r in enumerate(recs):
                order[r["eng"]].append(i)
        tok = [None] * n
        for e in ENG:
            c = self.cnt[e]
            for i in order[e]:
                r = recs[i]
                if r["dma"]:
                    tok[i] = r["tok"]
                else:
                    c += 1
                    tok[i] = ("E_" + e, c)
            self.cnt[e] = c
        ins = {e: [] for e in ENG}
        for e in ENG:
            waited = self.waited[e]
            first = True
            for i in order[e]:
                r = recs[i]
                waits = {}
                if first and self.pending_barrier:
                    for key, val in self._barrier_vals.items():
                        if val > 0 and waited.get(key, 0) < val:
                            waited[key] = val
                            waits[key] = val
                first = False
                for (p, kind) in preds[i]:
                    pr = recs[p]
                    if kind == "issue":
                        continue
                    same = (not pr["dma"]) and (not r["dma"]) and pr["eng"] == e
                    if same and (e == "pe" or (kind != "raw" and e != "pool")):
                        continue
                    key, val = tok[p]
                    if waited.get(key, 0) >= val:
                        continue
                    waited[key] = val
                    waits[key] = max(waits.get(key, 0), val)
                if r["dma"]:
                    ins[e].append((list(waits.items()), r["fn"], r["tok"][0], 16))
                else:
                    ins[e].append((list(waits.items()), r["fn"], "E_" + e, 1))
            if first and self.pending_barrier:
                waits = {}
                for key, val in self._barrier_vals.items():
                    if val > 0 and waited.get(key, 0) < val:
                        waited[key] = val
                        waits[key] = val
                if waits:
                    ins[e].append((list(waits.items()), None, None, 0))
        if final is not None:
            feng, fp = final
            waits = {}
            waited = self.waited[feng]
            for p in fp:
                key, val = tok[p]
                if waited.get(key, 0) < val:
                    waited[key] = val
                    waits[key] = max(waits.get(key, 0), val)
            if waits:
                ins[feng].append((list(waits.items()), None, None, 0))
        self.pending_barrier = False
        self._barrier_vals = {}
        for e in ENG[:4]:
            self._barrier_vals["E_" + e] = self.cnt[e]
        for key in self.sems:
            if not key.startswith("E_"):
                self._barrier_vals[key] = self.semcnt[key]
        nc = self.nc
        sems = self.sems
        with nc.Block() as block:
            def run(engname):
                def body(e):
                    for (waits, fn, key, inc) in ins[engname]:
                        for (k, v) in waits:
                            e.wait_ge(sems[k], v)
                        if fn is not None:
                            fn(e).then_inc(sems[key], inc)
                return body
            block.tensor(run("pe"))
            block.scalar(run("act"))
            block.vector(run("dve"))
            block.gpsimd(run("pool"))
            block.sync(run("sp"))
        self.recs = []
        self.phase += 1


NT = 64
OWN0 = 32
NTL = 47
NSUB = NTL * 4
NSLOT = NSUB * 128
EPS = 1e-6
DBG = False
CFG = dict(tiles=None, moe=True, nex=32)


class Cut(Exception):
    pass


def build(dbg=False):
    nc = bass.Bass("TRN2", target_bir_lowering=False)

    def din(name, shape, dt=F32):
        return nc.dram_tensor(name, shape, dt, kind="ExternalInput").ap()

    xg = din("xg", [8192, 1024]); c_col = din("c_col", [128, 8]); pos = din("pos", [128, 33], I32)
    flag = din("flag", [128, 1])
    w_ada = din("w_ada", [1024, 6144]); b_ada = din("b_ada", [1, 6144])
    b_ada_col = din("b_ada_col", [128, 48]); nm_col = din("nm_col", [128, 8]); nf_col = din("nf_col", [128, 8])
    w_in = din("w_in", [1024, 2832]); conv_wT = din("conv_wT", [128, 48]); a_log = din("a_log", [1, 8])
    dt_bias = din("dt_bias", [1, 8]); gon = din("gon", [1, 64]); qnw = din("qnw", [1, 64]); knw = din("knw", [1, 64])
    sinks = din("sinks", [1, 8]); w_out = din("w_out", [1024, 1024])
    w_gr = din("w_gr", [1024, 36]); b_gr = din("b_gr", [1, 36])
    wg_l = din("wg_l", [4096, 2048]); wu_l = din("wu_l", [4096, 2048]); wd_l = din("wd_l", [4096, 2048])
    c_Us = din("c_Us", [128, 128], BF16); c_meta0 = din("c_meta0", [128, 256], I32); c_minit = din("c_minit", [128, NSUB * 4], I32)
    c_thr = din("c_thr", [128, NTL]); c_pcol = din("c_pcol", [128, 1]); c_bnd = din("c_bnd", [128, 4], I32)
    c_ident = din("c_ident", [128, 128], BF16); c_U = din("c_U", [128, 128]); c_B = din("c_B", [128, 128])
    c_ind = din("c_ind", [128, 256]); c_mincl = din("c_mincl", [128, 128]); c_mslow = din("c_mslow", [128, 128])
    c_blk = din("c_blk", [128, 128], BF16); c_m01 = din("c_m01", [128, 256], BF16); c_invf = din("c_invf", [128, 32])
    y = nc.dram_tensor("y", [4096, 1024], F32, kind="ExternalOutput").ap()
    h2r_d = nc.dram_tensor("h2r_d", [4096, 1024], BF16, kind="Internal").ap()
    meta_d = nc.dram_tensor("meta_d", [NSLOT, 4], I32, kind="Internal").ap()
    contrib_d = nc.dram_tensor("contrib_d", [8192, 1024], F32, kind="Internal").ap()
    if dbg:
        dbg_o = nc.dram_tensor("dbg_o", [4096, 1024], F32, kind="ExternalOutput").ap()

    es0 = ExitStack()
    try:
      with es0:
        P = Prog(nc, es0)

        def cut(n):
            if CFG.get("cut") == n:
                P.emit()
                raise Cut()

        def mk(es):
            def sb(name, shape, dt=F32):
                return es.enter_context(nc.sbuf_tensor(name, shape, dt)), Buf(name)

            def ps(name, shape, dt=F32):
                return es.enter_context(nc.psum_tensor(name, shape, dt)), Buf(name)
            return sb, ps

        def fsz(ap):
            n = 1
            for d in ap.shape[1:]:
                n *= int(d)
            return n

        def MM(out, lhsT, rhs, start, stop, r, w):
            n = fsz(out)
            c = (30 + 1.8 * n) if lhsT.dtype == F32 else (30 + 0.45 * n)
            P.op("pe", lambda e: e.matmul(out, lhsT=lhsT, rhs=rhs, start=start, stop=stop), r, w, cost=c)

        def TR(out, in_, ident, r, w):
            P.op("pe", lambda e: e.transpose(out, in_, ident), r, w, cost=90)

        def ACT(out, in_, func, r, w, **kw):
            P.op("act", lambda e: e.activation(out=out, in_=in_, func=func, **kw), r, w, cost=200 + 0.83 * fsz(out))

        def TT(eng, out, in0, in1, op, r, w):
            c = (100 + 1.05 * fsz(out)) if eng == "dve" else (250 + 2.2 * fsz(out))
            P.op(eng, lambda e: e.tensor_tensor(out=out, in0=in0, in1=in1, op=op), r, w, cost=c)

        def TS(out, in0, s1, s2, op0, op1, r, w, eng="dve"):
            c = 100 + 1.05 * fsz(out)
            if op1 is None:
                P.op(eng, lambda e: e.tensor_scalar(out, in0, s1, None, op0=op0), r, w, cost=c)
            else:
                P.op(eng, lambda e: e.tensor_scalar(out, in0, s1, s2, op0=op0, op1=op1), r, w, cost=c)

        def STT(out, in0, scalar, in1, op0, op1, r, w):
            P.op("dve", lambda e: e.scalar_tensor_tensor(out=out, in0=in0, scalar=scalar, in1=in1, op0=op0, op1=op1), r, w,
                 cost=100 + 1.05 * fsz(out))

        def CP(eng, out, in_, r, w):
            c = (100 + 1.0 * fsz(out)) if eng == "dve" else (250 + 2.0 * fsz(out))
            P.op(eng, lambda e: e.tensor_copy(out, in_), r, w, cost=c)

        def RED(out, in_, op, r, w):
            P.op("dve", lambda e: e.tensor_reduce(out=out, in_=in_, axis=AX.X, op=op), r, w, cost=100 + 1.05 * fsz(in_))

        def RCP(out, in_, r, w):
            P.op("dve", lambda e: e.reciprocal(out, in_), r, w, cost=100 + 6.4 * fsz(out))

        def DMA(out, in_, r, w, sembuf=None, eng="sp"):
            nb = fsz(out) * int(out.shape[0]) * (2 if out.dtype == BF16 else 4)
            P.dma(lambda e: e.dma_start(out=out, in_=in_), r, w, sembuf, eng=eng, cost=2000 + nb / 150.0)

        def rsqrt_(dst, src, mul, add, bufs):
            TS(dst, src, mul, add, ALU.mult, ALU.add, bufs, bufs)
            ACT(dst, dst, AF.Sqrt, bufs, bufs)
            RCP(dst, dst, bufs, bufs)

        sb0, _ = mk(es0)
        G1, bG1 = sb0("G1", [128, 1024]); G2, bG2 = sb0("G2", [128, 1024])
        modc, bmodc = sb0("modc", [128, 32])
        epst, beps = sb0("epst", [128, 1])
        OH1, bOH1 = sb0("OH1", [128, 1024], BF16); OH2, bOH2 = sb0("OH2", [128, 1024], BF16)
        W12, bW12 = sb0("W12", [128, 64])
        flg, bflg = sb0("flg", [128, 1])
        by_d = [Buf("yd%d" % t) for t in range(32)]
        ysem = [Buf("ysem%d" % t) for t in range(2)]
        hsem = [Buf("hsem%d" % t) for t in range(2)]
        ysem2 = [Buf("ysemb%d" % t) for t in range(2)]
        bh2d = [Buf("h2d%d" % t) for t in range(32)]

        esW = ExitStack()
        with esW:
            sbW, _ = mk(esW)
            Winb, bWin = sbW("Winb", [128, 8, 2832], BF16)
            Woutb, bWout = sbW("Woutb", [128, 8, 1024], BF16)
            Wgrb, bWgr = sbW("Wgrb", [128, 8, 36], BF16)
            dg, bdg = sbW("dg", [128, 48, 128], BF16)
            ident, bid = sbW("ident", [128, 128], BF16)
            U32, bU = sbW("U32", [128, 128]); B32, bB = sbW("B32", [128, 128]); Cind, bCi = sbW("Cind", [128, 256])
            mincl, bmi = sbW("mincl", [128, 128]); mslow, bms = sbW("mslow", [128, 128])
            blk, bblk = sbW("blk", [128, 128], BF16); m01, bm01 = sbW("m01", [128, 256], BF16)
            invf, binv = sbW("invf", [128, 32])
            cst, bcst = sbW("cst", [128, 512])
            DMA(ident[:], c_ident[:, :], [], [bid]); DMA(U32[:], c_U[:, :], [], [bU]); DMA(B32[:], c_B[:, :], [], [bB])
            DMA(Cind[:], c_ind[:, :], [], [bCi]); DMA(mincl[:], c_mincl[:, :], [], [bmi]); DMA(mslow[:], c_mslow[:, :], [], [bms])
            DMA(blk[:], c_blk[:, :], [], [bblk]); DMA(m01[:], c_m01[:, :], [], [bm01]); DMA(invf[:], c_invf[:, :], [], [binv])
            DMA(flg[:], flag[:, :], [], [bflg])
            cst_l = [Buf("cst%d" % i) for i in range(8)]
            DMA(cst[:, 0:8], dt_bias[0:1, :].partition_broadcast(128), [], [cst_l[0]])
            DMA(cst[:, 8:16], a_log[0:1, :].partition_broadcast(128), [], [cst_l[1]])
            DMA(cst[:, 16:80], gon[0:1, :].partition_broadcast(128), [], [cst_l[2]])
            DMA(cst[:, 80:144], qnw[0:1, :].partition_broadcast(128), [], [cst_l[3]])
            DMA(cst[:, 144:208], knw[0:1, :].partition_broadcast(128), [], [cst_l[4]])
            DMA(cst[:, 208:216], sinks[0:1, :].partition_broadcast(128), [], [cst_l[5]])
            DMA(cst[:, 220:256], b_gr[0:1, :].partition_broadcast(128), [], [cst_l[6]])
            DMA(cst[:, 256:304], conv_wT[:, :], [], [cst_l[7]])
            cb = cst_l + [bcst]

            esP = ExitStack()
            with esP:
                sbP, psP = mk(esP)
                stg = [sbP("stg%d" % i, [128, 8, 512]) for i in range(2)]
                badt, bbad = sbP("badt", [128, 512])
                cact, bcact = sbP("cact", [128, 8]); cbb, bcbb = sbP("cbb", [128, 8, 128])
                pmod = [psP("pmod%d" % i, [128, 512]) for i in range(2)]
                ccol, bccol = sbP("ccol", [128, 8])
                tmpc, btmpc = sbP("tmpc", [128, 64])
                cut(1)
                ACT(cst[:, 8:16], cst[:, 8:16], AF.Exp, cb, cb)
                TS(cst[:, 8:16], cst[:, 8:16], -1.0, None, ALU.mult, None, cb, cb)
                TT("dve", tmpc[:, 0:64], cst[:, 80:144], cst[:, 80:144], ALU.mult, cb, [btmpc])
                RED(cst[:, 304:305], tmpc[:, 0:64], ALU.max, [btmpc], cb)
                TT("dve", tmpc[:, 0:64], cst[:, 144:208], cst[:, 144:208], ALU.mult, cb, [btmpc])
                RED(cst[:, 305:306], tmpc[:, 0:64], ALU.max, [btmpc], cb)
                TT("dve", cst[:, 306:307], cst[:, 304:305], cst[:, 305:306], ALU.mult, cb, cb)
                ACT(cst[:, 306:307], cst[:, 306:307], AF.Sqrt, cb, cb, scale=64.0)
                RED(cst[:, 307:308], cst[:, 208:216], ALU.max, cb, cb)
                TT("dve", cst[:, 306:307], cst[:, 306:307], cst[:, 307:308], ALU.max, cb, cb)
                TS(cst[:, 216:217], cst[:, 306:307], -1.0, None, ALU.mult, None, cb, cb)
                ACT(cst[:, 208:216], cst[:, 208:216], AF.Exp, cb, cb, bias=cst[:, 216:217])
                TS(cst[:, 80:144], cst[:, 80:144], 0.125, None, ALU.mult, None, cb, cb)
                cut(2)
                for jt in range(48):
                    TS(dg[:, jt, :], ident[:], cst[:, 256 + jt:257 + jt], None, ALU.mult, None, [bid] + cb, [bdg])
                cut(3)
                si = 0
                engs = ["act", "dve", "pool"]

                def cast(eng, out, in_, r, w):
                    if eng == "act":
                        ACT(out, in_, AF.Copy, r, w)
                    else:
                        CP(eng, out, in_, r, w)
                w_in_v = w_in.rearrange("(k p) n -> p k n", p=128)
                for cch in range(8):
                    st, bst = stg[si % 2]
                    DMA(st[:, :, 0:354], w_in_v[:, :, cch * 354:(cch + 1) * 354], [], [bst])
                    cast(engs[si % 3], Winb[:, :, cch * 354:(cch + 1) * 354], st[:, :, 0:354], [bst], [bWin])
                    si += 1
                w_out_v = w_out.rearrange("(k p) n -> p k n", p=128)
                for cch in range(2):
                    st, bst = stg[si % 2]
                    DMA(st[:, :, :], w_out_v[:, :, cch * 512:(cch + 1) * 512], [], [bst])
                    cast(engs[si % 3], Woutb[:, :, cch * 512:(cch + 1) * 512], st[:, :, :], [bst], [bWout])
                    si += 1
                st, bst = stg[si % 2]
                DMA(st[:, :, 0:36], w_gr.rearrange("(k p) n -> p k n", p=128), [], [bst])
                cast(engs[si % 3], Wgrb[:, :, :], st[:, :, 0:36], [bst], [bWgr])
                si += 1
                cut(4)
                P.op("pool", lambda e: e.memset(epst[:], EPS), [], [beps])
                DMA(ccol[:], c_col[:, :], [], [bccol])
                ACT(cact[:], ccol[:], AF.Silu, [bccol], [bcact])
                CP("dve", cbb[:], cact[:].unsqueeze(2).to_broadcast([128, 8, 128]), [bcact], [bcbb])
                bcolt, bbcol = sbP("bcolt", [128, 48]); nmc, bnmc = sbP("nmc", [128, 16]); colm, bcolm = sbP("colm", [128, 48])
                DMA(bcolt[:], b_ada_col[:, :], [], [bbcol])
                DMA(nmc[:, 0:8], nm_col[:, :], [], [bnmc]); DMA(nmc[:, 8:16], nf_col[:, :], [], [bnmc])
                pcol, bpcol = psP("pcol", [128, 64])
                w_ada_v = w_ada.rearrange("(k p) n -> p k n", p=128)
                for n in range(12):
                    st, bst = stg[si % 2]
                    si += 1
                    DMA(st[:, :, :], w_ada_v[:, :, n * 512:(n + 1) * 512], [], [bst])
                    if n in (4, 5, 10, 11):
                        DMA(badt[:], b_ada[0:1, n * 512:(n + 1) * 512].partition_broadcast(128), [], [bbad])
                        pm, bpm = pmod[n % 2]
                        for k in range(8):
                            MM(pm[:], cbb[:, k, :], st[:, k, :], k == 0, k == 7, [bcbb, bst], [bpm])
                        d, bd = (G1, bG1) if n < 6 else (G2, bG2)
                        dsl = d[:, (n % 2) * 512:(n % 2) * 512 + 512]
                        TT("dve", dsl, pm[:], badt[:], ALU.add, [bpm, bbad], [bd])
                    else:
                        for cc in range(4):
                            c = n * 4 + cc
                            for k in range(8):
                                MM(pcol[:, c:c + 1], st[:, k, cc * 128:(cc + 1) * 128], cact[:, k:k + 1], k == 0, k == 7, [bst, bcact], [bpcol])
                TT("dve", colm[:, 0:16], pcol[:, 0:16], bcolt[:, 0:16], ALU.add, [bpcol, bbcol], [bcolm])
                TT("dve", colm[:, 24:40], pcol[:, 24:40], bcolt[:, 24:40], ALU.add, [bpcol, bbcol], [bcolm])
                CP("dve", modc[:, 8:16], colm[:, 0:8], [bcolm], [bmodc])
                STT(modc[:, 0:8], colm[:, 8:16], 1.0, nmc[:, 0:8], ALU.add, ALU.mult, [bcolm, bnmc], [bmodc])
                CP("dve", modc[:, 24:32], colm[:, 24:32], [bcolm], [bmodc])
                STT(modc[:, 16:24], colm[:, 32:40], 1.0, nmc[:, 8:16], ALU.add, ALU.mult, [bcolm, bnmc], [bmodc])
                P.emit()
            P.barrier()

            S2, bS2 = sbW("S2", [128, 512]); Sb2, bSb = sbW("Sb2", [128, 512], BF16)
            pre, bpre = sbW("pre", [128, 12, 131], BF16)
            kTs = [sbW("kTs%d" % i, [128, 128], BF16) for i in range(3)]
            Vaug = [sbW("Vaug%d" % i, [128, 130], BF16) for i in range(3)]
            posi, bposi = sbW("posi", [128, 33], I32); posf, bposf = sbW("posf", [128, 33])

            def mixer_phase(tile_list, nset, own_phase, first):
              esM = ExitStack()
              with esM:
                sbM0, psM0 = mk(esM)
                tag = "o" if own_phase else "p"

                def sbM(name, shape, dt=F32):
                    return sbM0(name + tag, shape, dt)

                def psM(name, shape, dt=F32):
                    return psM0(name + tag, shape, dt)
                xt = [sbM("xt%d" % i, [128, 1024]) for i in range(3)]
                T4a_l = [sbM("T4a%d" % i, [128, 1024]) for i in range(nset)]
                T4b_l = [sbM("T4b%d" % i, [128, 1024]) for i in range(nset)]
                T4c_l = [sbM("T4c%d" % i, [128, 1024]) for i in range(nset)]
                jb_l = [sbM("jb%d" % i, [128, 1024], BF16) for i in range(nset)]
                hb_l = [sbM("hb%d" % i, [128, 1024], BF16) for i in range(nset)]
                hT_l = [sbM("hT%d" % i, [128, 1024], BF16) for i in range(nset)]
                vT_l = [sbM("vT%d" % i, [128, 4, 128], BF16) for i in range(nset)]
                vb_l = [sbM("vb%d" % i, [128, 512], BF16) for i in range(nset)]
                kbg_l = [sbM("kbg%d" % i, [128, 512], BF16) for i in range(nset)]
                Mm_l = [sbM("Mm%d" % i, [128, 1024], BF16) for i in range(nset)]
                MT_l = [sbM("MT%d" % i, [128, 1024], BF16) for i in range(nset)]
                XT_l = [[sbM("XT%d_%d" % (s, i), [128, 1024], BF16) for i in range(2)] for s in range(nset)]
                Pm_l = [[sbM("Pm%d_%d" % (s, i), [128, 1024], BF16) for i in range(2)] for s in range(nset)]
                PT_l = [[sbM("PT%d_%d" % (s, i), [128, 1024], BF16) for i in range(2)] for s in range(nset)]
                st8_l = [sbM("st8_%d" % i, [128, 8]) for i in range(nset)]
                qkT_ = [sbM("qkT%d" % i, [128, 8, 128], BF16) for i in range(2)]
                sc_ = [sbM("sc%d" % i, [128, 160]) for i in range(2)]
                sgl_ = [sbM("sgl%d" % i, [128, 16]) for i in range(2)]
                kdec_ = [sbM("kdec%d" % i, [128, 8, 128], BF16) for i in range(2)]
                u__ = [sbM("u%d" % i, [128, 512]) for i in range(2)]
                wT_ = [sbM("wT%d" % i, [128, 8, 128], BF16) for i in range(2)]
                vnew, bvn = sbM("vnew", [128, 512], BF16)
                if own_phase:
                    TL, bTL = sbM("TL", [128, 1024])
                    hb2, bhb2 = sbM("hb2", [128, 1024], BF16)
                    st8b, bst8b = sbM("st8b", [128, 8])
                    atT_ = [sbM("atT%d" % i, [128, 1024], BF16) for i in range(2)]
                    szb_ = [sbM("szb%d" % i, [128, 512], BF16) for i in range(2)]
                    o_, bo = sbM("o", [128, 512])
                    ocat, boc = sbM("ocat", [128, 1024], BF16); ocT, bocT = sbM("ocT", [128, 1024], BF16)
                    qr, bqr = sbM("qr", [128, 640], BF16)
                    qTs_ = [sbM("qTs%d" % i, [128, 512], BF16) for i in range(2)]
                    Pex = [sbM("Pex%d" % i, [128, 512], BF16) for i in range(2)]
                    Pmk = [sbM("Pmk%d" % i, [128, 512], BF16) for i in range(2)]
                    cs, bcs = sbM("cs", [128, 256]); ki, bki = sbM("ki", [128, 64], I32)
                    h2T, bh2T = sbM("h2T", [128, 1024], BF16)
                    rt, brt = sbM("rt", [128, 128]); rt1, brt1 = sbM("rt1", [128, 64])
                    qkc_t = sbM("qkc", [128, 1024]); rinv_t = sbM("rinv", [128, 1024]); swaraw, bswr = sbM("swaraw", [128, 768])
                else:
                    atT_ = szb_ = qTs_ = [(None, None), (None, None)]
                pT = [psM("pT%d" % i, [128, 1024], BF16) for i in range(2)]
                pF = [psM("pF%d" % i, [128, 512]) for i in range(6)]
                print("SBUF remaining (mixer phase own=%s)" % own_phase, nc.sbuf_bytes_remaining)

                if first:
                    P.op("pool", lambda e: e.memset(S2[:], 0.0), [], [bS2])
                    P.op("pool", lambda e: e.memset(Sb2[:], 0.0), [], [bSb])
                    P.op("pool", lambda e: e.memset(pre[:], 0.0), [], [bpre])
                    for i in range(3):
                        va, bva = Vaug[i]
                        P.op("pool", (lambda va: (lambda e: e.memset(va[:], 1.0)))(va), [], [bva])
                    DMA(posi[:], pos[:, :], [], [bposi])
                    CP("dve", posf[:], posi[:], [bposi], [bposf])

                def load_x(i):
                    x_, bx_ = xt[i % 3]
                    DMA(x_[:], xg[i * 128:(i + 1) * 128, :], [], [bx_])

                def norm_T(x_, bx_, c0, hbt, bhbt, stt_, bstt_, dstT, bdstT, pTi):
                    P.op("pool", lambda e: e.memset(stt_[:, 0:1], 0.0), [], [bstt_])
                    ACT(hbt[:], x_[:], AF.Square, [bx_], [bhbt, bstt_], accum_out=stt_[:, 0:1])
                    rsqrt_(stt_[:, 1:2], stt_[:, 0:1], 1.0 / 1024, EPS, [bstt_])
                    TS(hbt[:], x_[:], stt_[:, 1:2], None, ALU.mult, None, [bx_, bstt_], [bhbt])
                    p_, bp_ = pT[pTi]
                    for k in range(8):
                        TR(p_[:, k * 128:(k + 1) * 128], hbt[:, k * 128:(k + 1) * 128], ident[:], [bhbt, bid], [bp_])
                    for k in range(8):
                        ACT(dstT[:, k * 128:(k + 1) * 128], p_[:, k * 128:(k + 1) * 128], AF.Identity, [bp_, bmodc], [bdstT],
                            scale=modc[:, c0 + k:c0 + k + 1], bias=modc[:, c0 + 8 + k:c0 + 9 + k])

                def transpose8(src, bsrc, dstt, bdst, pTi, evac="act"):
                    p_, bp_ = pT[pTi]
                    for k in range(8):
                        TR(p_[:, k * 128:(k + 1) * 128], src[:, k * 128:(k + 1) * 128], ident[:], [bsrc, bid], [bp_])
                    if evac == "act":
                        ACT(dstt[:], p_[:], AF.Copy, [bp_], [bdst])
                    else:
                        CP("dve", dstt[:], p_[:], [bp_], [bdst])

                def stage1(i):
                    own = i >= OWN0
                    par = i % 2
                    ss_ = i % nset
                    T4a, bTa = T4a_l[ss_]; T4b, bTb = T4b_l[ss_]; T4c, bTc = T4c_l[ss_]
                    T4a_, bTa_ = T4a, bTa
                    jb, bjb = jb_l[ss_]; hb, bhb = hb_l[ss_]; hT, bhT = hT_l[ss_]; vT, bvT = vT_l[ss_]
                    vb, bvb = vb_l[ss_]; kbg, bkbg = kbg_l[ss_]; Mm, bMm = Mm_l[ss_]; MTt, bMT = MT_l[ss_]
                    XT = XT_l[ss_]; Pm_ = Pm_l[ss_]; PT_ = PT_l[ss_]; st8, bst8 = st8_l[ss_]
                    qkc, bqkc = qkc_t if own_phase else (T4c, bTc)
                    rinv, brinv = rinv_t if own_phase else (T4b, bTb)
                    BM = [0, 1, 2, 3] if own_phase else [2 * ss_, 2 * ss_ + 1, 2 * ss_, 2 * ss_ + 1]
                    pTs = 0 if own_phase else ss_

                    def LB(k):
                        return pF[BM[k]]
                    x_, bx_ = xt[i % 3]
                    qkT, bqkT = qkT_[par]; sc, bsc = sc_[par]; sgl, bsgl = sgl_[par]; kdec, bkdec = kdec_[par]
                    atT, batT = atT_[par]; u_, bu = u__[par]; wT, bwT = wT_[par]; szb, bszb = szb_[par]; qTs, bqTs = qTs_[par]
                    load_x(i)
                    norm_T(x_, bx_, 0, hb, bhb, st8, bst8, hT, bhT, pTs)
                    yield
                    jlist = list(range(12)) if i >= OWN0 - 1 else list(range(4, 12))
                    for j in jlist:
                        p_, bp_ = LB(j // 4)
                        for k in range(8):
                            MM(p_[:, (j % 4) * 128:(j % 4) * 128 + 128], Winb[:, k, j * 128:(j + 1) * 128],
                               hT[:, k * 128:(k + 1) * 128], k == 0, k == 7, [bWin, bhT], [bp_])
                        yield
                    for g in ([0, 1, 2] if i >= OWN0 - 1 else [1, 2]):
                        p_, bp_ = LB(g)
                        ACT(pre[:, g * 4:(g + 1) * 4, 3:131], p_[:].rearrange("p (a b) -> p a b", a=4), AF.Copy, [bp_], [bpre])
                    if i == OWN0:
                        TS(pre[:, :, 0:3], pre[:, :, 0:3], flg[:, 0:1], None, ALU.mult, None, [bpre, bflg], [bpre])
                    pab, bpab = LB(3)
                    for k in range(8):
                        MM(pab[:, 0:16], hT[:, k * 128:(k + 1) * 128], Winb[:, k, 2048:2064], k == 0, k == 7, [bhT, bWin], [bpab])
                    CP("dve", sc[:, 112:128], pab[:, 0:16], [bpab], [bsc])
                    yield
                    if i >= OWN0 - 1:
                        pkv, bpkv = LB(3)
                        for k in range(8):
                            MM(pkv[:, 0:256], hT[:, k * 128:(k + 1) * 128], Winb[:, k, 2576:2832], k == 0, k == 7, [bhT, bWin], [bpkv])
                        ACT(swaraw[:, 512:640], pkv[:, 0:128], AF.Copy, [bpkv], [bswr])
                        slot = i % 3
                        va, bva = Vaug[slot]
                        P.op("pool", (lambda va: (lambda e: e.memset(va[:].rearrange("p (h d) -> p h d", h=2)[:, :, 64:65], 1.0)))(va), [], [bva])
                        ACT(va[:].rearrange("p (h d) -> p h d", h=2)[:, :, 0:64], pkv[:, 128:256].rearrange("p (h d) -> p h d", h=2), AF.Copy, [bpkv], [bva])
                        if i == OWN0 - 1:
                            TS(va[:], va[:], flg[:, 0:1], None, ALU.mult, None, [bva, bflg], [bva])
                        if own:
                            pq, bpq = LB(0)
                            for k in range(8):
                                MM(pq[:], hT[:, k * 128:(k + 1) * 128], Winb[:, k, 2064:2576], k == 0, k == 7, [bhT, bWin], [bpq])
                            ACT(swaraw[:, 0:512], pq[:], AF.Copy, [bpq], [bswr])
                            pz, bpz = LB(1)
                            for k in range(8):
                                MM(pz[:], hT[:, k * 128:(k + 1) * 128], Winb[:, k, 1536:2048], k == 0, k == 7, [bhT, bWin], [bpz])
                            ACT(szb[:], pz[:], AF.Silu, [bpz], [bszb])
                        yield
                    def conv_group(g, bank):
                        p_, bp_ = LB(bank)
                        for jj in range(4):
                            j = g * 4 + jj
                            for tap in range(4):
                                MM(p_[:, jj * 128:(jj + 1) * 128], dg[:, j * 4 + tap, :], pre[:, j, tap:tap + 128],
                                   tap == 0, tap == 3, [bdg, bpre], [bp_])
                        return p_, bp_
                    pk_, bpk_ = conv_group(1, 0)
                    ACT(qkc[:, 512:1024], pk_[:], AF.Silu, [bpk_], [bqkc])
                    yield
                    pv_, bpv_ = conv_group(2, 1)
                    ACT(vT[:].rearrange("p a b -> p (a b)"), pv_[:], AF.Silu, [bpv_], [bvT])
                    yield
                    if own:
                        pq_, bpq_ = conv_group(0, 2)
                        ACT(qkc[:, 0:512], pq_[:], AF.Silu, [bpq_], [bqkc])
                    CP("pool", pre[:, :, 0:3], pre[:, :, 128:131], [bpre], [bpre])
                    yield
                    lo = 0 if own else 512
                    ACT(jb[:, lo:1024], qkc[:, lo:1024], AF.Square, [bqkc], [bjb])
                    for c in range(lo // 128, 8):
                        p_, bp_ = LB((3, 0)[c // 4])
                        MM(p_[:, (c % 4) * 128:(c % 4) * 128 + 128], blk[:], jb[:, c * 128:(c + 1) * 128], True, True, [bblk, bjb], [bp_])
                    for g in ([0, 1] if own else [1]):
                        p_, bp_ = LB((3, 0)[g])
                        ACT(rinv[:, g * 512:(g + 1) * 512], p_[:], AF.Ln, [bp_, beps], [brinv], bias=epst[:, 0:1])
                    ACT(rinv[:, lo:1024], rinv[:, lo:1024], AF.Exp, [brinv], [brinv], scale=-0.5)
                    if own:
                        STT(qkT[:, 0:4, :].rearrange("p a b -> p (a b)"), qkc[:, 0:512], 0.125, rinv[:, 0:512], ALU.mult, ALU.mult, [bqkc, brinv], [bqkT])
                    TT("pool", qkT[:, 4:8, :].rearrange("p a b -> p (a b)"), qkc[:, 512:1024], rinv[:, 512:1024], ALU.mult, [bqkc, brinv], [bqkT])
                    yield
                    TT("dve", sc[:, 0:8], sc[:, 112:120], cst[:, 0:8], ALU.add, [bsc] + cb, [bsc])
                    ACT(sc[:, 8:16], sc[:, 0:8], AF.Exp, [bsc], [bsc])
                    ACT(sc[:, 16:24], sc[:, 8:16], AF.Ln, [bsc], [bsc], bias=1.0)
                    TT("dve", sc[:, 24:32], sc[:, 16:24], cst[:, 8:16], ALU.mult, [bsc] + cb, [bsc])
                    ACT(sc[:, 32:40], sc[:, 120:128], AF.Exp, [bsc], [bsc], scale=-1.0)
                    ACT(sc[:, 56:64], sc[:, 32:40], AF.Ln, [bsc], [bsc], bias=1.0)
                    TS(sc[:, 56:64], sc[:, 56:64], -1.0, None, ALU.mult, None, [bsc], [bsc])
                    ACT(sc[:, 48:56], sc[:, 56:64], AF.Exp, [bsc], [bsc])
                    yield
                    pg, bpg = LB(2)
                    MM(pg[:, 0:8], U32[:], sc[:, 24:32], True, True, [bU, bsc], [bpg])
                    MM(pg[:, 8:16], B32[:], sc[:, 24:32], True, True, [bB, bsc], [bpg])
                    MM(pg[:, 16:24], Cind[:, 0:128], sc[:, 24:32], True, True, [bCi, bsc], [bpg])
                    MM(pg[:, 24:32], Cind[:, 128:256], sc[:, 24:32], True, True, [bCi, bsc], [bpg])
                    CP("dve", sc[:, 128:160], pg[:, 0:32], [bpg], [bsc])
                    CP("dve", sc[:, 64:72], sc[:, 128:136], [bsc], [bsc])
                    TT("dve", sc[:, 72:80], sc[:, 64:72], sc[:, 56:64], ALU.add, [bsc], [bsc])
                    TT("dve", sc[:, 80:88], sc[:, 136:144], sc[:, 64:72], ALU.subtract, [bsc], [bsc])
                    ACT(sc[:, 88:96], sc[:, 64:72], AF.Exp, [bsc], [bsc])
                    ACT(sc[:, 96:104], sc[:, 80:88], AF.Exp, [bsc], [bsc])
                    ACT(sgl[:, 0:16], sc[:, 144:160], AF.Exp, [bsc], [bsgl])
                    TT("dve", sc[:, 104:112], sc[:, 48:56], sc[:, 88:96], ALU.mult, [bsc], [bsc])
                    yield
                    ptk, bptk = pT[pTs]
                    for m in range(4):
                        TR(ptk[:, m * 128:(m + 1) * 128], qkT[:, 4 + m, :], ident[:], [bqkT, bid], [bptk])
                        TR(ptk[:, 512 + m * 128:512 + (m + 1) * 128], vT[:, m, :], ident[:], [bvT, bid], [bptk])
                    k_tm = ptk[:, 0:512].rearrange("p (h d) -> p h d", h=8)
                    v_tm = ptk[:, 512:1024].rearrange("p (h d) -> p h d", h=8)

                    def bc8(col):
                        return sc[:, col:col + 8].unsqueeze(2).to_broadcast([128, 8, 64])
                    TT("dve", vb[:].rearrange("p (h d) -> p h d", h=8), v_tm, bc8(48), ALU.mult, [bptk, bsc], [bvb])
                    TT("dve", kbg[:].rearrange("p (h d) -> p h d", h=8), k_tm, bc8(104), ALU.mult, [bptk, bsc], [bkbg])
                    TT("dve", kdec[:, :, 0:64], k_tm, bc8(96), ALU.mult, [bptk, bsc], [bkdec])
                    CP("pool", kdec[:, :, 64:128], kdec[:, :, 0:64], [bkdec], [bkdec])
                    TT("pool", T4a[:].rearrange("p (h c) -> p h c", h=8), U32[:].unsqueeze(1).to_broadcast([128, 8, 128]),
                       sc[:, 24:32].unsqueeze(2).to_broadcast([128, 8, 128]), ALU.mult, [bU, bsc], [bTa])
                    yield
                    for hg in range(2):
                        pgr, bpgr = LB(hg)
                        MM(pgr[:], B32[:], T4a[:, hg * 512:(hg + 1) * 512], True, True, [bB, bTa], [bpgr])
                        gsl = slice(hg * 512, (hg + 1) * 512)
                        v3 = lambda t_: t_[:, gsl].rearrange("p (h c) -> p h c", h=4)
                        if own:
                            TT("dve", v3(T4b), pgr[:].rearrange("p (h c) -> p h c", h=4),
                               sc[:, 64 + hg * 4:68 + hg * 4].unsqueeze(2).to_broadcast([128, 4, 128]), ALU.subtract, [bpgr, bsc], [bTb])
                            TT("dve", v3(T4b), v3(T4b), mincl[:].unsqueeze(1).to_broadcast([128, 4, 128]), ALU.min, [bTb, bmi], [bTb])
                            ACT(T4b[:, gsl], T4b[:, gsl], AF.Exp, [bTb], [bTb])
                        TT("dve", v3(T4c), pgr[:].rearrange("p (h c) -> p h c", h=4),
                           sc[:, 72 + hg * 4:76 + hg * 4].unsqueeze(2).to_broadcast([128, 4, 128]), ALU.subtract, [bpgr, bsc], [bTc])
                        STT(v3(T4c), v3(T4c), -1.0, mslow[:].unsqueeze(1).to_broadcast([128, 4, 128]), ALU.mult, ALU.min, [bTc, bms], [bTc])
                        ACT(T4c[:, gsl], T4c[:, gsl], AF.Exp, [bTc], [bTc])
                        yield

                    def parv(t_, par_):
                        return t_[:].rearrange("p (m two c) -> p two m c", two=2, c=128)[:, par_]
                    for par_ in range(2):
                        base = par_ * 64
                        pkk, bpkk = LB(2 + par_)
                        for m in range(4):
                            MM(pkk[:, m * 128:(m + 1) * 128], qkT[base:base + 64, 4 + m, :], qkT[base:base + 64, 4 + m, :], True, True, [bqkT], [bpkk])
                        if own:
                            pqk, bpqk = LB(par_)
                            for m in range(4):
                                MM(pqk[:, m * 128:(m + 1) * 128], qkT[base:base + 64, 4 + m, :], qkT[base:base + 64, m, :], True, True, [bqkT], [bpqk])
                    for par_ in range(2):
                        pkk, bpkk = LB(2 + par_)
                        TT("dve", parv(Mm, par_), pkk[:].rearrange("p (m c) -> p m c", m=4), parv(T4c, par_), ALU.mult, [bpkk, bTc], [bMm])
                        if own:
                            pqk, bpqk = LB(par_)
                            TT("dve", parv(atT, par_), pqk[:].rearrange("p (m c) -> p m c", m=4), parv(T4b, par_), ALU.mult, [bpqk, bTb], [batT])
                    yield
                    pmt, bpmt = pT[pTs]
                    for h in range(8):
                        TR(pmt[:, h * 128:(h + 1) * 128], Mm[:, h * 128:(h + 1) * 128], ident[:], [bMm, bid], [bpmt])
                    ACT(MTt[:], pmt[:], AF.Copy, [bpmt], [bMT])
                    X0, bX0 = XT[0]
                    TT("pool", X0[:].rearrange("p (h c) -> p h c", h=8), ident[:].unsqueeze(1).to_broadcast([128, 8, 128]),
                       MTt[:].rearrange("p (h c) -> p h c", h=8), ALU.subtract, [bid, bMT], [bX0])
                    yield
                    curP, bcurP = Mm, bMm
                    curPT, bcurPT = MTt, bMT
                    curX, bcurX = XT[0]
                    for j in range(1, 6):
                        nP, bnP = Pm_[j % 2]
                        nPT, bnPT = PT_[j % 2]
                        nX, bnX = XT[j % 2]
                        for hg in range(2):
                            gsl = slice(hg * 512, (hg + 1) * 512)
                            pp, bpp = LB(hg)
                            for hh in range(4):
                                hs = slice((hg * 4 + hh) * 128, (hg * 4 + hh + 1) * 128)
                                MM(pp[:, hh * 128:(hh + 1) * 128], curPT[:, hs], curP[:, hs], True, True, [bcurPT, bcurP], [bpp])
                            ACT(nP[:, gsl], pp[:], AF.Copy, [bpp], [bnP])
                            yield
                            if j < 5:
                                pq2, bpq2 = LB(2 + hg)
                                for hh in range(4):
                                    hs = slice((hg * 4 + hh) * 128, (hg * 4 + hh + 1) * 128)
                                    MM(pq2[:, hh * 128:(hh + 1) * 128], curP[:, hs], curPT[:, hs], True, True, [bcurPT, bcurP], [bpq2])
                                CP("dve", nPT[:, gsl], pq2[:], [bpq2], [bnPT])
                                yield
                            px, bpx = LB(hg)
                            for hh in range(4):
                                hs = slice((hg * 4 + hh) * 128, (hg * 4 + hh + 1) * 128)
                                MM(px[:, hh * 128:(hh + 1) * 128], nP[:, hs], curX[:, hs], True, True, [bnP, bcurX], [bpx])
                            TT("dve", nX[:, gsl], px[:], curX[:, gsl], ALU.add, [bpx, bcurX], [bnX])
                            yield
                        curP, bcurP, curPT, bcurPT, curX, bcurX = nP, bnP, nPT, bnPT, nX, bnX
                    TTm, bTTm = curX, bcurX
                    pu, bpu = LB(0)
                    for h in range(8):
                        hs = slice(h * 128, (h + 1) * 128)
                        MM(pu[:, h * 64:(h + 1) * 64], TTm[:, hs], vb[:, h * 64:(h + 1) * 64], True, True, [bTTm, bvb], [bpu])
                    ACT(u_[:], pu[:], AF.Copy, [bpu], [bu])
                    yield
                    for hg in range(2):
                        pw_, bpw_ = LB(1) if hg == 0 else LB(0)
                        for hh in range(4):
                            h = hg * 4 + hh
                            hs = slice(h * 128, (h + 1) * 128)
                            MM(pw_[0:64, hh * 128:(hh + 1) * 128], kbg[:, h * 64:(h + 1) * 64], TTm[:, hs], True, True, [bkbg, bTTm], [bpw_])
                        CP("dve", wT[0:64, hg * 4:(hg + 1) * 4, :].rearrange("p a b -> p (a b)"), pw_[0:64, :], [bpw_], [bwT])
                    yield
                    if i >= OWN0 - 1:
                        ti = i - (OWN0 - 1)
                        TS(cs[:, 64:96], invf[:], posf[:, ti:ti + 1], None, ALU.mult, None, [binv, bposf], [bcs])
                        for (col, shift) in ((0, 0.75), (32, 0.5)):
                            t_ = cs[:, 96:128]
                            TS(t_, cs[:, 64:96], 1.0 / (2 * np.pi), shift, ALU.mult, ALU.add, [bcs], [bcs])
                            CP("dve", ki[:, 0:32], t_, [bcs], [bki])
                            CP("dve", cs[:, 128:160], ki[:, 0:32], [bki], [bcs])
                            TT("dve", t_, t_, cs[:, 128:160], ALU.subtract, [bcs], [bcs])
                            P.op("dve", lambda e: e.tensor_single_scalar(cs[:, 160:192], cs[:, 96:128], 0.0, op=ALU.is_lt), [bcs], [bcs])
                            TT("dve", t_, t_, cs[:, 160:192], ALU.add, [bcs], [bcs])
                            TS(t_, t_, 2 * np.pi, -np.pi, ALU.mult, ALU.add, [bcs], [bcs])
                            TS(t_, t_, 3.1415925, -3.1415925, ALU.min, ALU.max, [bcs], [bcs])
                            ACT(cs[:, col:col + 32], t_, AF.Sin, [bcs], [bcs])
                        yield
                        nh_l = [(swaraw, bswr, 512, 2, cst[:, 144:208], 8)]
                        if own:
                            nh_l.append((swaraw, bswr, 0, 8, cst[:, 80:144], 0))
                        for (pp, bpp, c0, nh, gain, dsth) in nh_l:
                            Tq, bTq = (T4a, bTa) if nh == 2 else (T4b, bTb)
                            ACT(Tq[:, 0:nh * 64], pp[:, c0:c0 + nh * 64], AF.Copy, [bpp], [bTq])
                        yield
                        for (pp, bpp, c0, nh, gain, dsth) in nh_l:
                            W = nh * 64
                            T4a, bTa = (T4a_, bTa_) if nh == 2 else (T4b, bTb)
                            a3 = T4a[:, 0:W].rearrange("p (h d) -> p h d", h=nh)
                            ACT(T4a[:, 512:512 + W], T4a[:, 0:W], AF.Square, [bTa], [bTa])
                            RED(rt1[:, 0:nh], T4a[:, 512:512 + W].rearrange("p (h d) -> p h d", h=nh), ALU.add, [bTa], [brt1])
                            rsqrt_(rt1[:, 16:16 + nh], rt1[:, 0:nh], 1.0 / 64, EPS, [brt1])
                            TT("dve", a3, a3, rt1[:, 16:16 + nh].unsqueeze(2).to_broadcast([128, nh, 64]), ALU.mult, [bTa, brt1], [bTa])
                            TT("dve", a3, a3, gain.unsqueeze(1).to_broadcast([128, nh, 64]), ALU.mult, [bTa] + cb, [bTa])
                            cosb = cs[:, 0:32].unsqueeze(1).to_broadcast([128, nh, 32])
                            sinb = cs[:, 32:64].unsqueeze(1).to_broadcast([128, nh, 32])
                            b3 = T4a[:, 512:512 + W].rearrange("p (h d) -> p h d", h=nh)
                            x1, x2 = a3[:, :, 0:32], a3[:, :, 32:64]
                            TT("pool", b3[:, :, 0:32], x1, cosb, ALU.mult, [bTa, bcs], [bTa])
                            TT("pool", b3[:, :, 32:64], x2, sinb, ALU.mult, [bTa, bcs], [bTa])
                            if nh == 8:
                                d3 = qr[:, 0:512].rearrange("p (a g d) -> p g a d", a=4, g=2)
                                def dv(lo_, hi_, d3=d3):
                                    return d3[:, :, :, lo_:hi_]
                                def sv(t3, lo_, hi_):
                                    return t3.rearrange("p (g a) d -> p g a d", g=2)[:, :, :, lo_:hi_]
                            else:
                                d3 = qr[:, 512:640].rearrange("p (h d) -> p h d", h=2)
                                def dv(lo_, hi_, d3=d3):
                                    return d3[:, :, lo_:hi_]
                                def sv(t3, lo_, hi_):
                                    return t3[:, :, lo_:hi_]
                            TT("pool", dv(0, 32), sv(b3, 0, 32), sv(b3, 32, 64), ALU.subtract, [bTa], [bqr])
                            TT("pool", b3[:, :, 0:32], x1, sinb, ALU.mult, [bTa, bcs], [bTa])
                            TT("pool", b3[:, :, 32:64], x2, cosb, ALU.mult, [bTa, bcs], [bTa])
                            TT("pool", dv(32, 64), sv(b3, 0, 32), sv(b3, 32, 64), ALU.add, [bTa], [bqr])
                            yield
                        pts, bpts = pT[pTs]
                        kT_, bkT_ = kTs[slot]
                        TR(pts[:, 512:640], qr[:, 512:640], ident[:], [bqr, bid], [bpts])
                        if own:
                            for a in range(4):
                                TR(pts[:, a * 128:(a + 1) * 128], qr[:, a * 128:(a + 1) * 128], ident[:], [bqr, bid], [bpts])
                            ACT(qTs[:], pts[:, 0:512], AF.Copy, [bpts], [bqTs])
                        ACT(kT_[:], pts[:, 512:640], AF.Copy, [bpts], [bkT_])
                        yield

                def stage2(i):
                    own = i >= OWN0
                    oi = i - OWN0
                    par = i % 2
                    x_, bx_ = xt[i % 3]
                    qkT, bqkT = qkT_[par]; sc, bsc = sc_[par]; sgl, bsgl = sgl_[par]; kdec, bkdec = kdec_[par]
                    atT, batT = atT_[par]; u_, bu = u__[par]; wT, bwT = wT_[par]; szb, bszb = szb_[par]; qTs, bqTs = qTs_[par]
                    if i == OWN0:
                        TS(S2[:], S2[:], flg[:, 0:1], None, ALU.mult, None, [bS2, bflg], [bS2])
                        ACT(Sb2[:], S2[:], AF.Copy, [bS2], [bSb])
                    for ch in range(2):
                        rows = slice(ch * 64, ch * 64 + 64)
                        pv2, bpv2 = pF[5]
                        for h in range(8):
                            MM(pv2[:, h * 64:(h + 1) * 64], wT[0:64, h, :], Sb2[0:64, h * 64:(h + 1) * 64], True, True, [bwT, bSb], [bpv2])
                        STT(vnew[rows, :], pv2[rows, :], -1.0, u_[rows, :], ALU.mult, ALU.add, [bu, bpv2], [bvn])
                        yield
                        if own:
                            po1 = [pF[4], pF[5]]
                            po2, bpo2 = pF[4]
                            for par_ in range(2):
                                base = par_ * 64
                                pp1, bpp1 = po1[par_]
                                for m in range(4):
                                    h = 2 * m + par_
                                    MM(pp1[:, m * 64:(m + 1) * 64], qkT[base:base + 64, m, :], Sb2[base:base + 64, h * 64:(h + 1) * 64], True, True, [bqkT, bSb], [bpp1])
                            for par_ in range(2):
                                pp1, bpp1 = po1[par_]
                                TT("dve", o_[rows, :].rearrange("p (m two d) -> p two m d", two=2, d=64)[:, par_],
                                   pp1[rows, 0:256].rearrange("p (m d) -> p m d", m=4),
                                   sc[rows, 88:96].rearrange("p (m two) -> p two m", two=2)[:, par_].unsqueeze(2).to_broadcast([64, 4, 64]),
                                   ALU.mult, [bpp1, bsc], [bo])
                            yield
                            for h in range(8):
                                MM(po2[:, h * 64:(h + 1) * 64], atT[rows, h * 128:(h + 1) * 128], vnew[rows, h * 64:(h + 1) * 64], True, True, [batT, bvn], [bpo2])
                            TT("dve", o_[rows, :], po2[rows, :], o_[rows, :], ALU.add, [bo, bpo2], [bo])
                            yield
                        pc, bpc = pF[5]
                        for h in range(8):
                            MM(pc[:, h * 64:(h + 1) * 64], kdec[rows, h, :], vnew[rows, h * 64:(h + 1) * 64], True, True, [bkdec, bvn], [bpc])
                        TT("dve", S2[:].rearrange("p (h d) -> p h d", h=8), S2[:].rearrange("p (h d) -> p h d", h=8),
                           sgl[:, ch * 8:(ch + 1) * 8].unsqueeze(2).to_broadcast([128, 8, 64]), ALU.mult, [bS2, bsgl], [bS2])
                        TT("dve", S2[:], pc[:], S2[:], ALU.add, [bS2, bpc], [bS2])
                        ACT(Sb2[:], S2[:], AF.Copy, [bS2], [bSb])
                        yield
                    if not own:
                        return
                    ACT(TL[:, 0:512], o_[:], AF.Square, [bo], [bTL])
                    RED(rt[:, 32:40], TL[:, 0:512].rearrange("p (h d) -> p h d", h=8), ALU.add, [bTL], [brt])
                    rsqrt_(rt[:, 40:48], rt[:, 32:40], 1.0 / 64, EPS, [brt])
                    o3 = o_[:].rearrange("p (h d) -> p h d", h=8)
                    TT("pool", o3, o3, rt[:, 40:48].unsqueeze(2).to_broadcast([128, 8, 64]), ALU.mult, [bo, brt], [bo])
                    TT("pool", o3, o3, cst[:, 16:80].unsqueeze(1).to_broadcast([128, 8, 64]), ALU.mult, [bo] + cb, [bo])
                    TT("pool", ocat[:, 0:512], o_[:], szb[:], ALU.mult, [bo, bszb], [boc])
                    yield
                    ppv = [pF[4], pF[4]]
                    for kvh in range(2):
                        ps_ = slice(kvh * 64, kvh * 64 + 64)
                        for bi_, sl_ in enumerate(((i - 1) % 3, i % 3)):
                            kT_, bkT_ = kTs[sl_]
                            psc, bpsc = pF[4 + bi_]
                            MM(psc[:], kT_[ps_, :], qTs[ps_, :], True, True, [bkT_, bqTs], [bpsc])
                            pe_, bpe_ = Pex[bi_]
                            pk2, bpk2 = Pmk[bi_]
                            ACT(pe_[:], psc[:], AF.Exp, [bpsc] + cb, [bpe_], bias=cst[:, 216:217])
                            TT("pool", pk2[:].rearrange("p (a q) -> p a q", a=4), pe_[:].rearrange("p (a q) -> p a q", a=4),
                               m01[:, bi_ * 128:(bi_ + 1) * 128].unsqueeze(1).to_broadcast([128, 4, 128]), ALU.mult, [bpe_, bm01], [bpk2])
                        pv_, bpv_ = ppv[kvh]
                        for a in range(4):
                            for bi_, sl_ in enumerate(((i - 1) % 3, i % 3)):
                                va, bva = Vaug[sl_]
                                pk2, bpk2 = Pmk[bi_]
                                MM(pv_[:, a * 65:(a + 1) * 65], pk2[:, a * 128:(a + 1) * 128], va[:, kvh * 65:(kvh + 1) * 65], bi_ == 0, bi_ == 1, [bpk2, bva], [bpv_])
                        pv3 = pv_[:, 0:260].rearrange("p (a d) -> p a d", a=4)
                        TT("dve", rt[:, 48:52], pv3[:, :, 64], cst[:, 208 + kvh * 4:212 + kvh * 4], ALU.add, [bpv_] + cb, [brt])
                        RCP(rt[:, 52:56], rt[:, 48:52], [brt], [brt])
                        TT("dve", ocat[:, 512 + kvh * 256:768 + kvh * 256].rearrange("p (a d) -> p a d", a=4), pv3[:, :, 0:64],
                           rt[:, 52:56].unsqueeze(2).to_broadcast([128, 4, 64]), ALU.mult, [bpv_, brt], [boc])
                        yield
                    transpose8(ocat, boc, ocT, bocT, 1)
                    py2 = [pF[4], pF[5]]
                    for nh_ in range(2):
                        p_, bp_ = py2[nh_]
                        for k in range(8):
                            MM(p_[:], ocT[:, k * 128:(k + 1) * 128], Woutb[:, k, nh_ * 512:(nh_ + 1) * 512], k == 0, k == 7, [bocT, bWout], [bp_])
                        sl_ = slice(nh_ * 512, (nh_ + 1) * 512)
                        TT("dve", TL[:, sl_], p_[:], G1[:, sl_], ALU.mult, [bp_, bG1], [bTL])
                        TT("dve", x_[:, sl_], TL[:, sl_], x_[:, sl_], ALU.add, [bTL, bx_], [bx_])
                    DMA(y[oi * 128:(oi + 1) * 128, :], x_[:], [bx_], [by_d[oi]], ysem[oi % 2])
                    yield
                    norm_T(x_, bx_, 16, hb2, bhb2, st8b, bst8b, h2T, bh2T, 1)
                    DMA(h2r_d[oi * 128:(oi + 1) * 128, :], hb2[:], [bhb2], [bh2d[oi]], hsem[oi % 2])
                    pr, bpr = pF[4]
                    for k in range(8):
                        MM(pr[:, 0:36], h2T[:, k * 128:(k + 1) * 128], Wgrb[:, k, :], k == 0, k == 7, [bh2T, bWgr], [bpr])
                    R = rt
                    bR = [brt]
                    TT("dve", R[:, 64:100], pr[:, 0:36], cst[:, 220:256], ALU.add, [bpr] + cb, bR)
                    RED(R[:, 100:101], R[:, 64:68], ALU.max, bR, bR)
                    TS(R[:, 101:102], R[:, 100:101], -1.0, None, ALU.mult, None, bR, bR)
                    P.op("pool", lambda e: e.memset(rt[:, 102:103], 0.0), [], bR)
                    ACT(R[:, 104:108], R[:, 64:68], AF.Exp, bR, bR, bias=R[:, 101:102], accum_out=R[:, 102:103])
                    RCP(R[:, 103:104], R[:, 102:103], bR, bR)
                    TS(R[:, 104:108], R[:, 64:68], R[:, 100:101], None, ALU.is_equal, None, bR, bR)
                    TT("dve", TL[:, 0:32].rearrange("p (g e) -> p g e", g=4), R[:, 68:100].rearrange("p (g e) -> p g e", g=4),
                       R[:, 104:108].unsqueeze(2).to_broadcast([128, 4, 8]), ALU.mult, bR, [bTL])
                    RED(R[:, 108:116], TL[:, 0:32].rearrange("p (g e) -> p e g", g=4), ALU.add, [bTL], bR)
                    RED(R[:, 116:117], R[:, 108:116], ALU.max, bR, bR)
                    TS(TL[:, 32:40], R[:, 108:116], R[:, 116:117], None, ALU.is_equal, None, bR, [bTL])
                    STT(TL[:, 40:48], TL[:, 32:40], -1e30, R[:, 108:116], ALU.mult, ALU.add, [bTL] + bR, [bTL])
                    RED(R[:, 117:118], TL[:, 40:48], ALU.max, [bTL], bR)
                    TS(TL[:, 48:56], TL[:, 40:48], R[:, 117:118], None, ALU.is_equal, None, [bTL] + bR, [bTL])
                    TT("dve", R[:, 118:119], R[:, 117:118], R[:, 116:117], ALU.subtract, bR, bR)
                    ACT(R[:, 119:120], R[:, 118:119], AF.Exp, bR, bR)
                    TS(R[:, 120:121], R[:, 119:120], 1.0, None, ALU.add, None, bR, bR)
                    RCP(R[:, 121:122], R[:, 120:121], bR, bR)
                    TT("dve", R[:, 122:123], R[:, 121:122], R[:, 103:104], ALU.mult, bR, bR)
                    TT("dve", R[:, 123:124], R[:, 122:123], R[:, 119:120], ALU.mult, bR, bR)
                    g48 = R[:, 104:108].unsqueeze(2).to_broadcast([128, 4, 8])
                    TT("dve", OH1[:, oi * 32:(oi + 1) * 32].rearrange("p (g e) -> p g e", g=4),
                       TL[:, 32:40].unsqueeze(1).to_broadcast([128, 4, 8]), g48, ALU.mult, [bTL] + bR, [bOH1])
                    TT("dve", OH2[:, oi * 32:(oi + 1) * 32].rearrange("p (g e) -> p g e", g=4),
                       TL[:, 48:56].unsqueeze(1).to_broadcast([128, 4, 8]), g48, ALU.mult, [bTL] + bR, [bOH2])
                    CP("dve", W12[:].rearrange("p (r t) -> p r t", r=2)[:, :, oi], R[:, 122:124], bR, [bW12])
                    yield

                tiles = list(tile_list)
                s1g = {}
                nxt = 0
                active = []

                def start_s1():
                    nonlocal nxt
                    g = stage1(tiles[nxt])
                    s1g[nxt] = g
                    active.append(g)
                    nxt += 1

                def step(g):
                    try:
                        next(g)
                        return True
                    except StopIteration:
                        if g in active:
                            active.remove(g)
                        return False
                for n, i in enumerate(tiles):
                    if nxt <= n:
                        start_s1()
                    g1 = s1g[n]
                    while g1 in active:
                        step(g1)
                    g2 = stage2(i)
                    active.append(g2)
                    while nxt < len(tiles) and nxt <= n + nset:
                        start_s1()
                    while g2 in active:
                        for g in list(active):
                            step(g)
                while active:
                    for g in list(active):
                        step(g)
                P.emit()

            tl_all = list(CFG['tiles']) if CFG['tiles'] is not None else list(range(NT))
            tl_pre = [t for t in tl_all if t < OWN0 - 1]
            tl_own = [t for t in tl_all if t >= OWN0 - 1]
            mixer_phase(tl_pre, CFG.get('nset', 2), False, True)
            P.barrier()
            mixer_phase(tl_own, 1, True, False)
            P.barrier()

        esE = ExitStack()
        with esE:
            sbE, psE = mk(esE)
            identE, bidE = sbE("m_identE", [128, 128], BF16)
            UsE, bUsE = sbE("m_UsE", [128, 128], BF16)
            onesE, bonesE = sbE("m_onesE", [128, 128], BF16)
            Asb, bAsb = sbE("m_Asb", [128, 2048])
            Bx, bBx = sbE("m_Bx", [128, 65 * 32])
            srt, bsrt = sbE("m_srt", [128, 256])
            E3, bE3 = sbE("m_E3", [128, NTL * 32])
            posf, bposf = sbE("m_posf", [128, 64]); posi, bposi = sbE("m_posi", [128, 64], I32)
            META, bMETA = sbE("m_META", [128, 256], I32)
            minit, bminit = sbE("m_minit", [128, NSUB * 4], I32)
            metaS, _bms = sbE("m_metaS", [128, NSUB * 4], I32)
            thr, bthr = sbE("m_thr", [128, NTL]); pcol, bpcol = sbE("m_pcol", [128, 1])
            idxw, bidxw = sbE("m_idxw", [128, 64], I32)
            DMA(identE[:], c_ident[:, :], [], [bidE]); DMA(UsE[:], c_Us[:, :], [], [bUsE])
            P.op("pool", lambda e: e.memset(onesE[:], 1.0), [], [bonesE])
            esS = ExitStack()
            with esS:
                _, psS = mk(esS)
                pA = [psS("m_pA%d" % i, [128, 512]) for i in range(4)]
                pB = [psS("m_pB%d" % i, [128, 512]) for i in range(4)]
                for v in range(64):
                    OHt, bOHt = (OH1, bOH1) if v < 32 else (OH2, bOH2)
                    rhs = OHt[:, (v % 32) * 32:(v % 32 + 1) * 32]
                    a_, ba_ = pA[v // 16]; b_, bb_ = pB[v // 16]
                    c0 = (v % 16) * 32
                    MM(a_[:, c0:c0 + 32], UsE[:], rhs, True, True, [bUsE, bOHt], [ba_])
                    MM(b_[:, c0:c0 + 32], onesE[:], rhs, True, True, [bonesE, bOHt], [bb_])
                for i in range(4):
                    ACT(Asb[:, i * 512:(i + 1) * 512], pA[i][0][:], AF.Copy, [pA[i][1]], [bAsb])
                    CP("dve", Bx[:, 32 + i * 512:32 + (i + 1) * 512], pB[i][0][:], [pB[i][1]], [bBx])
                P.emit()
            P.barrier()
            P.op("pool", lambda e: e.memset(Bx[:, 0:32], 0.0), [], [bBx])
            for v in range(2, 65):
                TT("dve", Bx[:, v * 32:(v + 1) * 32], Bx[:, v * 32:(v + 1) * 32], Bx[:, (v - 1) * 32:v * 32], ALU.add, [bBx], [bBx])
            cnt = Bx[:, 2048:2080]
            nt_ = srt[:, 0:32]; pc_ = srt[:, 32:64]; st_ = srt[:, 64:96]; en_ = srt[:, 96:128]
            TS(nt_, cnt, 0.0, None, ALU.is_gt, None, [bBx], [bsrt])
            for jj in range(1, 8):
                STT(nt_, cnt, 512.0 * jj, nt_, ALU.is_gt, ALU.add, [bBx, bsrt], [bsrt])
            TS(pc_, nt_, 512.0, None, ALU.mult, None, [bsrt], [bsrt])
            P.op("pool", lambda e: e.memset(srt[:, 64:65], 0.0), [bsrt], [bsrt])
            for ee in range(1, 32):
                TT("dve", st_[:, ee:ee + 1], st_[:, ee - 1:ee], pc_[:, ee - 1:ee], ALU.add, [bsrt], [bsrt])
            TT("dve", en_, st_, pc_, ALU.add, [bsrt], [bsrt])
            A3 = Asb[:].rearrange("p (v e) -> p v e", e=32)
            TT("dve", Asb[:], Asb[:], Bx[:, 0:2048], ALU.add, [bAsb, bBx], [bAsb])
            TT("dve", A3, A3, st_.unsqueeze(1).to_broadcast([128, 64, 32]), ALU.add, [bAsb, bsrt], [bAsb])
            TT("dve", Asb[:, 0:1024], Asb[:, 0:1024], OH1[:], ALU.mult, [bAsb, bOH1], [bAsb])
            TT("dve", Asb[:, 1024:2048], Asb[:, 1024:2048], OH2[:], ALU.mult, [bAsb, bOH2], [bAsb])
            RED(posf[:], A3, ALU.add, [bAsb], [bposf])
            CP("dve", posi[:], posf[:], [bposf], [bposi])
            DMA(thr[:], c_thr[:, :], [], [bthr]); DMA(pcol[:], c_pcol[:, :], [], [bpcol])
            E33 = E3[:].rearrange("p (j e) -> p j e", e=32)
            TT("dve", E33, en_.unsqueeze(1).to_broadcast([128, NTL, 32]), thr[:].unsqueeze(2).to_broadcast([128, NTL, 32]), ALU.is_le,
               [bsrt, bthr], [bE3])
            bsrt2 = Buf("srt2")
            RED(srt[:, 128:128 + NTL], E33, ALU.add, [bE3], [bsrt2])
            TS(srt[:, 192:192 + NTL], srt[:, 128:128 + NTL], 128.0, pcol[:, 0:1], ALU.mult, ALU.add, [bsrt2, bpcol], [bsrt2])
            CP("dve", idxw[:, 0:NTL], srt[:, 192:192 + NTL], [bsrt2], [bidxw])
            DMA(META[:], c_meta0[:, :], [], [bMETA])
            MF = META[:].bitcast(F32).rearrange("p (v c) -> p v c", c=4)
            CP("dve", MF[:, :, 1], W12[:], [bW12, bMETA], [bMETA])
            bminit_d = Buf("minit_d")
            DMA(minit[:], c_minit[:, :], [], [bminit])
            DMA(meta_d.rearrange("(p n) c -> p (n c)", p=128), minit[:], [bminit], [bminit_d])

            bndt, bbndt = sbE("m_bndt", [128, 4], I32)
            DMA(bndt[:], c_bnd[:, :], [], [bbndt])
            bregs = Buf("bregs")
            REG = {}
            for bi_, bv_ in enumerate((4095, 8191, NSLOT - 1)):
                REG[bv_] = nc.alloc_registers("bnd%d" % bi_, engines=[mybir.EngineType.Pool])
                P.op("pool", (lambda rg, ap_: (lambda e: nc.regs_load(rg, ap_)[-1]))(REG[bv_], bndt[0:1, bi_:bi_ + 1]), [bbndt, bregs], [bregs], cost=300)

            def IGATHER(out, src_, idx_ap, bound, r, w, sembuf, nbytes):
                P.dma(lambda e: e.indirect_dma_start(out=out, out_offset=None, in_=src_,
                                                    in_offset=bass.IndirectOffsetOnAxis(ap=idx_ap, axis=0),
                                                    bounds_check=REG[bound], oob_is_err=False),
                      list(r) + [bregs], w, sembuf, eng="pool", cost=2500 + nbytes / 150.0, issue=1200.0)

            def ISCATTER(dst, idx_ap, src_, bound, r, w, sembuf, nbytes):
                def f_(e):
                    try:
                        return e.indirect_dma_start(out=dst, out_offset=bass.IndirectOffsetOnAxis(ap=idx_ap, axis=0),
                                                    in_=src_, in_offset=None, bounds_check=REG[bound], oob_is_err=False)
                    except Exception:
                        print("ISCATTER fail", dst.shape, dst.dtype, src_.shape, src_.dtype, idx_ap.shape, idx_ap.dtype, bound)
                        raise
                P.dma(f_, list(r) + [bregs], w, sembuf, eng="pool", cost=2500 + nbytes / 150.0, issue=1200.0)
            bmsc = Buf("msc")
            bmsAll = Buf("msAll"); bcoAll = Buf("coAll")
            bmsv = [Buf("msv%d" % v) for v in range(64)]
            for v in range(64):
                ISCATTER(meta_d[:, :], posi[:, v:v + 1], META[:, v * 4:(v + 1) * 4], NSLOT - 1, [bposi, bMETA, bminit_d], [bmsv[v], bmsAll], bmsc, 2048)
            bmS = [Buf("metaS%d" % q) for q in range(4)]
            bmSs = Buf("metaSs")
            for q in range(4):
                DMA(metaS[:, q * NTL * 4:(q + 1) * NTL * 4].rearrange("p (t c) -> p t c", c=4),
                    meta_d[q * NTL * 128:(q + 1) * NTL * 128, :].rearrange("(t p) c -> p t c", p=128), bmsv + [bminit_d], [bmS[q]], bmSs)
            MSI = metaS[:].rearrange("p (t c) -> p t c", c=4)
            MSF = metaS[:].bitcast(F32).rearrange("p (t c) -> p t c", c=4)

            Wg = [sbE("m_Wg%d" % i, [128, 8, 256], BF16) for i in range(3)]
            Wu = [sbE("m_Wu%d" % i, [128, 8, 256], BF16) for i in range(3)]
            Wd = [sbE("m_Wd%d" % i, [128, 2, 1024], BF16) for i in range(3)]
            Xg = [sbE("m_Xg%d" % i, [128, 4, 1024], BF16)[0] for i in range(3)]
            bXg = [[Buf("Xg%d_%d" % (i, s)) for s in range(4)] for i in range(3)]
            bXgs = [Buf("Xgs%d" % i) for i in range(3)]
            h2s = [sbE("m_h2s%d" % i, [128, 8, 512], BF16) for i in range(2)]
            sg = [sbE("m_sg%d" % i, [128, 512], BF16) for i in range(2)]
            hid = [sbE("m_hid%d" % i, [128, 512], BF16) for i in range(4)]
            yo = [sbE("m_yo%d" % i, [128, 1024]) for i in range(3)]
            xo = [sbE("m_xo%d" % i, [128, 1024]) for i in range(2)]
            c0b = [sbE("m_c0b%d" % i, [128, 1024]) for i in range(2)]
            c1b = [sbE("m_c1b%d" % i, [128, 1024]) for i in range(2)]
            pT2 = [psE("m_pT2_%d" % i, [128, 1024], BF16) for i in range(2)]
            pg_ = [psE("m_pg%d" % i, [128, 512]) for i in range(2)]
            pu_ = [psE("m_pu%d" % i, [128, 512]) for i in range(2)]
            pd, bpd = psE("m_pd", [128, 1024])
            print("SBUF remaining (moe)", nc.sbuf_bytes_remaining)
            for i in range(3):
                for s in range(4):
                    P.op("pool", (lambda i, s: (lambda e: e.memset(Xg[i][:, s, :], 0.0)))(i, s), [], [bXg[i][s]], cost=1500)
            bco = [Buf("co%d" % u) for u in range(NSUB)]

            def w_gather(j):
                sl = j % 3
                IGATHER(Wg[sl][0][:].rearrange("p k n -> p (k n)"), wg_l[:, :], idxw[:, j:j + 1], 4095, [bidxw], [Wg[sl][1]], None, 1 << 20)
                IGATHER(Wu[sl][0][:].rearrange("p k n -> p (k n)"), wu_l[:, :], idxw[:, j:j + 1], 4095, [bidxw], [Wu[sl][1]], None, 1 << 20)
                IGATHER(Wd[sl][0][:].rearrange("p k n -> p (k n)"), wd_l[:, :], idxw[:, j:j + 1], 4095, [bidxw], [Wd[sl][1]], None, 1 << 20)

            def x_gather(j):
                b3 = j % 3
                for s in range(4):
                    u = 4 * j + s
                    IGATHER(Xg[b3][:, s, :], h2r_d[:, :], MSI[:, u, 0:1], 4095, bmS, [bXg[b3][s]], None, 1 << 18)
            NTR = CFG.get('ntl', NTL)
            for j in range(min(2, NTR)):
                w_gather(j)
                x_gather(j)
            for j in range(NTR):
                if j + 2 < NTR:
                    w_gather(j + 2)
                    x_gather(j + 2)
                sl = j % 3
                wg_, bwg_ = Wg[sl]; wu_, bwu_ = Wu[sl]; wd_, bwd_ = Wd[sl]
                xg_ = Xg[sl]
                h2s_, bh2s_ = h2s[j % 2]
                for kp in range(4):
                    pt, bpt = pT2[kp % 2]
                    for kk in range(2):
                        k = kp * 2 + kk
                        for s in range(4):
                            TR(pt[:, kk * 512 + s * 128:kk * 512 + (s + 1) * 128], xg_[:, s, k * 128:(k + 1) * 128], identE[:],
                               bXg[sl] + [bidE], [bpt])
                    for kk in range(2):
                        k = kp * 2 + kk
                        ACT(h2s_[:, k, :], pt[:, kk * 512:(kk + 1) * 512], AF.Identity, [bpt, bmodc], [bh2s_],
                            scale=modc[:, 16 + k:17 + k], bias=modc[:, 24 + k:25 + k])
                for fc in range(2):
                    pgx, bpgx = pg_[fc]; pux, bpux = pu_[fc]
                    for k in range(8):
                        MM(pgx[:], wg_[:, k, fc * 128:(fc + 1) * 128], h2s_[:, k, :], k == 0, k == 7, [bwg_, bh2s_], [bpgx])
                    for k in range(8):
                        MM(pux[:], wu_[:, k, fc * 128:(fc + 1) * 128], h2s_[:, k, :], k == 0, k == 7, [bwu_, bh2s_], [bpux])
                    s_, bs_ = sg[fc]
                    hd, bhd = hid[(j % 2) * 2 + fc]
                    ACT(s_[:], pgx[:], AF.Silu, [bpgx], [bs_])
                    TT("dve", hd[:], pux[:], s_[:], ALU.mult, [bs_, bpux], [bhd])
                for t in range(4):
                    u = 4 * j + t
                    for nh_ in range(2):
                        for fc in range(2):
                            hd, bhd = hid[(j % 2) * 2 + fc]
                            MM(pd[:, nh_ * 512:(nh_ + 1) * 512], hd[:, t * 128:(t + 1) * 128], wd_[:, fc, nh_ * 512:(nh_ + 1) * 512], fc == 0, fc == 1,
                               [bhd, bwd_], [bpd])
                    yo_, byo_ = yo[u % 3]
                    STT(yo_[:], pd[:], MSF[:, u, 1:2], G2[:], ALU.mult, ALU.mult, [bpd, bG2] + bmS, [byo_])
                    ISCATTER(contrib_d[:, :], MSI[:, u, 2:3], yo_[:], 8191, [byo_] + bmS, [bco[u], bcoAll], byo_, 1 << 19)
            for oi in range(32):
                xo_, bxo_ = xo[oi % 2]; c0_, bc0_ = c0b[oi % 2]; c1_, bc1_ = c1b[oi % 2]
                DMA(xo_[:], y[oi * 128:(oi + 1) * 128, :], [by_d[oi]], [bxo_])
                DMA(c0_[:], contrib_d[oi * 128:(oi + 1) * 128, :], bco, [bc0_], eng="act")
                DMA(c1_[:], contrib_d[4096 + oi * 128:4096 + (oi + 1) * 128, :], bco, [bc1_], eng="act")
                TT("dve", c0_[:], c0_[:], c1_[:], ALU.add, [bc0_, bc1_], [bc0_])
                TT("dve", xo_[:], xo_[:], c0_[:], ALU.add, [bxo_, bc0_], [bxo_])
                DMA(y[oi * 128:(oi + 1) * 128, :], xo_[:], [bxo_], [by_d[oi]], ysem2[oi % 2], eng="pool")
            P.wait_all("sp", by_d)
            P.emit()
    except Cut:
        pass
    return nc


def _consts():
    idx = np.arange(128)
    same = (idx[:, None] // 64) == (idx[None, :] // 64)
    U = (same & (idx[:, None] <= idx[None, :])).astype(np.float32)
    B = same.astype(np.float32)
    ind = np.zeros((128, 2, 128), np.float32)
    ind[:64, 0, :] = 1.0
    ind[64:, 1, :] = 1.0
    mincl = np.where(same & (idx[None, :] >= idx[:, None]), 0.0, -30000.0).astype(np.float32)
    mslow = np.where(same & (idx[None, :] < idx[:, None]), 0.0, -30000.0).astype(np.float32)
    m01 = np.zeros((128, 2, 128), np.float32)
    m01[:, 0, :] = (idx[:, None] > idx[None, :])
    m01[:, 1, :] = (idx[:, None] <= idx[None, :])
    half = 32
    invf = (10000.0 ** (-np.arange(half, dtype=np.float32) / half)).astype(np.float32)
    Us = (idx[:, None] < idx[None, :]).astype(np.float32).astype(NPBF)
    meta0 = np.zeros((128, 64, 4), np.int32)
    vv = np.arange(64)
    meta0[:, :, 0] = (vv[None, :] % 32) * 128 + idx[:, None]
    meta0[:, :, 2] = (vv[None, :] // 32) * 4096 + (vv[None, :] % 32) * 128 + idx[:, None]
    extra = dict(c_Us=Us, c_meta0=meta0.reshape(128, 256), c_minit=np.full((128, NSUB * 4), 1 << 30, np.int32),
                 c_thr=np.ascontiguousarray(np.broadcast_to((512.0 * np.arange(NTL, dtype=np.float32))[None, :], (128, NTL))),
                 c_pcol=idx.astype(np.float32).reshape(128, 1),
                 c_bnd=np.ascontiguousarray(np.broadcast_to(np.array([4095, 8191, NSLOT - 1, 0], np.int32)[None, :], (128, 4))))
    return dict(c_ident=np.eye(128, dtype=np.float32).astype(NPBF), c_U=U, c_B=B, c_ind=ind.reshape(128, 256), **extra,
                c_mincl=mincl, c_mslow=mslow, c_blk=B.astype(NPBF), c_m01=m01.reshape(128, 256).astype(NPBF),
                c_invf=np.ascontiguousarray(np.broadcast_to(invf[None, :], (128, 32))))


_NC_CACHE = {}


def kernel(x, c, positions, w_ada, b_ada, norm_mix, w_in, conv_w, a_log, dt_bias, gdn_out_norm, q_norm, k_norm,
           sinks, w_out, norm_ffn, w_group, b_group, w_router, b_router, w_gate, w_up, w_down):
    f = lambda a: np.ascontiguousarray(np.asarray(a, dtype=np.float32))
    x = f(x); c = f(c); positions = np.asarray(positions).astype(np.int32)
    if "nc" not in _NC_CACHE:
        _NC_CACHE["nc"] = build(DBG)
    nc = _NC_CACHE["nc"]
    consts = _consts()
    shared = dict(
        w_ada=f(w_ada)[0], b_ada=f(b_ada), w_in=f(w_in)[0],
        b_ada_col=np.ascontiguousarray(f(b_ada).reshape(48, 128).T), nm_col=np.ascontiguousarray(f(norm_mix).reshape(8, 128).T),
        nf_col=np.ascontiguousarray(f(norm_ffn).reshape(8, 128).T),
        conv_wT=np.ascontiguousarray(f(conv_w)[0].T.reshape(12, 128, 4).transpose(1, 0, 2).reshape(128, 48)),
        a_log=f(a_log), dt_bias=f(dt_bias), gon=f(gdn_out_norm), qnw=f(q_norm), knw=f(k_norm), sinks=f(sinks),
        w_out=f(w_out)[0],
        w_gr=np.ascontiguousarray(np.concatenate([f(w_group)[0], f(w_router)[0]], axis=1)),
        b_gr=np.ascontiguousarray(np.concatenate([f(b_group), f(b_router)], axis=1)),
        wg_l=np.ascontiguousarray(f(w_gate)[0].reshape(32, 8, 128, 256).transpose(0, 2, 1, 3).reshape(4096, 2048)),
        wu_l=np.ascontiguousarray(f(w_up)[0].reshape(32, 8, 128, 256).transpose(0, 2, 1, 3).reshape(4096, 2048)),
        wd_l=np.ascontiguousarray(f(w_down)[0].reshape(32, 2, 128, 1024).transpose(0, 2, 1, 3).reshape(4096, 2048)),
        **consts)
    in_maps = []
    for core in range(8):
        b, half = core // 2, core % 2
        own = x[b, half * 4096:(half + 1) * 4096]
        xg = np.ascontiguousarray(np.concatenate([x[b, 0:4096], own], axis=0))
        if half == 1:
            pp = positions[b, 4096 - 128:8192]
        else:
            pp = np.concatenate([positions[b, 0:128], positions[b, 0:4096]])
        pos = np.ascontiguousarray(pp.reshape(33, 128).T)
        m = dict(shared)
        m.update(xg=xg, c_col=np.ascontiguousarray(c[b].reshape(8, 128).T), pos=pos,
                 flag=np.full((128, 1), float(half), np.float32))
        in_maps.append(m)
    res = run_bass_kernel_spmd(nc, in_maps, core_ids=list(range(8)))
    out = np.zeros((4, 8192, 1024), np.float32)
    for core in range(8):
        b, half = core // 2, core % 2
        out[b, half * 4096:(half + 1) * 4096] = res.results[core]["y"]
    if DBG:
        kernel.dbg = [res.results[core].get("dbg_o") for core in range(8)]
    return out
```

```python
import numpy as np
import concourse.bass as bass
import concourse.mybir as mybir
from concourse.bass_utils import run_bass_kernel_spmd
from contextlib import ExitStack
import ml_dtypes

F32 = mybir.dt.float32
BF16 = mybir.dt.bfloat16
I32 = mybir.dt.int32
ALU = mybir.AluOpType
AF = mybir.ActivationFunctionType
AX = mybir.AxisListType
NPBF = ml_dtypes.bfloat16


class Buf:
    __slots__ = ("name", "w", "readers", "dsem")

    def __init__(self, name):
        self.name = name
        self.w = None
        self.readers = []
        self.dsem = None


class Prog:
    ENG = ("pe", "act", "dve", "pool", "sp")
    LAT = 150.0

    def __init__(self, nc, es):
        self.nc = nc
        self.es = es
        self.recs = []
        self.cnt = {e: 0 for e in self.ENG}
        self.sems = {}
        self.semcnt = {}
        self.waited = {e: {} for e in self.ENG}
        for e in self.ENG[:4]:
            self._sem("E_" + e)
        self.nd = 0
        self.phase = 0
        self.pending_barrier = False
        self.sched = True
        self.final_wait_bufs = None

    def _sem(self, key):
        if key not in self.sems:
            self.sems[key] = self.es.enter_context(self.nc.semaphore("s_" + key))
            self.semcnt[key] = 0
        return key

    def op(self, eng, fn, reads=(), writes=(), cost=200.0):
        self.recs.append(dict(eng=eng, fn=fn, reads=list(reads), writes=list(writes), cost=float(cost), dma=False))

    def dma(self, fn, reads=(), writes=(), sembuf=None, eng="sp", cost=3000.0, issue=60.0):
        sb = sembuf if sembuf is not None else (writes[0] if writes else reads[0])
        if sb.dsem is None:
            self.nd += 1
            sb.dsem = self._sem("D%d_%s" % (self.nd, sb.name))
        key = sb.dsem
        self.semcnt[key] += 16
        self.recs.append(dict(eng=eng, fn=fn, reads=list(reads), writes=list(writes), cost=float(cost), dma=True,
                              tok=(key, self.semcnt[key]), issue=float(issue)))

    def wait_all(self, eng, bufs):
        self.final_wait_bufs = (eng, list(bufs))

    def barrier(self):
        self.pending_barrier = True

    def emit(self):
        import heapq
        recs = self.recs
        n = len(recs)
        ENG = self.ENG
        preds = [[] for _ in range(n)]
        ph = self.phase
        for i, r in enumerate(recs):
            for b in r["reads"]:
                if b.w is not None and b.w[0] == ph:
                    preds[i].append((b.w[1], "raw"))
            for b in r["writes"]:
                if b.w is not None and b.w[0] == ph:
                    preds[i].append((b.w[1], "waw"))
                for (p2, rid) in b.readers:
                    if p2 == ph and rid != i:
                        preds[i].append((rid, "war"))
            for b in r["reads"]:
                b.readers.append((ph, i))
            for b in r["writes"]:
                b.w = (ph, i)
                b.readers = []
        last_dma = {}
        for i, r in enumerate(recs):
            if r["dma"]:
                if r["eng"] in last_dma:
                    preds[i].append((last_dma[r["eng"]], "issue"))
                last_dma[r["eng"]] = i
        final = None
        if self.final_wait_bufs is not None:
            feng, fb = self.final_wait_bufs
            fp = []
            for b in fb:
                if b.w is not None and b.w[0] == ph:
                    fp.append(b.w[1])
                for (p2, rid) in b.readers:
                    if p2 == ph:
                        fp.append(rid)
            final = (feng, fp)
            self.final_wait_bufs = None
        order = {e: [] for e in ENG}
        if self.sched:
            succ = [[] for _ in range(n)]
            npred = [0] * n
            for i in range(n):
                ps = {}
                for (p, kd) in preds[i]:
                    ps[p] = ps.get(p, True) and kd == "issue"
                npred[i] = len(ps)
                for p, io in ps.items():
                    succ[p].append((i, io))
            finish = [0.0] * n
            startt = [0.0] * n
            ready = [0.0] * n
            heaps = {e: [] for e in ENG}
            free = {e: 0.0 for e in ENG}
            for i in range(n):
                if npred[i] == 0:
                    heapq.heappush(heaps[recs[i]["eng"]], (0.0, i))
            done = 0
            while done < n:
                best = None
                for e in ENG:
                    h = heaps[e]
                    if not h:
                        continue
                    t0 = max(free[e], h[0][0])
                    if best is None or t0 < best[0]:
                        best = (t0, e)
                t0, e = best
                h = heaps[e]
                cands = []
                while h and h[0][0] <= t0:
                    cands.append(heapq.heappop(h))
                cands.sort(key=lambda x: x[1])
                rt_, i = cands[0]
                for c in cands[1:]:
                    heapq.heappush(h, c)
                r = recs[i]
                startt[i] = t0
                if r["dma"]:
                    free[e] = t0 + r["issue"]
                    finish[i] = t0 + r["cost"]
                else:
                    free[e] = t0 + r["cost"]
                    finish[i] = free[e]
                order[e].append(i)
                done += 1
                for (s_, io) in succ[i]:
                    npred[s_] -= 1
                    lat = self.LAT if recs[s_]["eng"] != e or r["dma"] else 60.0
                    if io:
                        ready[s_] = max(ready[s_], t0 + r["issue"])
                    else:
                        ready[s_] = max(ready[s_], finish[i] + lat)
                    if npred[s_] == 0:
                        heapq.heappush(heaps[recs[s_]["eng"]], (ready[s_], s_))
            self.model_time = max(finish) if n else 0.0
            busy = {e: sum(recs[i]['cost'] if not recs[i]['dma'] else recs[i]['issue'] for i in order[e]) for e in ENG}
            print('[sched] phase', self.phase, 'ops', n, 'model_us', round(self.model_time / 1000, 1), 'busy_us', {e: round(v / 1000, 1) for e, v in busy.items()})
        else:
            for i, r in enumerate(recs):
                order[r["eng"]].append(i)
        tok = [None] * n
        for e in ENG:
            c = self.cnt[e]
            for i in order[e]:
                r = recs[i]
                if r["dma"]:
                    tok[i] = r["tok"]
                else:
                    c += 1
                    tok[i] = ("E_" + e, c)
            self.cnt[e] = c
        ins = {e: [] for e in ENG}
        for e in ENG:
            waited = self.waited[e]
            first = True
            for i in order[e]:
                r = recs[i]
                waits = {}
                if first and self.pending_barrier:
                    for key, val in self._barrier_vals.items():
                        if val > 0 and waited.get(key, 0) < val:
                            waited[key] = val
                            waits[key] = val
                first = False
                for (p, kind) in preds[i]:
                    pr = recs[p]
                    if kind == "issue":
                        continue
                    same = (not pr["dma"]) and (not r["dma"]) and pr["eng"] == e
                    if same and (e == "pe" or (kind != "raw" and e != "pool")):
                        continue
                    key, val = tok[p]
                    if waited.get(key, 0) >= val:
                        continue
                    waited[key] = val
                    waits[key] = max(waits.get(key, 0), val)
                if r["dma"]:
                    ins[e].append((list(waits.items()), r["fn"], r["tok"][0], 16))
                else:
                    ins[e].append((list(waits.items()), r["fn"], "E_" + e, 1))
            if first and self.pending_barrier:
                waits = {}
                for key, val in self._barrier_vals.items():
                    if val > 0 and waited.get(key, 0) < val:
                        waited[key] = val
                        waits[key] = val
                if waits:
                    ins[e].append((list(waits.items()), None, None, 0))
        if final is not None:
            feng, fp = final
            waits = {}
            waited = self.waited[feng]
            for p in fp:
                key, val = tok[p]
                if waited.get(key, 0) < val:
                    waited[key] = val
                    waits[key] = max(waits.get(key, 0), val)
            if waits:
                ins[feng].append((list(waits.items()), None, None, 0))
        self.pending_barrier = False
        self._barrier_vals = {}
        for e in ENG[:4]:
            self._barrier_vals["E_" + e] = self.cnt[e]
        for key in self.sems:
            if not key.startswith("E_"):
                self._barrier_vals[key] = self.semcnt[key]
        nc = self.nc
        sems = self.sems
        with nc.Block() as block:
            def run(engname):
                def body(e):
                    for (waits, fn, key, inc) in ins[engname]:
                        for (k, v) in waits:
                            e.wait_ge(sems[k], v)
                        if fn is not None:
                            fn(e).then_inc(sems[key], inc)
                return body
            block.tensor(run("pe"))
            block.scalar(run("act"))
            block.vector(run("dve"))
            block.gpsimd(run("pool"))
            block.sync(run("sp"))
        self.recs = []
        self.phase += 1


NT = 64
OWN0 = 32
NTL = 47
NSUB = NTL * 4
NSLOT = NSUB * 128
EPS = 1e-6
DBG = False
CFG = dict(tiles=None, moe=True, nex=32)


class Cut(Exception):
    pass


def build(dbg=False):
    nc = bass.Bass("TRN2", target_bir_lowering=False)

    def din(name, shape, dt=F32):
        return nc.dram_tensor(name, shape, dt, kind="ExternalInput").ap()

    xg = din("xg", [8192, 1024]); c_col = din("c_col", [128, 8]); pos = din("pos", [128, 33], I32)
    flag = din("flag", [128, 1])
    w_ada = din("w_ada", [1024, 6144]); b_ada = din("b_ada", [1, 6144])
    b_ada_col = din("b_ada_col", [128, 48]); nm_col = din("nm_col", [128, 8]); nf_col = din("nf_col", [128, 8])
    w_in = din("w_in", [1024, 2832]); conv_wT = din("conv_wT", [128, 48]); a_log = din("a_log", [1, 8])
    dt_bias = din("dt_bias", [1, 8]); gon = din("gon", [1, 64]); qnw = din("qnw", [1, 64]); knw = din("knw", [1, 64])
    sinks = din("sinks", [1, 8]); w_out = din("w_out", [1024, 1024])
    w_gr = din("w_gr", [1024, 36]); b_gr = din("b_gr", [1, 36])
    wg_l = din("wg_l", [4096, 2048]); wu_l = din("wu_l", [4096, 2048]); wd_l = din("wd_l", [4096, 2048])
    c_Us = din("c_Us", [128, 128], BF16); c_meta0 = din("c_meta0", [128, 256], I32); c_minit = din("c_minit", [128, NSUB * 4], I32)
    c_thr = din("c_thr", [128, NTL]); c_pcol = din("c_pcol", [128, 1]); c_bnd = din("c_bnd", [128, 4], I32)
    c_ident = din("c_ident", [128, 128], BF16); c_U = din("c_U", [128, 128]); c_B = din("c_B", [128, 128])
    c_ind = din("c_ind", [128, 256]); c_mincl = din("c_mincl", [128, 128]); c_mslow = din("c_mslow", [128, 128])
    c_blk = din("c_blk", [128, 128], BF16); c_m01 = din("c_m01", [128, 256], BF16); c_invf = din("c_invf", [128, 32])
    y = nc.dram_tensor("y", [4096, 1024], F32, kind="ExternalOutput").ap()
    h2r_d = nc.dram_tensor("h2r_d", [4096, 1024], BF16, kind="Internal").ap()
    meta_d = nc.dram_tensor("meta_d", [NSLOT, 4], I32, kind="Internal").ap()
    contrib_d = nc.dram_tensor("contrib_d", [8192, 1024], F32, kind="Internal").ap()
    if dbg:
        dbg_o = nc.dram_tensor("dbg_o", [4096, 1024], F32, kind="ExternalOutput").ap()

    es0 = ExitStack()
    try:
      with es0:
        P = Prog(nc, es0)

        def cut(n):
            if CFG.get("cut") == n:
                P.emit()
                raise Cut()

        def mk(es):
            def sb(name, shape, dt=F32):
                return es.enter_context(nc.sbuf_tensor(name, shape, dt)), Buf(name)

            def ps(name, shape, dt=F32):
                return es.enter_context(nc.psum_tensor(name, shape, dt)), Buf(name)
            return sb, ps

        def fsz(ap):
            n = 1
            for d in ap.shape[1:]:
                n *= int(d)
            return n

        def MM(out, lhsT, rhs, start, stop, r, w):
            n = fsz(out)
            c = (30 + 1.8 * n) if lhsT.dtype == F32 else (30 + 0.45 * n)
            P.op("pe", lambda e: e.matmul(out, lhsT=lhsT, rhs=rhs, start=start, stop=stop), r, w, cost=c)

        def TR(out, in_, ident, r, w):
            P.op("pe", lambda e: e.transpose(out, in_, ident), r, w, cost=90)

        def ACT(out, in_, func, r, w, **kw):
            P.op("act", lambda e: e.activation(out=out, in_=in_, func=func, **kw), r, w, cost=200 + 0.83 * fsz(out))

        def TT(eng, out, in0, in1, op, r, w):
            c = (100 + 1.05 * fsz(out)) if eng == "dve" else (250 + 2.2 * fsz(out))
            P.op(eng, lambda e: e.tensor_tensor(out=out, in0=in0, in1=in1, op=op), r, w, cost=c)

        def TS(out, in0, s1, s2, op0, op1, r, w, eng="dve"):
            c = 100 + 1.05 * fsz(out)
            if op1 is None:
                P.op(eng, lambda e: e.tensor_scalar(out, in0, s1, None, op0=op0), r, w, cost=c)
            else:
                P.op(eng, lambda e: e.tensor_scalar(out, in0, s1, s2, op0=op0, op1=op1), r, w, cost=c)

        def STT(out, in0, scalar, in1, op0, op1, r, w):
            P.op("dve", lambda e: e.scalar_tensor_tensor(out=out, in0=in0, scalar=scalar, in1=in1, op0=op0, op1=op1), r, w,
                 cost=100 + 1.05 * fsz(out))

        def CP(eng, out, in_, r, w):
            c = (100 + 1.0 * fsz(out)) if eng == "dve" else (250 + 2.0 * fsz(out))
            P.op(eng, lambda e: e.tensor_copy(out, in_), r, w, cost=c)

        def RED(out, in_, op, r, w):
            P.op("dve", lambda e: e.tensor_reduce(out=out, in_=in_, axis=AX.X, op=op), r, w, cost=100 + 1.05 * fsz(in_))

        def RCP(out, in_, r, w):
            P.op("dve", lambda e: e.reciprocal(out, in_), r, w, cost=100 + 6.4 * fsz(out))

        def DMA(out, in_, r, w, sembuf=None, eng="sp"):
            nb = fsz(out) * int(out.shape[0]) * (2 if out.dtype == BF16 else 4)
            P.dma(lambda e: e.dma_start(out=out, in_=in_), r, w, sembuf, eng=eng, cost=2000 + nb / 150.0)

        def rsqrt_(dst, src, mul, add, bufs):
            TS(dst, src, mul, add, ALU.mult, ALU.add, bufs, bufs)
            ACT(dst, dst, AF.Sqrt, bufs, bufs)
            RCP(dst, dst, bufs, bufs)

        sb0, _ = mk(es0)
        G1, bG1 = sb0("G1", [128, 1024]); G2, bG2 = sb0("G2", [128, 1024])
        modc, bmodc = sb0("modc", [128, 32])
        epst, beps = sb0("epst", [128, 1])
        OH1, bOH1 = sb0("OH1", [128, 1024], BF16); OH2, bOH2 = sb0("OH2", [128, 1024], BF16)
        W12, bW12 = sb0("W12", [128, 64])
        flg, bflg = sb0("flg", [128, 1])
        by_d = [Buf("yd%d" % t) for t in range(32)]
        ysem = [Buf("ysem%d" % t) for t in range(2)]
        hsem = [Buf("hsem%d" % t) for t in range(2)]
        ysem2 = [Buf("ysemb%d" % t) for t in range(2)]
        bh2d = [Buf("h2d%d" % t) for t in range(32)]

        esW = ExitStack()
        with esW:
            sbW, _ = mk(esW)
            Winb, bWin = sbW("Winb", [128, 8, 2832], BF16)
            Woutb, bWout = sbW("Woutb", [128, 8, 1024], BF16)
            Wgrb, bWgr = sbW("Wgrb", [128, 8, 36], BF16)
            dg, bdg = sbW("dg", [128, 48, 128], BF16)
            ident, bid = sbW("ident", [128, 128], BF16)
            U32, bU = sbW("U32", [128, 128]); B32, bB = sbW("B32", [128, 128]); Cind, bCi = sbW("Cind", [128, 256])
            mincl, bmi = sbW("mincl", [128, 128]); mslow, bms = sbW("mslow", [128, 128])
            blk, bblk = sbW("blk", [128, 128], BF16); m01, bm01 = sbW("m01", [128, 256], BF16)
            invf, binv = sbW("invf", [128, 32])
            cst, bcst = sbW("cst", [128, 512])
            DMA(ident[:], c_ident[:, :], [], [bid]); DMA(U32[:], c_U[:, :], [], [bU]); DMA(B32[:], c_B[:, :], [], [bB])
            DMA(Cind[:], c_ind[:, :], [], [bCi]); DMA(mincl[:], c_mincl[:, :], [], [bmi]); DMA(mslow[:], c_mslow[:, :], [], [bms])
            DMA(blk[:], c_blk[:, :], [], [bblk]); DMA(m01[:], c_m01[:, :], [], [bm01]); DMA(invf[:], c_invf[:, :], [], [binv])
            DMA(flg[:], flag[:, :], [], [bflg])
            cst_l = [Buf("cst%d" % i) for i in range(8)]
            DMA(cst[:, 0:8], dt_bias[0:1, :].partition_broadcast(128), [], [cst_l[0]])
            DMA(cst[:, 8:16], a_log[0:1, :].partition_broadcast(128), [], [cst_l[1]])
            DMA(cst[:, 16:80], gon[0:1, :].partition_broadcast(128), [], [cst_l[2]])
            DMA(cst[:, 80:144], qnw[0:1, :].partition_broadcast(128), [], [cst_l[3]])
            DMA(cst[:, 144:208], knw[0:1, :].partition_broadcast(128), [], [cst_l[4]])
            DMA(cst[:, 208:216], sinks[0:1, :].partition_broadcast(128), [], [cst_l[5]])
            DMA(cst[:, 220:256], b_gr[0:1, :].partition_broadcast(128), [], [cst_l[6]])
            DMA(cst[:, 256:304], conv_wT[:, :], [], [cst_l[7]])
            cb = cst_l + [bcst]

            esP = ExitStack()
            with esP:
                sbP, psP = mk(esP)
                stg = [sbP("stg%d" % i, [128, 8, 512]) for i in range(2)]
                badt, bbad = sbP("badt", [128, 512])
                cact, bcact = sbP("cact", [128, 8]); cbb, bcbb = sbP("cbb", [128, 8, 128])
                pmod = [psP("pmod%d" % i, [128, 512]) for i in range(2)]
                ccol, bccol = sbP("ccol", [128, 8])
                tmpc, btmpc = sbP("tmpc", [128, 64])
                cut(1)
                ACT(cst[:, 8:16], cst[:, 8:16], AF.Exp, cb, cb)
                TS(cst[:, 8:16], cst[:, 8:16], -1.0, None, ALU.mult, None, cb, cb)
                TT("dve", tmpc[:, 0:64], cst[:, 80:144], cst[:, 80:144], ALU.mult, cb, [btmpc])
                RED(cst[:, 304:305], tmpc[:, 0:64], ALU.max, [btmpc], cb)
                TT("dve", tmpc[:, 0:64], cst[:, 144:208], cst[:, 144:208], ALU.mult, cb, [btmpc])
                RED(cst[:, 305:306], tmpc[:, 0:64], ALU.max, [btmpc], cb)
                TT("dve", cst[:, 306:307], cst[:, 304:305], cst[:, 305:306], ALU.mult, cb, cb)
                ACT(cst[:, 306:307], cst[:, 306:307], AF.Sqrt, cb, cb, scale=64.0)
                RED(cst[:, 307:308], cst[:, 208:216], ALU.max, cb, cb)
                TT("dve", cst[:, 306:307], cst[:, 306:307], cst[:, 307:308], ALU.max, cb, cb)
                TS(cst[:, 216:217], cst[:, 306:307], -1.0, None, ALU.mult, None, cb, cb)
                ACT(cst[:, 208:216], cst[:, 208:216], AF.Exp, cb, cb, bias=cst[:, 216:217])
                TS(cst[:, 80:144], cst[:, 80:144], 0.125, None, ALU.mult, None, cb, cb)
                cut(2)
                for jt in range(48):
                    TS(dg[:, jt, :], ident[:], cst[:, 256 + jt:257 + jt], None, ALU.mult, None, [bid] + cb, [bdg])
                cut(3)
                si = 0
                engs = ["act", "dve", "pool"]

                def cast(eng, out, in_, r, w):
                    if eng == "act":
                        ACT(out, in_, AF.Copy, r, w)
                    else:
                        CP(eng, out, in_, r, w)
                w_in_v = w_in.rearrange("(k p) n -> p k n", p=128)
                for cch in range(8):
                    st, bst = stg[si % 2]
                    DMA(st[:, :, 0:354], w_in_v[:, :, cch * 354:(cch + 1) * 354], [], [bst])
                    cast(engs[si % 3], Winb[:, :, cch * 354:(cch + 1) * 354], st[:, :, 0:354], [bst], [bWin])
                    si += 1
                w_out_v = w_out.rearrange("(k p) n -> p k n", p=128)
                for cch in range(2):
                    st, bst = stg[si % 2]
                    DMA(st[:, :, :], w_out_v[:, :, cch * 512:(cch + 1) * 512], [], [bst])
                    cast(engs[si % 3], Woutb[:, :, cch * 512:(cch + 1) * 512], st[:, :, :], [bst], [bWout])
                    si += 1
                st, bst = stg[si % 2]
                DMA(st[:, :, 0:36], w_gr.rearrange("(k p) n -> p k n", p=128), [], [bst])
                cast(engs[si % 3], Wgrb[:, :, :], st[:, :, 0:36], [bst], [bWgr])
                si += 1
                cut(4)
                P.op("pool", lambda e: e.memset(epst[:], EPS), [], [beps])
                DMA(ccol[:], c_col[:, :], [], [bccol])
                ACT(cact[:], ccol[:], AF.Silu, [bccol], [bcact])
                CP("dve", cbb[:], cact[:].unsqueeze(2).to_broadcast([128, 8, 128]), [bcact], [bcbb])
                bcolt, bbcol = sbP("bcolt", [128, 48]); nmc, bnmc = sbP("nmc", [128, 16]); colm, bcolm = sbP("colm", [128, 48])
                DMA(bcolt[:], b_ada_col[:, :], [], [bbcol])
                DMA(nmc[:, 0:8], nm_col[:, :], [], [bnmc]); DMA(nmc[:, 8:16], nf_col[:, :], [], [bnmc])
                pcol, bpcol = psP("pcol", [128, 64])
                w_ada_v = w_ada.rearrange("(k p) n -> p k n", p=128)
                for n in range(12):
                    st, bst = stg[si % 2]
                    si += 1
                    DMA(st[:, :, :], w_ada_v[:, :, n * 512:(n + 1) * 512], [], [bst])
                    if n in (4, 5, 10, 11):
                        DMA(badt[:], b_ada[0:1, n * 512:(n + 1) * 512].partition_broadcast(128), [], [bbad])
                        pm, bpm = pmod[n % 2]
                        for k in range(8):
                            MM(pm[:], cbb[:, k, :], st[:, k, :], k == 0, k == 7, [bcbb, bst], [bpm])
                        d, bd = (G1, bG1) if n < 6 else (G2, bG2)
                        dsl = d[:, (n % 2) * 512:(n % 2) * 512 + 512]
                        TT("dve", dsl, pm[:], badt[:], ALU.add, [bpm, bbad], [bd])
                    else:
                        for cc in range(4):
                            c = n * 4 + cc
                            for k in range(8):
                                MM(pcol[:, c:c + 1], st[:, k, cc * 128:(cc + 1) * 128], cact[:, k:k + 1], k == 0, k == 7, [bst, bcact], [bpcol])
                TT("dve", colm[:, 0:16], pcol[:, 0:16], bcolt[:, 0:16], ALU.add, [bpcol, bbcol], [bcolm])
                TT("dve", colm[:, 24:40], pcol[:, 24:40], bcolt[:, 24:40], ALU.add, [bpcol, bbcol], [bcolm])
                CP("dve", modc[:, 8:16], colm[:, 0:8], [bcolm], [bmodc])
                STT(modc[:, 0:8], colm[:, 8:16], 1.0, nmc[:, 0:8], ALU.add, ALU.mult, [bcolm, bnmc], [bmodc])
                CP("dve", modc[:, 24:32], colm[:, 24:32], [bcolm], [bmodc])
                STT(modc[:, 16:24], colm[:, 32:40], 1.0, nmc[:, 8:16], ALU.add, ALU.mult, [bcolm, bnmc], [bmodc])
                P.emit()
            P.barrier()

            S2, bS2 = sbW("S2", [128, 512]); Sb2, bSb = sbW("Sb2", [128, 512], BF16)
            pre, bpre = sbW("pre", [128, 12, 131], BF16)
            kTs = [sbW("kTs%d" % i, [128, 128], BF16) for i in range(3)]
            Vaug = [sbW("Vaug%d" % i, [128, 130], BF16) for i in range(3)]
            posi, bposi = sbW("posi", [128, 33], I32); posf, bposf = sbW("posf", [128, 33])

            def mixer_phase(tile_list, nset, own_phase, first):
              esM = ExitStack()
              with esM:
                sbM0, psM0 = mk(esM)
                tag = "o" if own_phase else "p"

                def sbM(name, shape, dt=F32):
                    return sbM0(name + tag, shape, dt)

                def psM(name, shape, dt=F32):
                    return psM0(name + tag, shape, dt)
                xt = [sbM("xt%d" % i, [128, 1024]) for i in range(3)]
                T4a_l = [sbM("T4a%d" % i, [128, 1024]) for i in range(nset)]
                T4b_l = [sbM("T4b%d" % i, [128, 1024]) for i in range(nset)]
                T4c_l = [sbM("T4c%d" % i, [128, 1024]) for i in range(nset)]
                jb_l = [sbM("jb%d" % i, [128, 1024], BF16) for i in range(nset)]
                hb_l = [sbM("hb%d" % i, [128, 1024], BF16) for i in range(nset)]
                hT_l = [sbM("hT%d" % i, [128, 1024], BF16) for i in range(nset)]
                vT_l = [sbM("vT%d" % i, [128, 4, 128], BF16) for i in range(nset)]
                vb_l = [sbM("vb%d" % i, [128, 512], BF16) for i in range(nset)]
                kbg_l = [sbM("kbg%d" % i, [128, 512], BF16) for i in range(nset)]
                Mm_l = [sbM("Mm%d" % i, [128, 1024], BF16) for i in range(nset)]
                MT_l = [sbM("MT%d" % i, [128, 1024], BF16) for i in range(nset)]
                XT_l = [[sbM("XT%d_%d" % (s, i), [128, 1024], BF16) for i in range(2)] for s in range(nset)]
                Pm_l = [[sbM("Pm%d_%d" % (s, i), [128, 1024], BF16) for i in range(2)] for s in range(nset)]
                PT_l = [[sbM("PT%d_%d" % (s, i), [128, 1024], BF16) for i in range(2)] for s in range(nset)]
                st8_l = [sbM("st8_%d" % i, [128, 8]) for i in range(nset)]
                qkT_ = [sbM("qkT%d" % i, [128, 8, 128], BF16) for i in range(2)]
                sc_ = [sbM("sc%d" % i, [128, 160]) for i in range(2)]
                sgl_ = [sbM("sgl%d" % i, [128, 16]) for i in range(2)]
                kdec_ = [sbM("kdec%d" % i, [128, 8, 128], BF16) for i in range(2)]
                u__ = [sbM("u%d" % i, [128, 512]) for i in range(2)]
                wT_ = [sbM("wT%d" % i, [128, 8, 128], BF16) for i in range(2)]
                vnew, bvn = sbM("vnew", [128, 512], BF16)
                if own_phase:
                    TL, bTL = sbM("TL", [128, 1024])
                    hb2, bhb2 = sbM("hb2", [128, 1024], BF16)
                    st8b, bst8b = sbM("st8b", [128, 8])
                    atT_ = [sbM("atT%d" % i, [128, 1024], BF16) for i in range(2)]
                    szb_ = [sbM("szb%d" % i, [128, 512], BF16) for i in range(2)]
                    o_, bo = sbM("o", [128, 512])
                    ocat, boc = sbM("ocat", [128, 1024], BF16); ocT, bocT = sbM("ocT", [128, 1024], BF16)
                    qr, bqr = sbM("qr", [128, 640], BF16)
                    qTs_ = [sbM("qTs%d" % i, [128, 512], BF16) for i in range(2)]
                    Pex = [sbM("Pex%d" % i, [128, 512], BF16) for i in range(2)]
                    Pmk = [sbM("Pmk%d" % i, [128, 512], BF16) for i in range(2)]
                    cs, bcs = sbM("cs", [128, 256]); ki, bki = sbM("ki", [128, 64], I32)
                    h2T, bh2T = sbM("h2T", [128, 1024], BF16)
                    rt, brt = sbM("rt", [128, 128]); rt1, brt1 = sbM("rt1", [128, 64])
                    qkc_t = sbM("qkc", [128, 1024]); rinv_t = sbM("rinv", [128, 1024]); swaraw, bswr = sbM("swaraw", [128, 768])
                else:
                    atT_ = szb_ = qTs_ = [(None, None), (None, None)]
                pT = [psM("pT%d" % i, [128, 1024], BF16) for i in range(2)]
                pF = [psM("pF%d" % i, [128, 512]) for i in range(6)]
                print("SBUF remaining (mixer phase own=%s)" % own_phase, nc.sbuf_bytes_remaining)

                if first:
                    P.op("pool", lambda e: e.memset(S2[:], 0.0), [], [bS2])
                    P.op("pool", lambda e: e.memset(Sb2[:], 0.0), [], [bSb])
                    P.op("pool", lambda e: e.memset(pre[:], 0.0), [], [bpre])
                    for i in range(3):
                        va, bva = Vaug[i]
                        P.op("pool", (lambda va: (lambda e: e.memset(va[:], 1.0)))(va), [], [bva])
                    DMA(posi[:], pos[:, :], [], [bposi])
                    CP("dve", posf[:], posi[:], [bposi], [bposf])

                def load_x(i):
                    x_, bx_ = xt[i % 3]
                    DMA(x_[:], xg[i * 128:(i + 1) * 128, :], [], [bx_])

                def norm_T(x_, bx_, c0, hbt, bhbt, stt_, bstt_, dstT, bdstT, pTi):
                    P.op("pool", lambda e: e.memset(stt_[:, 0:1], 0.0), [], [bstt_])
                    ACT(hbt[:], x_[:], AF.Square, [bx_], [bhbt, bstt_], accum_out=stt_[:, 0:1])
                    rsqrt_(stt_[:, 1:2], stt_[:, 0:1], 1.0 / 1024, EPS, [bstt_])
                    TS(hbt[:], x_[:], stt_[:, 1:2], None, ALU.mult, None, [bx_, bstt_], [bhbt])
                    p_, bp_ = pT[pTi]
                    for k in range(8):
                        TR(p_[:, k * 128:(k + 1) * 128], hbt[:, k * 128:(k + 1) * 128], ident[:], [bhbt, bid], [bp_])
                    for k in range(8):
                        ACT(dstT[:, k * 128:(k + 1) * 128], p_[:, k * 128:(k + 1) * 128], AF.Identity, [bp_, bmodc], [bdstT],
                            scale=modc[:, c0 + k:c0 + k + 1], bias=modc[:, c0 + 8 + k:c0 + 9 + k])

                def transpose8(src, bsrc, dstt, bdst, pTi, evac="act"):
                    p_, bp_ = pT[pTi]
                    for k in range(8):
                        TR(p_[:, k * 128:(k + 1) * 128], src[:, k * 128:(k + 1) * 128], ident[:], [bsrc, bid], [bp_])
                    if evac == "act":
                        ACT(dstt[:], p_[:], AF.Copy, [bp_], [bdst])
                    else:
                        CP("dve", dstt[:], p_[:], [bp_], [bdst])

                def stage1(i):
                    own = i >= OWN0
                    par = i % 2
                    ss_ = i % nset
                    T4a, bTa = T4a_l[ss_]; T4b, bTb = T4b_l[ss_]; T4c, bTc = T4c_l[ss_]
                    T4a_, bTa_ = T4a, bTa
                    jb, bjb = jb_l[ss_]; hb, bhb = hb_l[ss_]; hT, bhT = hT_l[ss_]; vT, bvT = vT_l[ss_]
                    vb, bvb = vb_l[ss_]; kbg, bkbg = kbg_l[ss_]; Mm, bMm = Mm_l[ss_]; MTt, bMT = MT_l[ss_]
                    XT = XT_l[ss_]; Pm_ = Pm_l[ss_]; PT_ = PT_l[ss_]; st8, bst8 = st8_l[ss_]
                    qkc, bqkc = qkc_t if own_phase else (T4c, bTc)
                    rinv, brinv = rinv_t if own_phase else (T4b, bTb)
                    BM = [0, 1, 2, 3] if own_phase else [2 * ss_, 2 * ss_ + 1, 2 * ss_, 2 * ss_ + 1]
                    pTs = 0 if own_phase else ss_

                    def LB(k):
                        return pF[BM[k]]
                    x_, bx_ = xt[i % 3]
                    qkT, bqkT = qkT_[par]; sc, bsc = sc_[par]; sgl, bsgl = sgl_[par]; kdec, bkdec = kdec_[par]
                    atT, batT = atT_[par]; u_, bu = u__[par]; wT, bwT = wT_[par]; szb, bszb = szb_[par]; qTs, bqTs = qTs_[par]
                    load_x(i)
                    norm_T(x_, bx_, 0, hb, bhb, st8, bst8, hT, bhT, pTs)
                    yield
                    jlist = list(range(12)) if i >= OWN0 - 1 else list(range(4, 12))
                    for j in jlist:
                        p_, bp_ = LB(j // 4)
                        for k in range(8):
                            MM(p_[:, (j % 4) * 128:(j % 4) * 128 + 128], Winb[:, k, j * 128:(j + 1) * 128],
                               hT[:, k * 128:(k + 1) * 128], k == 0, k == 7, [bWin, bhT], [bp_])
                        yield
                    for g in ([0, 1, 2] if i >= OWN0 - 1 else [1, 2]):
                        p_, bp_ = LB(g)
                        ACT(pre[:, g * 4:(g + 1) * 4, 3:131], p_[:].rearrange("p (a b) -> p a b", a=4), AF.Copy, [bp_], [bpre])
                    if i == OWN0:
                        TS(pre[:, :, 0:3], pre[:, :, 0:3], flg[:, 0:1], None, ALU.mult, None, [bpre, bflg], [bpre])
                    pab, bpab = LB(3)
                    for k in range(8):
                        MM(pab[:, 0:16], hT[:, k * 128:(k + 1) * 128], Winb[:, k, 2048:2064], k == 0, k == 7, [bhT, bWin], [bpab])
                    CP("dve", sc[:, 112:128], pab[:, 0:16], [bpab], [bsc])
                    yield
                    if i >= OWN0 - 1:
                        pkv, bpkv = LB(3)
                        for k in range(8):
                            MM(pkv[:, 0:256], hT[:, k * 128:(k + 1) * 128], Winb[:, k, 2576:2832], k == 0, k == 7, [bhT, bWin], [bpkv])
                        ACT(swaraw[:, 512:640], pkv[:, 0:128], AF.Copy, [bpkv], [bswr])
                        slot = i % 3
                        va, bva = Vaug[slot]
                        P.op("pool", (lambda va: (lambda e: e.memset(va[:].rearrange("p (h d) -> p h d", h=2)[:, :, 64:65], 1.0)))(va), [], [bva])
                        ACT(va[:].rearrange("p (h d) -> p h d", h=2)[:, :, 0:64], pkv[:, 128:256].rearrange("p (h d) -> p h d", h=2), AF.Copy, [bpkv], [bva])
                        if i == OWN0 - 1:
                            TS(va[:], va[:], flg[:, 0:1], None, ALU.mult, None, [bva, bflg], [bva])
                        if own:
                            pq, bpq = LB(0)
                            for k in range(8):
                                MM(pq[:], hT[:, k * 128:(k + 1) * 128], Winb[:, k, 2064:2576], k == 0, k == 7, [bhT, bWin], [bpq])
                            ACT(swaraw[:, 0:512], pq[:], AF.Copy, [bpq], [bswr])
                            pz, bpz = LB(1)
                            for k in range(8):
                                MM(pz[:], hT[:, k * 128:(k + 1) * 128], Winb[:, k, 1536:2048], k == 0, k == 7, [bhT, bWin], [bpz])
                            ACT(szb[:], pz[:], AF.Silu, [bpz], [bszb])
                        yield
                    def conv_group(g, bank):
                        p_, bp_ = LB(bank)
                        for jj in range(4):
                            j = g * 4 + jj
                            for tap in range(4):
                                MM(p_[:, jj * 128:(jj + 1) * 128], dg[:, j * 4 + tap, :], pre[:, j, tap:tap + 128],
                                   tap == 0, tap == 3, [bdg, bpre], [bp_])
                        return p_, bp_
                    pk_, bpk_ = conv_group(1, 0)
                    ACT(qkc[:, 512:1024], pk_[:], AF.Silu, [bpk_], [bqkc])
                    yield
                    pv_, bpv_ = conv_group(2, 1)
                    ACT(vT[:].rearrange("p a b -> p (a b)"), pv_[:], AF.Silu, [bpv_], [bvT])
                    yield
                    if own:
                        pq_, bpq_ = conv_group(0, 2)
                        ACT(qkc[:, 0:512], pq_[:], AF.Silu, [bpq_], [bqkc])
                    CP("pool", pre[:, :, 0:3], pre[:, :, 128:131], [bpre], [bpre])
                    yield
                    lo = 0 if own else 512
                    ACT(jb[:, lo:1024], qkc[:, lo:1024], AF.Square, [bqkc], [bjb])
                    for c in range(lo // 128, 8):
                        p_, bp_ = LB((3, 0)[c // 4])
                        MM(p_[:, (c % 4) * 128:(c % 4) * 128 + 128], blk[:], jb[:, c * 128:(c + 1) * 128], True, True, [bblk, bjb], [bp_])
                    for g in ([0, 1] if own else [1]):
                        p_, bp_ = LB((3, 0)[g])
                        ACT(rinv[:, g * 512:(g + 1) * 512], p_[:], AF.Ln, [bp_, beps], [brinv], bias=epst[:, 0:1])
                    ACT(rinv[:, lo:1024], rinv[:, lo:1024], AF.Exp, [brinv], [brinv], scale=-0.5)
                    if own:
                        STT(qkT[:, 0:4, :].rearrange("p a b -> p (a b)"), qkc[:, 0:512], 0.125, rinv[:, 0:512], ALU.mult, ALU.mult, [bqkc, brinv], [bqkT])
                    TT("pool", qkT[:, 4:8, :].rearrange("p a b -> p (a b)"), qkc[:, 512:1024], rinv[:, 512:1024], ALU.mult, [bqkc, brinv], [bqkT])
                    yield
                    TT("dve", sc[:, 0:8], sc[:, 112:120], cst[:, 0:8], ALU.add, [bsc] + cb, [bsc])
                    ACT(sc[:, 8:16], sc[:, 0:8], AF.Exp, [bsc], [bsc])
                    ACT(sc[:, 16:24], sc[:, 8:16], AF.Ln, [bsc], [bsc], bias=1.0)
                    TT("dve", sc[:, 24:32], sc[:, 16:24], cst[:, 8:16], ALU.mult, [bsc] + cb, [bsc])
                    ACT(sc[:, 32:40], sc[:, 120:128], AF.Exp, [bsc], [bsc], scale=-1.0)
                    ACT(sc[:, 56:64], sc[:, 32:40], AF.Ln, [bsc], [bsc], bias=1.0)
                    TS(sc[:, 56:64], sc[:, 56:64], -1.0, None, ALU.mult, None, [bsc], [bsc])
                    ACT(sc[:, 48:56], sc[:, 56:64], AF.Exp, [bsc], [bsc])
                    yield
                    pg, bpg = LB(2)
                    MM(pg[:, 0:8], U32[:], sc[:, 24:32], True, True, [bU, bsc], [bpg])
                    MM(pg[:, 8:16], B32[:], sc[:, 24:32], True, True, [bB, bsc], [bpg])
                    MM(pg[:, 16:24], Cind[:, 0:128], sc[:, 24:32], True, True, [bCi, bsc], [bpg])
                    MM(pg[:, 24:32], Cind[:, 128:256], sc[:, 24:32], True, True, [bCi, bsc], [bpg])
                    CP("dve", sc[:, 128:160], pg[:, 0:32], [bpg], [bsc])
                    CP("dve", sc[:, 64:72], sc[:, 128:136], [bsc], [bsc])
                    TT("dve", sc[:, 72:80], sc[:, 64:72], sc[:, 56:64], ALU.add, [bsc], [bsc])
                    TT("dve", sc[:, 80:88], sc[:, 136:144], sc[:, 64:72], ALU.subtract, [bsc], [bsc])
                    ACT(sc[:, 88:96], sc[:, 64:72], AF.Exp, [bsc], [bsc])
                    ACT(sc[:, 96:104], sc[:, 80:88], AF.Exp, [bsc], [bsc])
                    ACT(sgl[:, 0:16], sc[:, 144:160], AF.Exp, [bsc], [bsgl])
                    TT("dve", sc[:, 104:112], sc[:, 48:56], sc[:, 88:96], ALU.mult, [bsc], [bsc])
                    yield
                    ptk, bptk = pT[pTs]
                    for m in range(4):
                        TR(ptk[:, m * 128:(m + 1) * 128], qkT[:, 4 + m, :], ident[:], [bqkT, bid], [bptk])
                        TR(ptk[:, 512 + m * 128:512 + (m + 1) * 128], vT[:, m, :], ident[:], [bvT, bid], [bptk])
                    k_tm = ptk[:, 0:512].rearrange("p (h d) -> p h d", h=8)
                    v_tm = ptk[:, 512:1024].rearrange("p (h d) -> p h d", h=8)

                    def bc8(col):
                        return sc[:, col:col + 8].unsqueeze(2).to_broadcast([128, 8, 64])
                    TT("dve", vb[:].rearrange("p (h d) -> p h d", h=8), v_tm, bc8(48), ALU.mult, [bptk, bsc], [bvb])
                    TT("dve", kbg[:].rearrange("p (h d) -> p h d", h=8), k_tm, bc8(104), ALU.mult, [bptk, bsc], [bkbg])
                    TT("dve", kdec[:, :, 0:64], k_tm, bc8(96), ALU.mult, [bptk, bsc], [bkdec])
                    CP("pool", kdec[:, :, 64:128], kdec[:, :, 0:64], [bkdec], [bkdec])
                    TT("pool", T4a[:].rearrange("p (h c) -> p h c", h=8), U32[:].unsqueeze(1).to_broadcast([128, 8, 128]),
                       sc[:, 24:32].unsqueeze(2).to_broadcast([128, 8, 128]), ALU.mult, [bU, bsc], [bTa])
                    yield
                    for hg in range(2):
                        pgr, bpgr = LB(hg)
                        MM(pgr[:], B32[:], T4a[:, hg * 512:(hg + 1) * 512], True, True, [bB, bTa], [bpgr])
                        gsl = slice(hg * 512, (hg + 1) * 512)
                        v3 = lambda t_: t_[:, gsl].rearrange("p (h c) -> p h c", h=4)
                        if own:
                            TT("dve", v3(T4b), pgr[:].rearrange("p (h c) -> p h c", h=4),
                               sc[:, 64 + hg * 4:68 + hg * 4].unsqueeze(2).to_broadcast([128, 4, 128]), ALU.subtract, [bpgr, bsc], [bTb])
                            TT("dve", v3(T4b), v3(T4b), mincl[:].unsqueeze(1).to_broadcast([128, 4, 128]), ALU.min, [bTb, bmi], [bTb])
                            ACT(T4b[:, gsl], T4b[:, gsl], AF.Exp, [bTb], [bTb])
                        TT("dve", v3(T4c), pgr[:].rearrange("p (h c) -> p h c", h=4),
                           sc[:, 72 + hg * 4:76 + hg * 4].unsqueeze(2).to_broadcast([128, 4, 128]), ALU.subtract, [bpgr, bsc], [bTc])
                        STT(v3(T4c), v3(T4c), -1.0, mslow[:].unsqueeze(1).to_broadcast([128, 4, 128]), ALU.mult, ALU.min, [bTc, bms], [bTc])
                        ACT(T4c[:, gsl], T4c[:, gsl], AF.Exp, [bTc], [bTc])
                        yield

                    def parv(t_, par_):
                        return t_[:].rearrange("p (m two c) -> p two m c", two=2, c=128)[:, par_]
                    for par_ in range(2):
                        base = par_ * 64
                        pkk, bpkk = LB(2 + par_)
                        for m in range(4):
                            MM(pkk[:, m * 128:(m + 1) * 128], qkT[base:base + 64, 4 + m, :], qkT[base:base + 64, 4 + m, :], True, True, [bqkT], [bpkk])
                        if own:
                            pqk, bpqk = LB(par_)
                            for m in range(4):
                                MM(pqk[:, m * 128:(m + 1) * 128], qkT[base:base + 64, 4 + m, :], qkT[base:base + 64, m, :], True, True, [bqkT], [bpqk])
                    for par_ in range(2):
                        pkk, bpkk = LB(2 + par_)
                        TT("dve", parv(Mm, par_), pkk[:].rearrange("p (m c) -> p m c", m=4), parv(T4c, par_), ALU.mult, [bpkk, bTc], [bMm])
                        if own:
                            pqk, bpqk = LB(par_)
                            TT("dve", parv(atT, par_), pqk[:].rearrange("p (m c) -> p m c", m=4), parv(T4b, par_), ALU.mult, [bpqk, bTb], [batT])
                    yield
                    pmt, bpmt = pT[pTs]
                    for h in range(8):
                        TR(pmt[:, h * 128:(h + 1) * 128], Mm[:, h * 128:(h + 1) * 128], ident[:], [bMm, bid], [bpmt])
                    ACT(MTt[:], pmt[:], AF.Copy, [bpmt], [bMT])
                    X0, bX0 = XT[0]
                    TT("pool", X0[:].rearrange("p (h c) -> p h c", h=8), ident[:].unsqueeze(1).to_broadcast([128, 8, 128]),
                       MTt[:].rearrange("p (h c) -> p h c", h=8), ALU.subtract, [bid, bMT], [bX0])
                    yield
                    curP, bcurP = Mm, bMm
                    curPT, bcurPT = MTt, bMT
                    curX, bcurX = XT[0]
                    for j in range(1, 6):
                        nP, bnP = Pm_[j % 2]
                        nPT, bnPT = PT_[j % 2]
                        nX, bnX = XT[j % 2]
                        for hg in range(2):
                            gsl = slice(hg * 512, (hg + 1) * 512)
                            pp, bpp = LB(hg)
                            for hh in range(4):
                                hs = slice((hg * 4 + hh) * 128, (hg * 4 + hh + 1) * 128)
                                MM(pp[:, hh * 128:(hh + 1) * 128], curPT[:, hs], curP[:, hs], True, True, [bcurPT, bcurP], [bpp])
                            ACT(nP[:, gsl], pp[:], AF.Copy, [bpp], [bnP])
                            yield
                            if j < 5:
                                pq2, bpq2 = LB(2 + hg)
                                for hh in range(4):
                                    hs = slice((hg * 4 + hh) * 128, (hg * 4 + hh + 1) * 128)
                                    MM(pq2[:, hh * 128:(hh + 1) * 128], curP[:, hs], curPT[:, hs], True, True, [bcurPT, bcurP], [bpq2])
                                CP("dve", nPT[:, gsl], pq2[:], [bpq2], [bnPT])
                                yield
                            px, bpx = LB(hg)
                            for hh in range(4):
                                hs = slice((hg * 4 + hh) * 128, (hg * 4 + hh + 1) * 128)
                                MM(px[:, hh * 128:(hh + 1) * 128], nP[:, hs], curX[:, hs], True, True, [bnP, bcurX], [bpx])
                            TT("dve", nX[:, gsl], px[:], curX[:, gsl], ALU.add, [bpx, bcurX], [bnX])
                            yield
                        curP, bcurP, curPT, bcurPT, curX, bcurX = nP, bnP, nPT, bnPT, nX, bnX
                    TTm, bTTm = curX, bcurX
                    pu, bpu = LB(0)
                    for h in range(8):
                        hs = slice(h * 128, (h + 1) * 128)
                        MM(pu[:, h * 64:(h + 1) * 64], TTm[:, hs], vb[:, h * 64:(h + 1) * 64], True, True, [bTTm, bvb], [bpu])
                    ACT(u_[:], pu[:], AF.Copy, [bpu], [bu])
                    yield
                    for hg in range(2):
                        pw_, bpw_ = LB(1) if hg == 0 else LB(0)
                        for hh in range(4):
                            h = hg * 4 + hh
                            hs = slice(h * 128, (h + 1) * 128)
                            MM(pw_[0:64, hh * 128:(hh + 1) * 128], kbg[:, h * 64:(h + 1) * 64], TTm[:, hs], True, True, [bkbg, bTTm], [bpw_])
                        CP("dve", wT[0:64, hg * 4:(hg + 1) * 4, :].rearrange("p a b -> p (a b)"), pw_[0:64, :], [bpw_], [bwT])
                    yield
                    if i >= OWN0 - 1:
                        ti = i - (OWN0 - 1)
                        TS(cs[:, 64:96], invf[:], posf[:, ti:ti + 1], None, ALU.mult, None, [binv, bposf], [bcs])
                        for (col, shift) in ((0, 0.75), (32, 0.5)):
                            t_ = cs[:, 96:128]
                            TS(t_, cs[:, 64:96], 1.0 / (2 * np.pi), shift, ALU.mult, ALU.add, [bcs], [bcs])
                            CP("dve", ki[:, 0:32], t_, [bcs], [bki])
                            CP("dve", cs[:, 128:160], ki[:, 0:32], [bki], [bcs])
                            TT("dve", t_, t_, cs[:, 128:160], ALU.subtract, [bcs], [bcs])
                            P.op("dve", lambda e: e.tensor_single_scalar(cs[:, 160:192], cs[:, 96:128], 0.0, op=ALU.is_lt), [bcs], [bcs])
                            TT("dve", t_, t_, cs[:, 160:192], ALU.add, [bcs], [bcs])
                            TS(t_, t_, 2 * np.pi, -np.pi, ALU.mult, ALU.add, [bcs], [bcs])
                            TS(t_, t_, 3.1415925, -3.1415925, ALU.min, ALU.max, [bcs], [bcs])
                            ACT(cs[:, col:col + 32], t_, AF.Sin, [bcs], [bcs])
                        yield
                        nh_l = [(swaraw, bswr, 512, 2, cst[:, 144:208], 8)]
                        if own:
                            nh_l.append((swaraw, bswr, 0, 8, cst[:, 80:144], 0))
                        for (pp, bpp, c0, nh, gain, dsth) in nh_l:
                            Tq, bTq = (T4a, bTa) if nh == 2 else (T4b, bTb)
                            ACT(Tq[:, 0:nh * 64], pp[:, c0:c0 + nh * 64], AF.Copy, [bpp], [bTq])
                        yield
                        for (pp, bpp, c0, nh, gain, dsth) in nh_l:
                            W = nh * 64
                            T4a, bTa = (T4a_, bTa_) if nh == 2 else (T4b, bTb)
                            a3 = T4a[:, 0:W].rearrange("p (h d) -> p h d", h=nh)
                            ACT(T4a[:, 512:512 + W], T4a[:, 0:W], AF.Square, [bTa], [bTa])
                            RED(rt1[:, 0:nh], T4a[:, 512:512 + W].rearrange("p (h d) -> p h d", h=nh), ALU.add, [bTa], [brt1])
                            rsqrt_(rt1[:, 16:16 + nh], rt1[:, 0:nh], 1.0 / 64, EPS, [brt1])
                            TT("dve", a3, a3, rt1[:, 16:16 + nh].unsqueeze(2).to_broadcast([128, nh, 64]), ALU.mult, [bTa, brt1], [bTa])
                            TT("dve", a3, a3, gain.unsqueeze(1).to_broadcast([128, nh, 64]), ALU.mult, [bTa] + cb, [bTa])
                            cosb = cs[:, 0:32].unsqueeze(1).to_broadcast([128, nh, 32])
                            sinb = cs[:, 32:64].unsqueeze(1).to_broadcast([128, nh, 32])
                            b3 = T4a[:, 512:512 + W].rearrange("p (h d) -> p h d", h=nh)
                            x1, x2 = a3[:, :, 0:32], a3[:, :, 32:64]
                            TT("pool", b3[:, :, 0:32], x1, cosb, ALU.mult, [bTa, bcs], [bTa])
                            TT("pool", b3[:, :, 32:64], x2, sinb, ALU.mult, [bTa, bcs], [bTa])
                            if nh == 8:
                                d3 = qr[:, 0:512].rearrange("p (a g d) -> p g a d", a=4, g=2)
                                def dv(lo_, hi_, d3=d3):
                                    return d3[:, :, :, lo_:hi_]
                                def sv(t3, lo_, hi_):
                                    return t3.rearrange("p (g a) d -> p g a d", g=2)[:, :, :, lo_:hi_]
                            else:
                                d3 = qr[:, 512:640].rearrange("p (h d) -> p h d", h=2)
                                def dv(lo_, hi_, d3=d3):
                                    return d3[:, :, lo_:hi_]
                                def sv(t3, lo_, hi_):
                                    return t3[:, :, lo_:hi_]
                            TT("pool", dv(0, 32), sv(b3, 0, 32), sv(b3, 32, 64), ALU.subtract, [bTa], [bqr])
                            TT("pool", b3[:, :, 0:32], x1, sinb, ALU.mult, [bTa, bcs], [bTa])
                            TT("pool", b3[:, :, 32:64], x2, cosb, ALU.mult, [bTa, bcs], [bTa])
                            TT("pool", dv(32, 64), sv(b3, 0, 32), sv(b3, 32, 64), ALU.add, [bTa], [bqr])
                            yield
                        pts, bpts = pT[pTs]
                        kT_, bkT_ = kTs[slot]
                        TR(pts[:, 512:640], qr[:, 512:640], ident[:], [bqr, bid], [bpts])
                        if own:
                            for a in range(4):
                                TR(pts[:, a * 128:(a + 1) * 128], qr[:, a * 128:(a + 1) * 128], ident[:], [bqr, bid], [bpts])
                            ACT(qTs[:], pts[:, 0:512], AF.Copy, [bpts], [bqTs])
                        ACT(kT_[:], pts[:, 512:640], AF.Copy, [bpts], [bkT_])
                        yield

                def stage2(i):
                    own = i >= OWN0
                    oi = i - OWN0
                    par = i % 2
                    x_, bx_ = xt[i % 3]
                    qkT, bqkT = qkT_[par]; sc, bsc = sc_[par]; sgl, bsgl = sgl_[par]; kdec, bkdec = kdec_[par]
                    atT, batT = atT_[par]; u_, bu = u__[par]; wT, bwT = wT_[par]; szb, bszb = szb_[par]; qTs, bqTs = qTs_[par]
                    if i == OWN0:
                        TS(S2[:], S2[:], flg[:, 0:1], None, ALU.mult, None, [bS2, bflg], [bS2])
                        ACT(Sb2[:], S2[:], AF.Copy, [bS2], [bSb])
                    for ch in range(2):
                        rows = slice(ch * 64, ch * 64 + 64)
                        pv2, bpv2 = pF[5]
                        for h in range(8):
                            MM(pv2[:, h * 64:(h + 1) * 64], wT[0:64, h, :], Sb2[0:64, h * 64:(h + 1) * 64], True, True, [bwT, bSb], [bpv2])
                        STT(vnew[rows, :], pv2[rows, :], -1.0, u_[rows, :], ALU.mult, ALU.add, [bu, bpv2], [bvn])
                        yield
                        if own:
                            po1 = [pF[4], pF[5]]
                            po2, bpo2 = pF[4]
                            for par_ in range(2):
                                base = par_ * 64
                                pp1, bpp1 = po1[par_]
                                for m in range(4):
                                    h = 2 * m + par_
                                    MM(pp1[:, m * 64:(m + 1) * 64], qkT[base:base + 64, m, :], Sb2[base:base + 64, h * 64:(h + 1) * 64], True, True, [bqkT, bSb], [bpp1])
                            for par_ in range(2):
                                pp1, bpp1 = po1[par_]
                                TT("dve", o_[rows, :].rearrange("p (m two d) -> p two m d", two=2, d=64)[:, par_],
                                   pp1[rows, 0:256].rearrange("p (m d) -> p m d", m=4),
                                   sc[rows, 88:96].rearrange("p (m two) -> p two m", two=2)[:, par_].unsqueeze(2).to_broadcast([64, 4, 64]),
                                   ALU.mult, [bpp1, bsc], [bo])
                            yield
                            for h in range(8):
                                MM(po2[:, h * 64:(h + 1) * 64], atT[rows, h * 128:(h + 1) * 128], vnew[rows, h * 64:(h + 1) * 64], True, True, [batT, bvn], [bpo2])
                            TT("dve", o_[rows, :], po2[rows, :], o_[rows, :], ALU.add, [bo, bpo2], [bo])
                            yield
                        pc, bpc = pF[5]
                        for h in range(8):
                            MM(pc[:, h * 64:(h + 1) * 64], kdec[rows, h, :], vnew[rows, h * 64:(h + 1) * 64], True, True, [bkdec, bvn], [bpc])
                        TT("dve", S2[:].rearrange("p (h d) -> p h d", h=8), S2[:].rearrange("p (h d) -> p h d", h=8),
                           sgl[:, ch * 8:(ch + 1) * 8].unsqueeze(2).to_broadcast([128, 8, 64]), ALU.mult, [bS2, bsgl], [bS2])
                        TT("dve", S2[:], pc[:], S2[:], ALU.add, [bS2, bpc], [bS2])
                        ACT(Sb2[:], S2[:], AF.Copy, [bS2], [bSb])
                        yield
                    if not own:
                        return
                    ACT(TL[:, 0:512], o_[:], AF.Square, [bo], [bTL])
                    RED(rt[:, 32:40], TL[:, 0:512].rearrange("p (h d) -> p h d", h=8), ALU.add, [bTL], [brt])
                    rsqrt_(rt[:, 40:48], rt[:, 32:40], 1.0 / 64, EPS, [brt])
                    o3 = o_[:].rearrange("p (h d) -> p h d", h=8)
                    TT("pool", o3, o3, rt[:, 40:48].unsqueeze(2).to_broadcast([128, 8, 64]), ALU.mult, [bo, brt], [bo])
                    TT("pool", o3, o3, cst[:, 16:80].unsqueeze(1).to_broadcast([128, 8, 64]), ALU.mult, [bo] + cb, [bo])
                    TT("pool", ocat[:, 0:512], o_[:], szb[:], ALU.mult, [bo, bszb], [boc])
                    yield
                    ppv = [pF[4], pF[4]]
                    for kvh in range(2):
                        ps_ = slice(kvh * 64, kvh * 64 + 64)
                        for bi_, sl_ in enumerate(((i - 1) % 3, i % 3)):
                            kT_, bkT_ = kTs[sl_]
                            psc, bpsc = pF[4 + bi_]
                            MM(psc[:], kT_[ps_, :], qTs[ps_, :], True, True, [bkT_, bqTs], [bpsc])
                            pe_, bpe_ = Pex[bi_]
                            pk2, bpk2 = Pmk[bi_]
                            ACT(pe_[:], psc[:], AF.Exp, [bpsc] + cb, [bpe_], bias=cst[:, 216:217])
                            TT("pool", pk2[:].rearrange("p (a q) -> p a q", a=4), pe_[:].rearrange("p (a q) -> p a q", a=4),
                               m01[:, bi_ * 128:(bi_ + 1) * 128].unsqueeze(1).to_broadcast([128, 4, 128]), ALU.mult, [bpe_, bm01], [bpk2])
                        pv_, bpv_ = ppv[kvh]
                        for a in range(4):
                            for bi_, sl_ in enumerate(((i - 1) % 3, i % 3)):
                                va, bva = Vaug[sl_]
                                pk2, bpk2 = Pmk[bi_]
                                MM(pv_[:, a * 65:(a + 1) * 65], pk2[:, a * 128:(a + 1) * 128], va[:, kvh * 65:(kvh + 1) * 65], bi_ == 0, bi_ == 1, [bpk2, bva], [bpv_])
                        pv3 = pv_[:, 0:260].rearrange("p (a d) -> p a d", a=4)
                        TT("dve", rt[:, 48:52], pv3[:, :, 64], cst[:, 208 + kvh * 4:212 + kvh * 4], ALU.add, [bpv_] + cb, [brt])
                        RCP(rt[:, 52:56], rt[:, 48:52], [brt], [brt])
                        TT("dve", ocat[:, 512 + kvh * 256:768 + kvh * 256].rearrange("p (a d) -> p a d", a=4), pv3[:, :, 0:64],
                           rt[:, 52:56].unsqueeze(2).to_broadcast([128, 4, 64]), ALU.mult, [bpv_, brt], [boc])
                        yield
                    transpose8(ocat, boc, ocT, bocT, 1)
                    py2 = [pF[4], pF[5]]
                    for nh_ in range(2):
                        p_, bp_ = py2[nh_]
                        for k in range(8):
                            MM(p_[:], ocT[:, k * 128:(k + 1) * 128], Woutb[:, k, nh_ * 512:(nh_ + 1) * 512], k == 0, k == 7, [bocT, bWout], [bp_])
                        sl_ = slice(nh_ * 512, (nh_ + 1) * 512)
                        TT("dve", TL[:, sl_], p_[:], G1[:, sl_], ALU.mult, [bp_, bG1], [bTL])
                        TT("dve", x_[:, sl_], TL[:, sl_], x_[:, sl_], ALU.add, [bTL, bx_], [bx_])
                    DMA(y[oi * 128:(oi + 1) * 128, :], x_[:], [bx_], [by_d[oi]], ysem[oi % 2])
                    yield
                    norm_T(x_, bx_, 16, hb2, bhb2, st8b, bst8b, h2T, bh2T, 1)
                    DMA(h2r_d[oi * 128:(oi + 1) * 128, :], hb2[:], [bhb2], [bh2d[oi]], hsem[oi % 2])
                    pr, bpr = pF[4]
                    for k in range(8):
                        MM(pr[:, 0:36], h2T[:, k * 128:(k + 1) * 128], Wgrb[:, k, :], k == 0, k == 7, [bh2T, bWgr], [bpr])
                    R = rt
                    bR = [brt]
                    TT("dve", R[:, 64:100], pr[:, 0:36], cst[:, 220:256], ALU.add, [bpr] + cb, bR)
                    RED(R[:, 100:101], R[:, 64:68], ALU.max, bR, bR)
                    TS(R[:, 101:102], R[:, 100:101], -1.0, None, ALU.mult, None, bR, bR)
                    P.op("pool", lambda e: e.memset(rt[:, 102:103], 0.0), [], bR)
                    ACT(R[:, 104:108], R[:, 64:68], AF.Exp, bR, bR, bias=R[:, 101:102], accum_out=R[:, 102:103])
                    RCP(R[:, 103:104], R[:, 102:103], bR, bR)
                    TS(R[:, 104:108], R[:, 64:68], R[:, 100:101], None, ALU.is_equal, None, bR, bR)
                    TT("dve", TL[:, 0:32].rearrange("p (g e) -> p g e", g=4), R[:, 68:100].rearrange("p (g e) -> p g e", g=4),
                       R[:, 104:108].unsqueeze(2).to_broadcast([128, 4, 8]), ALU.mult, bR, [bTL])
                    RED(R[:, 108:116], TL[:, 0:32].rearrange("p (g e) -> p e g", g=4), ALU.add, [bTL], bR)
                    RED(R[:, 116:117], R[:, 108:116], ALU.max, bR, bR)
                    TS(TL[:, 32:40], R[:, 108:116], R[:, 116:117], None, ALU.is_equal, None, bR, [bTL])
                    STT(TL[:, 40:48], TL[:, 32:40], -1e30, R[:, 108:116], ALU.mult, ALU.add, [bTL] + bR, [bTL])
                    RED(R[:, 117:118], TL[:, 40:48], ALU.max, [bTL], bR)
                    TS(TL[:, 48:56], TL[:, 40:48], R[:, 117:118], None, ALU.is_equal, None, [bTL] + bR, [bTL])
                    TT("dve", R[:, 118:119], R[:, 117:118], R[:, 116:117], ALU.subtract, bR, bR)
                    ACT(R[:, 119:120], R[:, 118:119], AF.Exp, bR, bR)
                    TS(R[:, 120:121], R[:, 119:120], 1.0, None, ALU.add, None, bR, bR)
                    RCP(R[:, 121:122], R[:, 120:121], bR, bR)
                    TT("dve", R[:, 122:123], R[:, 121:122], R[:, 103:104], ALU.mult, bR, bR)
                    TT("dve", R[:, 123:124], R[:, 122:123], R[:, 119:120], ALU.mult, bR, bR)
                    g48 = R[:, 104:108].unsqueeze(2).to_broadcast([128, 4, 8])
                    TT("dve", OH1[:, oi * 32:(oi + 1) * 32].rearrange("p (g e) -> p g e", g=4),
                       TL[:, 32:40].unsqueeze(1).to_broadcast([128, 4, 8]), g48, ALU.mult, [bTL] + bR, [bOH1])
                    TT("dve", OH2[:, oi * 32:(oi + 1) * 32].rearrange("p (g e) -> p g e", g=4),
                       TL[:, 48:56].unsqueeze(1).to_broadcast([128, 4, 8]), g48, ALU.mult, [bTL] + bR, [bOH2])
                    CP("dve", W12[:].rearrange("p (r t) -> p r t", r=2)[:, :, oi], R[:, 122:124], bR, [bW12])
                    yield

                tiles = list(tile_list)
                s1g = {}
                nxt = 0
                active = []

                def start_s1():
                    nonlocal nxt
                    g = stage1(tiles[nxt])
                    s1g[nxt] = g
                    active.append(g)
                    nxt += 1

                def step(g):
                    try:
                        next(g)
                        return True
                    except StopIteration:
                        if g in active:
                            active.remove(g)
                        return False
                for n, i in enumerate(tiles):
                    if nxt <= n:
                        start_s1()
                    g1 = s1g[n]
                    while g1 in active:
                        step(g1)
                    g2 = stage2(i)
                    active.append(g2)
                    while nxt < len(tiles) and nxt <= n + nset:
                        start_s1()
                    while g2 in active:
                        for g in list(active):
                            step(g)
                while active:
                    for g in list(active):
                        step(g)
                P.emit()

            tl_all = list(CFG['tiles']) if CFG['tiles'] is not None else list(range(NT))
            tl_pre = [t for t in tl_all if t < OWN0 - 1]
            tl_own = [t for t in tl_all if t >= OWN0 - 1]
            mixer_phase(tl_pre, CFG.get('nset', 2), False, True)
            P.barrier()
            mixer_phase(tl_own, 1, True, False)
            P.barrier()

        esE = ExitStack()
        with esE:
            sbE, psE = mk(esE)
            identE, bidE = sbE("m_identE", [128, 128], BF16)
            UsE, bUsE = sbE("m_UsE", [128, 128], BF16)
            onesE, bonesE = sbE("m_onesE", [128, 128], BF16)
            Asb, bAsb = sbE("m_Asb", [128, 2048])
            Bx, bBx = sbE("m_Bx", [128, 65 * 32])
            srt, bsrt = sbE("m_srt", [128, 256])
            E3, bE3 = sbE("m_E3", [128, NTL * 32])
            posf, bposf = sbE("m_posf", [128, 64]); posi, bposi = sbE("m_posi", [128, 64], I32)
            META, bMETA = sbE("m_META", [128, 256], I32)
            minit, bminit = sbE("m_minit", [128, NSUB * 4], I32)
            metaS, _bms = sbE("m_metaS", [128, NSUB * 4], I32)
            thr, bthr = sbE("m_thr", [128, NTL]); pcol, bpcol = sbE("m_pcol", [128, 1])
            idxw, bidxw = sbE("m_idxw", [128, 64], I32)
            DMA(identE[:], c_ident[:, :], [], [bidE]); DMA(UsE[:], c_Us[:, :], [], [bUsE])
            P.op("pool", lambda e: e.memset(onesE[:], 1.0), [], [bonesE])
            esS = ExitStack()
            with esS:
                _, psS = mk(esS)
                pA = [psS("m_pA%d" % i, [128, 512]) for i in range(4)]
                pB = [psS("m_pB%d" % i, [128, 512]) for i in range(4)]
                for v in range(64):
                    OHt, bOHt = (OH1, bOH1) if v < 32 else (OH2, bOH2)
                    rhs = OHt[:, (v % 32) * 32:(v % 32 + 1) * 32]
                    a_, ba_ = pA[v // 16]; b_, bb_ = pB[v // 16]
                    c0 = (v % 16) * 32
                    MM(a_[:, c0:c0 + 32], UsE[:], rhs, True, True, [bUsE, bOHt], [ba_])
                    MM(b_[:, c0:c0 + 32], onesE[:], rhs, True, True, [bonesE, bOHt], [bb_])
                for i in range(4):
                    ACT(Asb[:, i * 512:(i + 1) * 512], pA[i][0][:], AF.Copy, [pA[i][1]], [bAsb])
                    CP("dve", Bx[:, 32 + i * 512:32 + (i + 1) * 512], pB[i][0][:], [pB[i][1]], [bBx])
                P.emit()
            P.barrier()
            P.op("pool", lambda e: e.memset(Bx[:, 0:32], 0.0), [], [bBx])
            for v in range(2, 65):
                TT("dve", Bx[:, v * 32:(v + 1) * 32], Bx[:, v * 32:(v + 1) * 32], Bx[:, (v - 1) * 32:v * 32], ALU.add, [bBx], [bBx])
            cnt = Bx[:, 2048:2080]
            nt_ = srt[:, 0:32]; pc_ = srt[:, 32:64]; st_ = srt[:, 64:96]; en_ = srt[:, 96:128]
            TS(nt_, cnt, 0.0, None, ALU.is_gt, None, [bBx], [bsrt])
            for jj in range(1, 8):
                STT(nt_, cnt, 512.0 * jj, nt_, ALU.is_gt, ALU.add, [bBx, bsrt], [bsrt])
            TS(pc_, nt_, 512.0, None, ALU.mult, None, [bsrt], [bsrt])
            P.op("pool", lambda e: e.memset(srt[:, 64:65], 0.0), [bsrt], [bsrt])
            for ee in range(1, 32):
                TT("dve", st_[:, ee:ee + 1], st_[:, ee - 1:ee], pc_[:, ee - 1:ee], ALU.add, [bsrt], [bsrt])
            TT("dve", en_, st_, pc_, ALU.add, [bsrt], [bsrt])
            A3 = Asb[:].rearrange("p (v e) -> p v e", e=32)
            TT("dve", Asb[:], Asb[:], Bx[:, 0:2048], ALU.add, [bAsb, bBx], [bAsb])
            TT("dve", A3, A3, st_.unsqueeze(1).to_broadcast([128, 64, 32]), ALU.add, [bAsb, bsrt], [bAsb])
            TT("dve", Asb[:, 0:1024], Asb[:, 0:1024], OH1[:], ALU.mult, [bAsb, bOH1], [bAsb])
            TT("dve", Asb[:, 1024:2048], Asb[:, 1024:2048], OH2[:], ALU.mult, [bAsb, bOH2], [bAsb])
            RED(posf[:], A3, ALU.add, [bAsb], [bposf])
            CP("dve", posi[:], posf[:], [bposf], [bposi])
            DMA(thr[:], c_thr[:, :], [], [bthr]); DMA(pcol[:], c_pcol[:, :], [], [bpcol])
            E33 = E3[:].rearrange("p (j e) -> p j e", e=32)
            TT("dve", E33, en_.unsqueeze(1).to_broadcast([128, NTL, 32]), thr[:].unsqueeze(2).to_broadcast([128, NTL, 32]), ALU.is_le,
               [bsrt, bthr], [bE3])
            bsrt2 = Buf("srt2")
            RED(srt[:, 128:128 + NTL], E33, ALU.add, [bE3], [bsrt2])
            TS(srt[:, 192:192 + NTL], srt[:, 128:128 + NTL], 128.0, pcol[:, 0:1], ALU.mult, ALU.add, [bsrt2, bpcol], [bsrt2])
            CP("dve", idxw[:, 0:NTL], srt[:, 192:192 + NTL], [bsrt2], [bidxw])
            DMA(META[:], c_meta0[:, :], [], [bMETA])
            MF = META[:].bitcast(F32).rearrange("p (v c) -> p v c", c=4)
            CP("dve", MF[:, :, 1], W12[:], [bW12, bMETA], [bMETA])
            bminit_d = Buf("minit_d")
            DMA(minit[:], c_minit[:, :], [], [bminit])
            DMA(meta_d.rearrange("(p n) c -> p (n c)", p=128), minit[:], [bminit], [bminit_d])

            bndt, bbndt = sbE("m_bndt", [128, 4], I32)
            DMA(bndt[:], c_bnd[:, :], [], [bbndt])
            bregs = Buf("bregs")
            REG = {}
            for bi_, bv_ in enumerate((4095, 8191, NSLOT - 1)):
                REG[bv_] = nc.alloc_registers("bnd%d" % bi_, engines=[mybir.EngineType.Pool])
                P.op("pool", (lambda rg, ap_: (lambda e: nc.regs_load(rg, ap_)[-1]))(REG[bv_], bndt[0:1, bi_:bi_ + 1]), [bbndt, bregs], [bregs], cost=300)

            def IGATHER(out, src_, idx_ap, bound, r, w, sembuf, nbytes):
                P.dma(lambda e: e.indirect_dma_start(out=out, out_offset=None, in_=src_,
                                                    in_offset=bass.IndirectOffsetOnAxis(ap=idx_ap, axis=0),
                                                    bounds_check=REG[bound], oob_is_err=False),
                      list(r) + [bregs], w, sembuf, eng="pool", cost=2500 + nbytes / 150.0, issue=1200.0)

            def ISCATTER(dst, idx_ap, src_, bound, r, w, sembuf, nbytes):
                def f_(e):
                    try:
                        return e.indirect_dma_start(out=dst, out_offset=bass.IndirectOffsetOnAxis(ap=idx_ap, axis=0),
                                                    in_=src_, in_offset=None, bounds_check=REG[bound], oob_is_err=False)
                    except Exception:
                        print("ISCATTER fail", dst.shape, dst.dtype, src_.shape, src_.dtype, idx_ap.shape, idx_ap.dtype, bound)
                        raise
                P.dma(f_, list(r) + [bregs], w, sembuf, eng="pool", cost=2500 + nbytes / 150.0, issue=1200.0)
            bmsc = Buf("msc")
            bmsv = [Buf("msv%d" % v) for v in range(64)]
            for v in range(64):
                ISCATTER(meta_d[:, :], posi[:, v:v + 1], META[:, v * 4:(v + 1) * 4], NSLOT - 1, [bposi, bMETA, bminit_d], [bmsv[v]], bmsc, 2048)
            bmS = [Buf("metaS%d" % q) for q in range(4)]
            bmSs = Buf("metaSs")
            for q in range(4):
                DMA(metaS[:, q * NTL * 4:(q + 1) * NTL * 4].rearrange("p (t c) -> p t c", c=4),
                    meta_d[q * NTL * 128:(q + 1) * NTL * 128, :].rearrange("(t p) c -> p t c", p=128), bmsv + [bminit_d], [bmS[q]], bmSs)
            MSI = metaS[:].rearrange("p (t c) -> p t c", c=4)
            MSF = metaS[:].bitcast(F32).rearrange("p (t c) -> p t c", c=4)

            Wg = [sbE("m_Wg%d" % i, [128, 8, 256], BF16) for i in range(3)]
            Wu = [sbE("m_Wu%d" % i, [128, 8, 256], BF16) for i in range(3)]
            Wd = [sbE("m_Wd%d" % i, [128, 2, 1024], BF16) for i in range(3)]
            Xg = [sbE("m_Xg%d" % i, [128, 4, 1024], BF16)[0] for i in range(3)]
            bXg = [[Buf("Xg%d_%d" % (i, s)) for s in range(4)] for i in range(3)]
            bXgs = [Buf("Xgs%d" % i) for i in range(3)]
            h2s = [sbE("m_h2s%d" % i, [128, 8, 512], BF16) for i in range(2)]
            sg = [sbE("m_sg%d" % i, [128, 512], BF16) for i in range(2)]
            hid = [sbE("m_hid%d" % i, [128, 512], BF16) for i in range(4)]
            yo = [sbE("m_yo%d" % i, [128, 1024]) for i in range(3)]
            xo = [sbE("m_xo%d" % i, [128, 1024]) for i in range(2)]
            c0b = [sbE("m_c0b%d" % i, [128, 1024]) for i in range(2)]
            c1b = [sbE("m_c1b%d" % i, [128, 1024]) for i in range(2)]
            pT2 = [psE("m_pT2_%d" % i, [128, 1024], BF16) for i in range(2)]
            pg_ = [psE("m_pg%d" % i, [128, 512]) for i in range(2)]
            pu_ = [psE("m_pu%d" % i, [128, 512]) for i in range(2)]
            pd, bpd = psE("m_pd", [128, 1024])
            print("SBUF remaining (moe)", nc.sbuf_bytes_remaining)
            for i in range(3):
                for s in range(4):
                    P.op("pool", (lambda i, s: (lambda e: e.memset(Xg[i][:, s, :], 0.0)))(i, s), [], [bXg[i][s]], cost=1500)
            bco = [Buf("co%d" % u) for u in range(NSUB)]

            def w_gather(j):
                sl = j % 3
                IGATHER(Wg[sl][0][:].rearrange("p k n -> p (k n)"), wg_l[:, :], idxw[:, j:j + 1], 4095, [bidxw], [Wg[sl][1]], None, 1 << 20)
                IGATHER(Wu[sl][0][:].rearrange("p k n -> p (k n)"), wu_l[:, :], idxw[:, j:j + 1], 4095, [bidxw], [Wu[sl][1]], None, 1 << 20)
                IGATHER(Wd[sl][0][:].rearrange("p k n -> p (k n)"), wd_l[:, :], idxw[:, j:j + 1], 4095, [bidxw], [Wd[sl][1]], None, 1 << 20)

            def x_gather(j):
                b3 = j % 3
                for s in range(4):
                    u = 4 * j + s
                    IGATHER(Xg[b3][:, s, :], h2r_d[:, :], MSI[:, u, 0:1], 4095, bmS, [bXg[b3][s]], None, 1 << 18)
            NTR = CFG.get('ntl', NTL)
            for j in range(min(2, NTR)):
                w_gather(j)
                x_gather(j)
            for j in range(NTR):
                if j + 2 < NTR:
                    w_gather(j + 2)
                    x_gather(j + 2)
                sl = j % 3
                wg_, bwg_ = Wg[sl]; wu_, bwu_ = Wu[sl]; wd_, bwd_ = Wd[sl]
                xg_ = Xg[sl]
                h2s_, bh2s_ = h2s[j % 2]
                for kp in range(4):
                    pt, bpt = pT2[kp % 2]
                    for kk in range(2):
                        k = kp * 2 + kk
                        for s in range(4):
                            TR(pt[:, kk * 512 + s * 128:kk * 512 + (s + 1) * 128], xg_[:, s, k * 128:(k + 1) * 128], identE[:],
                               bXg[sl] + [bidE], [bpt])
                    for kk in range(2):
                        k = kp * 2 + kk
                        ACT(h2s_[:, k, :], pt[:, kk * 512:(kk + 1) * 512], AF.Identity, [bpt, bmodc], [bh2s_],
                            scale=modc[:, 16 + k:17 + k], bias=modc[:, 24 + k:25 + k])
                for fc in range(2):
                    pgx, bpgx = pg_[fc]; pux, bpux = pu_[fc]
                    for k in range(8):
                        MM(pgx[:], wg_[:, k, fc * 128:(fc + 1) * 128], h2s_[:, k, :], k == 0, k == 7, [bwg_, bh2s_], [bpgx])
                    for k in range(8):
                        MM(pux[:], wu_[:, k, fc * 128:(fc + 1) * 128], h2s_[:, k, :], k == 0, k == 7, [bwu_, bh2s_], [bpux])
                    s_, bs_ = sg[fc]
                    hd, bhd = hid[(j % 2) * 2 + fc]
                    ACT(s_[:], pgx[:], AF.Silu, [bpgx], [bs_])
                    TT("dve", hd[:], pux[:], s_[:], ALU.mult, [bs_, bpux], [bhd])
                for t in range(4):
                    u = 4 * j + t
                    for nh_ in range(2):
                        for fc in range(2):
                            hd, bhd = hid[(j % 2) * 2 + fc]
                            MM(pd[:, nh_ * 512:(nh_ + 1) * 512], hd[:, t * 128:(t + 1) * 128], wd_[:, fc, nh_ * 512:(nh_ + 1) * 512], fc == 0, fc == 1,
                               [bhd, bwd_], [bpd])
                    yo_, byo_ = yo[u % 3]
                    STT(yo_[:], pd[:], MSF[:, u, 1:2], G2[:], ALU.mult, ALU.mult, [bpd, bG2] + bmS, [byo_])
                    ISCATTER(contrib_d[:, :], MSI[:, u, 2:3], yo_[:], 8191, [byo_] + bmS, [bco[u]], byo_, 1 << 19)
            for oi in range(32):
                xo_, bxo_ = xo[oi % 2]; c0_, bc0_ = c0b[oi % 2]; c1_, bc1_ = c1b[oi % 2]
                DMA(xo_[:], y[oi * 128:(oi + 1) * 128, :], [by_d[oi]], [bxo_])
                DMA(c0_[:], contrib_d[oi * 128:(oi + 1) * 128, :], bco, [bc0_], eng="act")
                DMA(c1_[:], contrib_d[4096 + oi * 128:4096 + (oi + 1) * 128, :], bco, [bc1_], eng="act")
                TT("dve", c0_[:], c0_[:], c1_[:], ALU.add, [bc0_, bc1_], [bc0_])
                TT("dve", xo_[:], xo_[:], c0_[:], ALU.add, [bxo_, bc0_], [bxo_])
                DMA(y[oi * 128:(oi + 1) * 128, :], xo_[:], [bxo_], [by_d[oi]], ysem2[oi % 2], eng="pool")
            P.wait_all("sp", by_d)
            P.emit()
    except Cut:
        pass
    return nc


def _consts():
    idx = np.arange(128)
    same = (idx[:, None] // 64) == (idx[None, :] // 64)
    U = (same & (idx[:, None] <= idx[None, :])).astype(np.float32)
    B = same.astype(np.float32)
    ind = np.zeros((128, 2, 128), np.float32)
    ind[:64, 0, :] = 1.0
    ind[64:, 1, :] = 1.0
    mincl = np.where(same & (idx[None, :] >= idx[:, None]), 0.0, -30000.0).astype(np.float32)
    mslow = np.where(same & (idx[None, :] < idx[:, None]), 0.0, -30000.0).astype(np.float32)
    m01 = np.zeros((128, 2, 128), np.float32)
    m01[:, 0, :] = (idx[:, None] > idx[None, :])
    m01[:, 1, :] = (idx[:, None] <= idx[None, :])
    half = 32
    invf = (10000.0 ** (-np.arange(half, dtype=np.float32) / half)).astype(np.float32)
    Us = (idx[:, None] < idx[None, :]).astype(np.float32).astype(NPBF)
    meta0 = np.zeros((128, 64, 4), np.int32)
    vv = np.arange(64)
    meta0[:, :, 0] = (vv[None, :] % 32) * 128 + idx[:, None]
    meta0[:, :, 2] = (vv[None, :] // 32) * 4096 + (vv[None, :] % 32) * 128 + idx[:, None]
    extra = dict(c_Us=Us, c_meta0=meta0.reshape(128, 256), c_minit=np.full((128, NSUB * 4), 1 << 30, np.int32),
                 c_thr=np.ascontiguousarray(np.broadcast_to((512.0 * np.arange(NTL, dtype=np.float32))[None, :], (128, NTL))),
                 c_pcol=idx.astype(np.float32).reshape(128, 1),
                 c_bnd=np.ascontiguousarray(np.broadcast_to(np.array([4095, 8191, NSLOT - 1, 0], np.int32)[None, :], (128, 4))))
    return dict(c_ident=np.eye(128, dtype=np.float32).astype(NPBF), c_U=U, c_B=B, c_ind=ind.reshape(128, 256), **extra,
                c_mincl=mincl, c_mslow=mslow, c_blk=B.astype(NPBF), c_m01=m01.reshape(128, 256).astype(NPBF),
                c_invf=np.ascontiguousarray(np.broadcast_to(invf[None, :], (128, 32))))


_NC_CACHE = {}


def kernel(x, c, positions, w_ada, b_ada, norm_mix, w_in, conv_w, a_log, dt_bias, gdn_out_norm, q_norm, k_norm,
           sinks, w_out, norm_ffn, w_group, b_group, w_router, b_router, w_gate, w_up, w_down):
    f = lambda a: np.ascontiguousarray(np.asarray(a, dtype=np.float32))
    x = f(x); c = f(c); positions = np.asarray(positions).astype(np.int32)
    if "nc" not in _NC_CACHE:
        _NC_CACHE["nc"] = build(DBG)
    nc = _NC_CACHE["nc"]
    consts = _consts()
    shared = dict(
        w_ada=f(w_ada)[0], b_ada=f(b_ada), w_in=f(w_in)[0],
        b_ada_col=np.ascontiguousarray(f(b_ada).reshape(48, 128).T), nm_col=np.ascontiguousarray(f(norm_mix).reshape(8, 128).T),
        nf_col=np.ascontiguousarray(f(norm_ffn).reshape(8, 128).T),
        conv_wT=np.ascontiguousarray(f(conv_w)[0].T.reshape(12, 128, 4).transpose(1, 0, 2).reshape(128, 48)),
        a_log=f(a_log), dt_bias=f(dt_bias), gon=f(gdn_out_norm), qnw=f(q_norm), knw=f(k_norm), sinks=f(sinks),
        w_out=f(w_out)[0],
        w_gr=np.ascontiguousarray(np.concatenate([f(w_group)[0], f(w_router)[0]], axis=1)),
        b_gr=np.ascontiguousarray(np.concatenate([f(b_group), f(b_router)], axis=1)),
        wg_l=np.ascontiguousarray(f(w_gate)[0].reshape(32, 8, 128, 256).transpose(0, 2, 1, 3).reshape(4096, 2048)),
        wu_l=np.ascontiguousarray(f(w_up)[0].reshape(32, 8, 128, 256).transpose(0, 2, 1, 3).reshape(4096, 2048)),
        wd_l=np.ascontiguousarray(f(w_down)[0].reshape(32, 2, 128, 1024).transpose(0, 2, 1, 3).reshape(4096, 2048)),
        **consts)
    in_maps = []
    for core in range(8):
        b, half = core // 2, core % 2
        own = x[b, half * 4096:(half + 1) * 4096]
        xg = np.ascontiguousarray(np.concatenate([x[b, 0:4096], own], axis=0))
        if half == 1:
            pp = positions[b, 4096 - 128:8192]
        else:
            pp = np.concatenate([positions[b, 0:128], positions[b, 0:4096]])
        pos = np.ascontiguousarray(pp.reshape(33, 128).T)
        m = dict(shared)
        m.update(xg=xg, c_col=np.ascontiguousarray(c[b].reshape(8, 128).T), pos=pos,
                 flag=np.full((128, 1), float(half), np.float32))
        in_maps.append(m)
    res = run_bass_kernel_spmd(nc, in_maps, core_ids=list(range(8)))
    out = np.zeros((4, 8192, 1024), np.float32)
    for core in range(8):
        b, half = core // 2, core % 2
        out[b, half * 4096:(half + 1) * 4096] = res.results[core]["y"]
    if DBG:
        kernel.dbg = [res.results[core].get("dbg_o") for core in range(8)]
    return out
```
